# Optimizing a Trainium2 kernel written in Bass

```python
import math
import jax
import jax.numpy as jnp
from jax import lax
import numpy as np

D_MODEL = 2048
BATCH = 2
SEQ = 4096
DEPTH = 2

N_MEM = 256
MIX_A = D_MODEL // 4
MIX_B = D_MODEL // 4
MIX_C = D_MODEL // 4
MIX_D = D_MODEL - MIX_A - MIX_B - MIX_C
MIX_WIDTH = MIX_A + MIX_B + MIX_C + MIX_D

A_HEADS = 4
A_VDIM = MIX_A // A_HEADS
A_QKDIM = A_VDIM // 2
B_HDIM = 64
B_HEADS = MIX_B // B_HDIM
B_DECAY_LORA = 64
B_AAA_LORA = 64
B_GATE_LORA = 128
LN_X_EPS = 64e-5
C_HEADS = 4
C_HDIM = MIX_C // C_HEADS
C_PATTERNS = ((128, 1), (512, 4), (2048, 16))
D_GSIZE = 16
D_GROUPS = MIX_D // D_GSIZE
D_STATE = 64

IN_A = 3 * MIX_A
IN_B = 3 * MIX_B + 2 * B_DECAY_LORA + 2 * B_AAA_LORA + B_GATE_LORA
IN_C = 3 * MIX_C
IN_D = MIX_D
IN_TOTAL = IN_A + IN_B + IN_C + IN_D

XA_HEADS = 4
XA_HDIM = D_MODEL // XA_HEADS

N_GROUPS = 4
EXPERTS_PER_GROUP = 8
N_EXPERTS = N_GROUPS * EXPERTS_PER_GROUP
TOP_K = 2
D_EXPERT = D_MODEL // 4
MOE_BLOCK = 128

ROPE_THETA = 10000.0
Q_BLOCK = 128
RMS_EPS = 1e-6
NEG_INF = -1e30

kernel_name = 'hybrid_parallel_heads_encoder'

F32 = jnp.float32


def rms_norm(x, g, eps=RMS_EPS):
    xf = x.astype(F32)
    y = xf * lax.rsqrt(jnp.mean(xf * xf, axis=-1, keepdims=True) + eps)
    return (y * g.astype(F32)).astype(x.dtype)


def rope_tables(seq, dim):
    inv = 1.0 / (ROPE_THETA ** (jnp.arange(0, dim, 2, dtype=F32) / dim))
    ang = jnp.arange(seq, dtype=F32)[:, None] * inv[None, :]
    return jnp.cos(ang), jnp.sin(ang)


def apply_rope(x, cos, sin):
    xf = x.astype(F32)
    x1, x2 = jnp.split(xf, 2, axis=-1)
    return jnp.concatenate([x1 * cos - x2 * sin, x2 * cos + x1 * sin], axis=-1).astype(x.dtype)


def diff_attention(z, lam_vecs, subln_g, layer_idx):
    b, s, _ = z.shape
    q, k, v = jnp.split(z, 3, axis=-1)
    q = q.reshape(b, s, A_HEADS, 2, A_QKDIM).transpose(0, 2, 3, 1, 4)
    k = k.reshape(b, s, A_HEADS, 2, A_QKDIM).transpose(0, 2, 3, 1, 4)
    v = v.reshape(b, s, A_HEADS, A_VDIM).transpose(0, 2, 1, 3)
    cos, sin = rope_tables(s, A_QKDIM)
    q = apply_rope(q, cos, sin)
    k = apply_rope(k, cos, sin)
    lam_init = 0.8 - 0.6 * math.exp(-0.3 * layer_idx)
    lf = lam_vecs.astype(F32)
    lam = jnp.exp(jnp.sum(lf[0] * lf[1])) - jnp.exp(jnp.sum(lf[2] * lf[3])) + lam_init
    scale = A_QKDIM ** -0.5
    nb = s // Q_BLOCK
    qb = q.reshape(b, A_HEADS, 2, nb, Q_BLOCK, A_QKDIM).transpose(3, 0, 1, 2, 4, 5)

    def block(qi):
        sc = jnp.einsum('bhcqd,bhckd->bhcqk', qi, k).astype(F32) * scale
        p = jax.nn.softmax(sc, axis=-1)
        attn = p[:, :, 0] - lam * p[:, :, 1]
        return jnp.einsum('bhqk,bhkd->bhqd', attn.astype(v.dtype), v)

    o = lax.map(block, qb)
    o = o.transpose(1, 0, 3, 2, 4).reshape(b, s, A_HEADS, A_VDIM)
    o = rms_norm(o.astype(F32), subln_g) * (1.0 - lam_init)
    return o.reshape(b, s, MIX_A)


def token_shift_centred(z, mu):
    zp = jnp.pad(z, ((0, 0), (1, 0), (0, 0)))[:, :-1]
    zn = jnp.pad(z, ((0, 0), (0, 1), (0, 0)))[:, 1:]
    return z + mu[0] * (zp - z) + mu[1] * (zn - z)


def rwkv7_bidir(z, mu, w0, w2, a0, a2, g2, k_k, k_a, r_k, lnx_w, lnx_b):
    b, s, _ = z.shape
    z = token_shift_centred(z.astype(F32), mu.astype(F32))
    cuts = [MIX_B, 2 * MIX_B, 3 * MIX_B, 3 * MIX_B + 2 * B_DECAY_LORA,
            3 * MIX_B + 2 * B_DECAY_LORA + 2 * B_AAA_LORA]
    r, k, v, wd, ad, gd = jnp.split(z, cuts, axis=-1)
    wd = wd.reshape(b, s, 2, B_DECAY_LORA)
    ad = ad.reshape(b, s, 2, B_AAA_LORA)
    logw = -jax.nn.softplus(-(w0 + jnp.einsum('bsdr,drc->bsdc', jnp.tanh(wd), w2))) - 0.5
    decay = jnp.exp(-jnp.exp(logw))
    a = jax.nn.sigmoid(a0 + jnp.einsum('bsdr,drc->bsdc', ad, a2))
    g = jax.nn.sigmoid(gd) @ g2

    def heads(t):
        return t.reshape(t.shape[:-1] + (B_HEADS, B_HDIM))

    kk = heads(k * k_k)
    kk = kk * lax.rsqrt(jnp.sum(kk * kk, axis=-1, keepdims=True) + 1e-12)
    kmod = k[:, :, None, :] * (1.0 + (a - 1.0) * k_a)
    r_h, v_h = heads(r), heads(v)
    a_h, w_h, k_h = heads(a), heads(decay), heads(kmod)

    def dirs(t):
        t = jnp.broadcast_to(t, (b, s, 2, B_HEADS, B_HDIM))
        t = jnp.stack([t[:, :, 0], jnp.flip(t[:, :, 1], axis=1)], axis=0)
        return jnp.moveaxis(t, 2, 0)

    xs = (dirs(r_h[:, :, None]), dirs(w_h), dirs(k_h), dirs(v_h[:, :, None]),
          dirs(-kk[:, :, None]), dirs(kk[:, :, None] * a_h))

    def step(state, inp):
        rt, wt, kt, vt, at, bt = inp
        sa = jnp.einsum('dbhij,dbhj->dbhi', state, at)
        state = state * wt[..., None, :] + sa[..., :, None] * bt[..., None, :] + vt[..., :, None] * kt[..., None, :]
        return state, jnp.einsum('dbhij,dbhj->dbhi', state, rt)

    s0 = jnp.zeros((2, b, B_HEADS, B_HDIM, B_HDIM), F32)
    _, ys = lax.scan(step, s0, xs)
    y = jnp.moveaxis(ys[:, 0] + jnp.flip(ys[:, 1], axis=0), 0, 1)
    mean = jnp.mean(y, axis=-1, keepdims=True)
    var = jnp.mean(jnp.square(y - mean), axis=-1, keepdims=True)
    y = ((y - mean) * lax.rsqrt(var + LN_X_EPS)).reshape(b, s, MIX_B) * lnx_w + lnx_b
    k_bonus = heads(0.5 * (kmod[:, :, 0] + kmod[:, :, 1]))
    bonus = jnp.sum(r_h * k_bonus * r_k, axis=-1, keepdims=True) * v_h
    return (y + bonus.reshape(b, s, MIX_B)) * g


def dilated_attention(z):
    b, s, _ = z.shape
    q, k, v = jnp.split(z, 3, axis=-1)
    q = q.reshape(b, s, C_HEADS, C_HDIM).transpose(0, 2, 1, 3)
    k = k.reshape(b, s, C_HEADS, C_HDIM).transpose(0, 2, 1, 3)
    v = v.reshape(b, s, C_HEADS, C_HDIM).transpose(0, 2, 1, 3)
    cos, sin = rope_tables(s, C_HDIM)
    q = apply_rope(q, cos, sin)
    k = apply_rope(k, cos, sin)
    offs = jnp.asarray(np.stack([np.arange(-(w // (2 * d)), w // (2 * d) + 1) * d
                                 for w, d in C_PATTERNS]), jnp.int32)
    scale = C_HDIM ** -0.5
    nb = s // Q_BLOCK
    qb = q.reshape(b, C_HEADS, nb, Q_BLOCK, C_HDIM).transpose(2, 0, 1, 3, 4)
    starts = jnp.arange(nb, dtype=jnp.int32) * Q_BLOCK

    def block(args):
        qi, st = args
        pos = st + jnp.arange(Q_BLOCK, dtype=jnp.int32)
        idx = pos[None, :, None] + offs[:, None, :]
        valid = (idx >= 0) & (idx < s)
        idx = jnp.clip(idx, 0, s - 1)
        kg = k[:, :, idx]
        vg = v[:, :, idx]
        sc = jnp.einsum('bhqd,bhpqkd->bhpqk', qi, kg).astype(F32) * scale
        sc = jnp.where(valid, sc, NEG_INF)
        m = jnp.max(sc, axis=-1, keepdims=True)
        e = jnp.exp(sc - m)
        den = jnp.sum(e, axis=-1, keepdims=True)
        o = jnp.einsum('bhpqk,bhpqkd->bhpqd', (e / den).astype(v.dtype), vg).astype(F32)
        lse = (m + jnp.log(den))[..., 0]
        wgt = jax.nn.softmax(lse, axis=2)
        return jnp.einsum('bhpq,bhpqd->bhqd', wgt, o)

    o = lax.map(block, (qb, starts))
    return o.transpose(1, 0, 3, 2, 4).reshape(b, s, MIX_C)


def cmul(ar, ai, br, bi):
    return ar * br - ai * bi, ar * bi + ai * br


def s5_combine(e1, e2):
    a1r, a1i, x1r, x1i = e1
    a2r, a2i, x2r, x2i = e2
    ar, ai = cmul(a2r, a2i, a1r, a1i)
    xr, xi = cmul(a2r, a2i, x1r, x1i)
    return ar, ai, xr + x2r, xi + x2i


def s5_bidir(u, a_re, a_im, log_dt, b_re, b_im, c_re, c_im, d_skip, glu_w, glu_b):
    b, s, _ = u.shape
    uf = u.astype(F32)
    ug = uf.reshape(b, s, D_GROUPS, D_GSIZE)
    b_re, b_im = b_re.astype(F32), b_im.astype(F32)
    x_re = jnp.zeros((b, s, D_GROUPS, D_STATE), F32)
    x_im = jnp.zeros((b, s, D_GROUPS, D_STATE), F32)
    for direction in range(2):
        lr = jnp.minimum(a_re[direction].astype(F32), -1e-4)
        li = a_im[direction].astype(F32)
        dt = jnp.exp(log_dt[direction].astype(F32))[:, None]
        mag = jnp.exp(dt * lr)
        abr, abi = mag * jnp.cos(dt * li), mag * jnp.sin(dt * li)
        den = lr * lr + li * li
        fr, fi = cmul(abr - 1.0, abi, lr / den, -li / den)
        bbr, bbi = cmul(fr[..., None], fi[..., None], b_re, b_im)
        bur = jnp.einsum('gnc,bsgc->bsgn', bbr, ug)
        bui = jnp.einsum('gnc,bsgc->bsgn', bbi, ug)
        ar = jnp.broadcast_to(abr, bur.shape)
        ai = jnp.broadcast_to(abi, bur.shape)
        _, _, xr, xi = lax.associative_scan(s5_combine, (ar, ai, bur, bui),
                                            reverse=(direction == 1), axis=1)
        x_re = x_re + xr
        x_im = x_im + xi
    y = (jnp.einsum('gcn,bsgn->bsgc', c_re.astype(F32), x_re)
         - jnp.einsum('gcn,bsgn->bsgc', c_im.astype(F32), x_im))
    y = y.reshape(b, s, MIX_D) + d_skip * uf
    gl = jax.nn.gelu(y)
    return gl * jax.nn.sigmoid(gl @ glu_w + glu_b)


def cross_attention(h, memn, wq, wkv, wo):
    b, s, _ = h.shape
    m = memn.shape[1]
    q = (h @ wq).reshape(b, s, XA_HEADS, XA_HDIM)
    k, v = jnp.split(memn @ wkv, 2, axis=-1)
    k = k.reshape(b, m, XA_HEADS, XA_HDIM)
    v = v.reshape(b, m, XA_HEADS, XA_HDIM)
    sc = jnp.einsum('bshd,bmhd->bhsm', q, k).astype(F32) * (XA_HDIM ** -0.5)
    p = jax.nn.softmax(sc, axis=-1)
    o = jnp.einsum('bhsm,bmhd->bshd', p.astype(v.dtype), v).reshape(b, s, D_MODEL)
    return o @ wo


def hier_moe(h, wr_group, wr_expert, w1, w3, w2):
    b, s, d = h.shape
    t = b * s
    xt = h.reshape(t, d)
    gp = jax.nn.softmax((xt @ wr_group).astype(F32), axis=-1)
    g_idx = jnp.argmax(gp, axis=-1)
    g_w = jnp.take_along_axis(gp, g_idx[:, None], axis=-1)
    el = (xt @ wr_expert).astype(F32).reshape(t, N_GROUPS, EXPERTS_PER_GROUP)
    el = jnp.take_along_axis(el, g_idx[:, None, None], axis=1)[:, 0]
    ep = jax.nn.softmax(el, axis=-1)
    top_w, top_i = lax.top_k(ep, TOP_K)
    top_w = top_w / jnp.sum(top_w, axis=-1, keepdims=True)
    eid = (g_idx[:, None] * EXPERTS_PER_GROUP + top_i).reshape(-1).astype(jnp.int32)
    gate = (g_w * top_w).reshape(-1)
    tok = jnp.repeat(jnp.arange(t, dtype=jnp.int32), TOP_K)
    n_assign = t * TOP_K
    order = jnp.argsort(eid)
    se = eid[order]
    counts = jnp.bincount(eid, length=N_EXPERTS)
    padded = ((counts + MOE_BLOCK - 1) // MOE_BLOCK) * MOE_BLOCK
    pad_end = jnp.cumsum(padded)
    pad_start = pad_end - padded
    start = jnp.cumsum(counts) - counts
    dest = pad_start[se] + (jnp.arange(n_assign, dtype=jnp.int32) - start[se])
    cap = n_assign + N_EXPERTS * MOE_BLOCK
    nb = cap // MOE_BLOCK
    slot_tok = jnp.zeros((cap,), jnp.int32).at[dest].set(tok[order])
    slot_gate = jnp.zeros((cap,), F32).at[dest].set(gate[order])
    blk_exp = jnp.minimum(jnp.searchsorted(pad_end, jnp.arange(nb) * MOE_BLOCK, side='right'),
                          N_EXPERTS - 1)
    xs = xt[slot_tok].reshape(nb, MOE_BLOCK, d)

    def run(args):
        xb, e = args
        return (jax.nn.silu(xb @ w1[e]) * (xb @ w3[e])) @ w2[e]

    ys = lax.map(run, (xs, blk_exp)).reshape(cap, d)
    out = jnp.zeros((t, d), F32).at[slot_tok].add(ys.astype(F32) * slot_gate[:, None])
    return out.reshape(b, s, d).astype(h.dtype)


def setup_inputs(seed: int = 0) -> dict:
    key = jax.random.key(seed)
    keys = jax.random.split(key, 64)
    counter = [0]

    def nxt():
        counter[0] += 1
        return keys[counter[0] - 1]

    def nrm(shape, scale):
        return jax.random.normal(nxt(), shape, F32) * scale

    def unif(shape, lo, hi):
        return jax.random.uniform(nxt(), shape, F32, lo, hi)

    def gain(shape):
        return 1.0 + nrm(shape, 0.05)

    L, D = DEPTH, D_MODEL
    state_n = jnp.arange(D_STATE, dtype=F32)
    return {
        'x': nrm((BATCH, SEQ, D), 1.0),
        'mem': nrm((BATCH, N_MEM, D), 1.0),
        'norm_mix': gain((L, D)),
        'w_in': nrm((L, D, IN_TOTAL), D ** -0.5),
        'w_out': nrm((L, MIX_WIDTH, D), MIX_WIDTH ** -0.5),
        'diff_lambda': nrm((L, 4, A_QKDIM), 0.1),
        'diff_subln': gain((L, A_VDIM)),
        'rwkv_mu': unif((L, 2, IN_B), 0.0, 0.5),
        'rwkv_w0': unif((L, 2, MIX_B), -6.0, -1.0),
        'rwkv_w2': nrm((L, 2, B_DECAY_LORA, MIX_B), 0.1),
        'rwkv_a0': nrm((L, 2, MIX_B), 0.1),
        'rwkv_a2': nrm((L, 2, B_AAA_LORA, MIX_B), 0.1),
        'rwkv_g2': nrm((L, B_GATE_LORA, MIX_B), B_GATE_LORA ** -0.5),
        'rwkv_kk': 0.85 + nrm((L, MIX_B), 0.05),
        'rwkv_ka': gain((L, MIX_B)),
        'rwkv_rk': nrm((L, B_HEADS, B_HDIM), 0.1),
        'rwkv_lnx_w': gain((L, MIX_B)),
        'rwkv_lnx_b': nrm((L, MIX_B), 0.01),
        's5_a_re': -0.5 + nrm((L, 2, D_GROUPS, D_STATE), 0.02),
        's5_a_im': math.pi * state_n + nrm((L, 2, D_GROUPS, D_STATE), 0.02),
        's5_log_dt': unif((L, 2, D_GROUPS), math.log(0.001), math.log(0.1)),
        's5_b_re': nrm((L, D_GROUPS, D_STATE, D_GSIZE), (2 * D_GSIZE) ** -0.5),
        's5_b_im': nrm((L, D_GROUPS, D_STATE, D_GSIZE), (2 * D_GSIZE) ** -0.5),
        's5_c_re': nrm((L, D_GROUPS, D_GSIZE, D_STATE), (2 * D_STATE) ** -0.5),
        's5_c_im': nrm((L, D_GROUPS, D_GSIZE, D_STATE), (2 * D_STATE) ** -0.5),
        's5_d': nrm((L, MIX_D), 1.0),
        's5_glu_w': nrm((L, MIX_D, MIX_D), MIX_D ** -0.5),
        's5_glu_b': nrm((L, MIX_D), 0.01),
        'mix_out_norm': gain((L, 2, MIX_C)),
        'norm_cross': gain((L, D)),
        'norm_mem': gain((L, D)),
        'xa_wq': nrm((L, D, D), D ** -0.5),
        'xa_wkv': nrm((L, D, 2 * D), D ** -0.5),
        'xa_wo': nrm((L, D, D), D ** -0.5),
        'norm_moe': gain((L, D)),
        'router_group': nrm((L, D, N_GROUPS), D ** -0.5),
        'router_expert': nrm((L, D, N_EXPERTS), D ** -0.5),
        'moe_w1': nrm((L, N_EXPERTS, D, D_EXPERT), D ** -0.5),
        'moe_w3': nrm((L, N_EXPERTS, D, D_EXPERT), D ** -0.5),
        'moe_w2': nrm((L, N_EXPERTS, D_EXPERT, D), D_EXPERT ** -0.5),
        'norm_final': gain((D,)),
    }


def reference(x, mem, norm_mix, w_in, w_out, diff_lambda, diff_subln, rwkv_mu, rwkv_w0, rwkv_w2,
              rwkv_a0, rwkv_a2, rwkv_g2, rwkv_kk, rwkv_ka, rwkv_rk, rwkv_lnx_w, rwkv_lnx_b,
              s5_a_re, s5_a_im, s5_log_dt, s5_b_re, s5_b_im, s5_c_re, s5_c_im, s5_d, s5_glu_w,
              s5_glu_b, mix_out_norm, norm_cross, norm_mem, xa_wq, xa_wkv, xa_wo, norm_moe,
              router_group, router_expert, moe_w1, moe_w3, moe_w2, norm_final):
    h = x
    for l in range(DEPTH):
        z = rms_norm(h, norm_mix[l]) @ w_in[l]
        za, zb, zc, zd = jnp.split(z, [IN_A, IN_A + IN_B, IN_A + IN_B + IN_C], axis=-1)
        oa = diff_attention(za, diff_lambda[l], diff_subln[l], l)
        ob = rwkv7_bidir(zb, rwkv_mu[l], rwkv_w0[l], rwkv_w2[l], rwkv_a0[l], rwkv_a2[l], rwkv_g2[l],
                         rwkv_kk[l], rwkv_ka[l], rwkv_rk[l], rwkv_lnx_w[l], rwkv_lnx_b[l])
        oc = rms_norm(dilated_attention(zc).astype(F32), mix_out_norm[l, 0])
        od = rms_norm(s5_bidir(zd, s5_a_re[l], s5_a_im[l], s5_log_dt[l], s5_b_re[l], s5_b_im[l],
                               s5_c_re[l], s5_c_im[l], s5_d[l], s5_glu_w[l], s5_glu_b[l]),
                      mix_out_norm[l, 1])
        mix = jnp.concatenate([oa, ob, oc, od], axis=-1).astype(h.dtype)
        h = h + mix @ w_out[l]
        h = h + cross_attention(rms_norm(h, norm_cross[l]), rms_norm(mem, norm_mem[l]),
                                xa_wq[l], xa_wkv[l], xa_wo[l])
        h = h + hier_moe(rms_norm(h, norm_moe[l]), router_group[l], router_expert[l],
                         moe_w1[l], moe_w3[l], moe_w2[l])
    return rms_norm(h, norm_final)
```

```python
import math


import contextlib
import numpy as np
import concourse.bass as bass
import concourse.mybir as mybir

F32 = mybir.dt.float32
BF16 = mybir.dt.bfloat16
I32 = mybir.dt.int32
U32 = mybir.dt.uint32
AF = mybir.ActivationFunctionType
ALU = mybir.AluOpType
AX = mybir.AxisListType

ENGS = ("pe", "act", "dve", "pool", "sp")


class Buf:
    __slots__ = ("t", "last_w", "readers", "name")

    def __init__(self, t, name=""):
        self.t = t
        self.last_w = None
        self.readers = []
        self.name = name

    def __getitem__(self, k):
        return self.t[k]


class Op:
    __slots__ = ("eng", "fn", "deps", "marked", "tick", "is_dma", "dsem", "dval", "dwaits", "inc", "xw", "xs")

    def __init__(self, eng, fn, is_dma=False):
        self.eng = eng
        self.fn = fn
        self.deps = []
        self.dwaits = {}
        self.marked = False
        self.tick = 0
        self.is_dma = is_dma
        self.dsem = None
        self.dval = 0
        self.inc = 16
        self.xw = None
        self.xs = None


_GUID = [0]


class Prog:
    def __init__(self, nc, n_dma_sems=40):
        _GUID[0] += 1
        self.pid = _GUID[0]
        self.nc = nc
        self.st = contextlib.ExitStack()
        self.ops = {e: [] for e in ENGS}
        self.n_dma_sems = n_dma_sems
        self.dma_tot = [0] * n_dma_sems
        self.dma_rr = 0
        self.buf_sem = {}
        self.uid = 0
        self.coll = []

    def sb(self, shape, dt=F32, name=None):
        self.uid += 1
        t = self.st.enter_context(self.nc.sbuf_tensor(f"S{self.pid}_{self.uid}_" + (name or "sb"), list(shape), dt))
        return Buf(t, name or f"sb{self.uid}")

    def ps(self, shape, dt=F32, name=None):
        self.uid += 1
        t = self.st.enter_context(self.nc.psum_tensor(f"P{self.pid}_{self.uid}_" + (name or "ps"), list(shape), dt))
        return Buf(t, name or f"ps{self.uid}")

    def dram(self, name, shape, dt=F32, kind="Internal"):
        t = self.nc.dram_tensor(name, list(shape), dt, kind=kind)
        return Buf(t, name)

    def carve(self, parent, ap, name=""):
        b = Buf(ap, name)
        b.last_w = parent.last_w
        b.readers = list(parent.readers)
        return b

    def view(self, name=""):
        return Buf(None, name)

    def _dep_on(self, op, ev):
        if ev is None:
            return
        if ev.is_dma:
            s = ev.dsem
            op.dwaits[s] = max(op.dwaits.get(s, 0), self.dma_tot[s])
        else:
            if ev.eng == "pe" and op.eng == "pe":
                return
            op.deps.append(ev)
            ev.marked = True

    def op(self, eng, fn, reads=(), writes=()):
        o = Op(eng, fn)
        self._track(o, reads, writes)
        self.ops[eng].append(o)
        return o

    def _track(self, o, reads, writes):
        for b in reads:
            self._dep_on(o, b.last_w)
        for b in writes:
            self._dep_on(o, b.last_w)
            for r in b.readers:
                if r is not o and not (r.eng == o.eng and not r.is_dma):
                    self._dep_on(o, r)
        for b in writes:
            b.last_w = o
            b.readers = []
        for b in reads:
            if b.last_w is o:
                continue
            if not o.is_dma:
                b.readers = [r for r in b.readers if r.is_dma or r.eng != o.eng]
            b.readers.append(o)

    def dma(self, eng, out_ap, in_ap, reads=(), writes=(), sem_key=None, **kw):
        o = Op(eng, lambda e: e.dma_start(out=out_ap, in_=in_ap, **kw), is_dma=True)
        key = sem_key if sem_key is not None else (id(writes[0]) if writes else id(reads[0]))
        if key not in self.buf_sem:
            self.buf_sem[key] = self.dma_rr % self.n_dma_sems
            self.dma_rr += 1
        s = self.buf_sem[key]
        self._track(o, reads, writes)
        self.dma_tot[s] += 16
        o.dsem = s
        o.dval = self.dma_tot[s]
        self.ops[eng].append(o)
        return o

    def ext_wait(self, eng, sem, val):
        o = Op(eng, None)
        o.xw = (sem, val)
        self.ops[eng].append(o)
        return o

    def collective(self, kind, in_ap, out_ap, groups, reads=(), writes=(), op=None, ext_sem=None):
        alu = op if op is not None else mybir.AluOpType.bypass
        o = Op("pool", lambda e: e.collective_compute(kind, alu, replica_groups=groups, ins=[in_ap], outs=[out_ap]), is_dma=True)
        if ext_sem is not None:
            for b in reads:
                self._dep_on(o, b.last_w)
            o.xs = ext_sem
            o.dsem = None
            self.ops["pool"].append(o)
            return o
        s = self.n_dma_sems + len(self.coll)
        self.coll.append(o)
        self.dma_tot.append(0)
        self._track(o, reads, writes)
        self.dma_tot[s] += 1
        o.dsem = s
        o.dval = 1
        o.inc = 1
        self.ops["pool"].append(o)
        return o

    def wait_all_dma(self, eng="sp"):
        tot = list(self.dma_tot)
        o = Op(eng, None)
        o.dwaits = {s: v for s, v in enumerate(tot) if v > 0}
        self.ops[eng].append(o)

    def emit(self):
        nc = self.nc
        st = self.st
        esem = {e: nc.alloc_semaphore(name=f"es{self.pid}_{e}") for e in ENGS}
        dsem = [nc.alloc_semaphore(name=f"ds{self.pid}_{i}") for i in range(self.n_dma_sems + len(self.coll))]
        for e in ENGS:
            c = 0
            for o in self.ops[e]:
                if o.marked and not o.is_dma:
                    c += 1
                    o.tick = c
        ops = self.ops

        def replay(ename, h):
            known = {}
            for o in ops[ename]:
                for p in o.deps:
                    k = ("e", p.eng)
                    if known.get(k, 0) < p.tick:
                        h.wait_ge(esem[p.eng], p.tick)
                        known[k] = p.tick
                for s, v in o.dwaits.items():
                    k = ("d", s)
                    if known.get(k, 0) < v:
                        h.wait_ge(dsem[s], v)
                        known[k] = v
                if o.xw is not None:
                    h.wait_ge(o.xw[0], o.xw[1])
                if o.fn is None:
                    continue
                ins = o.fn(h)
                if o.xs is not None:
                    ins.then_inc(o.xs, 1)
                elif o.is_dma:
                    ins.then_inc(dsem[o.dsem], getattr(o, "inc", 16))
                elif o.marked:
                    ins.then_inc(esem[ename], 1)

        with nc.Block() as block:
            @block.tensor
            def _(h):
                replay("pe", h)

            @block.scalar
            def _(h):
                replay("act", h)

            @block.vector
            def _(h):
                replay("dve", h)

            @block.gpsimd
            def _(h):
                replay("pool", h)

            @block.sync
            def _(h):
                replay("sp", h)
        st.close()
        nc.all_engine_barrier()
        nc.clear_and_free_semaphores(list(esem.values()) + dsem)
        nc.all_engine_barrier()


def _mk(P):
    def mm(out, lhsT, rhs, start=True, stop=True, reads=(), writes=()):
        return P.op("pe", lambda e: e.matmul(out, lhsT, rhs, start=start, stop=stop), reads, writes)

    def tr(out, in_, ident, reads=(), writes=()):
        return P.op("pe", lambda e: e.transpose(out, in_, ident), reads, writes)

    def act(out, in_, func, reads=(), writes=(), **kw):
        return P.op("act", lambda e: e.activation(out, in_, func, **kw), reads, writes)

    def tt(eng, out, in0, in1, op, reads=(), writes=()):
        return P.op(eng, lambda e: e.tensor_tensor(out, in0, in1, op), reads, writes)

    def ts(eng, out, in0, s1, s2, op0, op1=None, reads=(), writes=(), **kw):
        if op1 is None:
            return P.op(eng, lambda e: e.tensor_scalar(out, in0, s1, None, op0, **kw), reads, writes)
        return P.op(eng, lambda e: e.tensor_scalar(out, in0, s1, s2, op0, op1, **kw), reads, writes)

    def stt(out, in0, scalar, in1, op0, op1, reads=(), writes=(), **kw):
        return P.op("dve", lambda e: e.scalar_tensor_tensor(out, in0, scalar, in1, op0, op1, **kw), reads, writes)

    def cp(eng, out, in_, reads=(), writes=()):
        if eng == "act":
            return P.op(eng, lambda e: e.copy(out, in_), reads, writes)
        return P.op(eng, lambda e: e.tensor_copy(out, in_), reads, writes)

    def red(out, in_, op, axis=AX.X, reads=(), writes=()):
        return P.op("dve", lambda e: e.tensor_reduce(out, in_, axis, op), reads, writes)

    def memset(eng, ap, val, writes=()):
        return P.op(eng, lambda e: e.memset(ap, val), (), writes)

    def scan(out, d0, d1, init, op0, op1, reads=(), writes=()):
        return P.op("dve", lambda e: e.tensor_tensor_scan(out, d0, d1, init, op0, op1), reads, writes)

    def recip(out, in_, reads=(), writes=()):
        return P.op("dve", lambda e: e.reciprocal(out, in_), reads, writes)

    P.mm, P.tr, P.act, P.tt, P.ts, P.stt, P.cp, P.red, P.memset, P.scan, P.recip = (
        mm, tr, act, tt, ts, stt, cp, red, memset, scan, recip)
    return P


def new_prog(n_dma_sems=40):
    nc = bass.Bass("TRN2", target_bir_lowering=False)
    return _mk(Prog(nc, n_dma_sems))


import math
import numpy as np

S = 4096; D = 2048; KT = 16; CH = 512; NCH = S // CH
RMS_EPS = 1e-6


def bcast_rows(d, nelem, parts=128, offset=0):
    return bass.AP(d.t, offset, [[0, parts], [1, nelem]])


def load_w_bf16(P, w_dram, ncols, name, q0="sp", q1="pool", cast_eng="pool", stg_buf=None):
    stg = stg_buf if stg_buf is not None else P.sb([128, KT, ncols], F32, name + "_stg")
    wb = P.sb([128, KT, ncols], BF16, name)
    src = w_dram[:].rearrange("(kt p) n -> p kt n", p=128)
    h = KT // 2
    P.dma(q0, stg[:, 0:h, 0:ncols], src[:, 0:h, :], writes=[stg])
    P.dma(q1, stg[:, h:, 0:ncols], src[:, h:, :], writes=[stg])
    P.cp(cast_eng, wb[:], stg[:, :, 0:ncols], reads=[stg], writes=[wb])
    return wb


class NormCtx:
    def __init__(self, P, g_dram, ch=CH):
        self.P = P
        self.ch = ch
        CH = ch
        self.ones_bf = P.sb([128, 128], BF16, "ones_bf")
        P.memset("pool", self.ones_bf[:], 1.0, writes=[self.ones_bf])
        self.g = P.sb([128, KT], F32, "g_norm")
        P.dma("sp", self.g[:], g_dram[:].rearrange("(kt p) -> p kt", p=128), writes=[self.g],
              allow_slow_non_contiguous=True)
        self.eps = P.sb([128, 1], F32, "eps_t")
        P.memset("pool", self.eps[:], RMS_EPS, writes=[self.eps])
        self.x32 = [P.sb([128, KT, CH], F32, f"x32_{i}") for i in range(2)]
        self.hn = [P.sb([128, KT, CH], BF16, f"hn_{i}") for i in range(2)]
        self.sq = [P.sb([128, CH], BF16, f"sq_{i}") for i in range(3)]
        self.sd = P.sb([128, CH], F32, "sd")
        self.rstd = P.sb([128, CH], F32, "rstd")

    def load(self, hT_dram, c, col0=None):
        P = self.P
        CH = self.ch
        x32 = self.x32[c % 2]
        c0 = c * CH if col0 is None else col0
        if callable(hT_dram):
            src, sbuf_ = hT_dram(c0, CH)
        else:
            src, sbuf_ = hT_dram[:, c0:c0 + CH].rearrange("(kt p) t -> p kt t", p=128), hT_dram
        h = KT // 2
        P.dma("sp", x32[:, 0:h, :], src[:, 0:h, :], reads=[sbuf_], writes=[x32])
        P.dma("pool", x32[:, h:, :], src[:, h:, :], reads=[sbuf_], writes=[x32])

    def norm(self, c, ps_ss, dmodel=D):
        P = self.P
        CH = self.ch
        x32 = self.x32[c % 2]; hn = self.hn[c % 2]
        for k in range(KT):
            sq = self.sq[k % 3]
            P.act(sq[:], x32[:, k, :], AF.Square, reads=[x32], writes=[sq])
            P.mm(ps_ss[:, 0:CH], self.ones_bf[:], sq[:], start=(k == 0), stop=(k == KT - 1),
                 reads=[self.ones_bf, sq], writes=[ps_ss])
        P.act(self.sd[:], ps_ss[:, 0:CH], AF.Sqrt, reads=[ps_ss, self.eps], writes=[self.sd],
              scale=1.0 / dmodel, bias=self.eps[:])
        P.recip(self.rstd[:], self.sd[:], reads=[self.sd], writes=[self.rstd])
        for k in range(KT):
            P.stt(hn[:, k, :], x32[:, k, :], self.g[:, k:k + 1], self.rstd[:], ALU.mult, ALU.mult,
                  reads=[x32, self.g, self.rstd], writes=[hn])
        return hn


def proj_qkv(P, nctx, hT, wb, pb, cosT, sinT, rm, QT, KTt, V):
    q32 = [P.sb([128, CH], F32, f"q32_{i}") for i in range(2)]
    t1 = [P.sb([128, CH], F32, f"t1_{i}") for i in range(2)]
    t2 = [P.sb([128, CH], F32, f"t2_{i}") for i in range(2)]
    nctx.load(hT, 0)
    for c in range(NCH):
        if c + 1 < NCH:
            nctx.load(hT, c + 1)
        hn = nctx.norm(c, pb[0])
        cs = slice(c * CH, (c + 1) * CH)
        for qi, (dst, col0) in enumerate(((QT, 0), (KTt, 128))):
            pq = pb[1 + 2 * qi]; pr = pb[2 + 2 * qi]
            for k in range(KT):
                P.mm(pq[:], wb[:, k, col0:col0 + 128], hn[:, k, :], start=(k == 0), stop=(k == KT - 1),
                     reads=[wb, hn], writes=[pq])
            P.act(q32[qi][:], pq[:], AF.Copy, reads=[pq], writes=[q32[qi]])
            P.mm(pr[:], rm[:], q32[qi][:], reads=[rm, q32[qi]], writes=[pr])
            P.tt("dve", t1[qi][:], q32[qi][:], cosT[:, cs], ALU.mult, reads=[q32[qi], cosT], writes=[t1[qi]])
            P.tt("dve", t2[qi][:], pr[:], sinT[:, cs], ALU.mult, reads=[pr, sinT], writes=[t2[qi]])
            P.tt("dve", dst[:, cs], t1[qi][:], t2[qi][:], ALU.add, reads=[t1[qi], t2[qi]], writes=[dst])
        pv = pb[5]
        for ts_ in range(4):
            for k in range(KT):
                P.mm(pv[:, ts_ * 128:(ts_ + 1) * 128], hn[:, k, ts_ * 128:(ts_ + 1) * 128], wb[:, k, 256:384],
                     start=(k == 0), stop=(k == KT - 1), reads=[hn, wb], writes=[pv])
        P.act(V[:, c * 4:(c + 1) * 4, :], pv[:].rearrange("p (a b) -> p a b", a=4), AF.Copy, reads=[pv], writes=[V])


def simple_out(P, out_d):
    def f(buf, ap, c0, n):
        P.dma("sp", out_d[:, c0:c0 + n], ap, reads=[buf], writes=[out_d])
    return f


def masked_out(rsin_t, g, rmask_d):
    def mk(P, alloc):
        tmp = [alloc(0), alloc(1)]
        cnt = [0]
        rsin = Buf(rsin_t, "rsin")
        rmask = P.sb([128, 4], F32, "rmask")
        P.dma("sp", rmask[:], rmask_d[:], writes=[rmask])

        def f(buf, ap, c0, n):
            tq, tl = divmod(c0, 1024)
            for r in range(4):
                t_ = tmp[cnt[0] % 2]; cnt[0] += 1
                P.ts("pool", t_[:, 0:n], ap, rmask[:, r:r + 1], None, ALU.mult, reads=[buf, rmask], writes=[t_])
                P.dma("sp", rsin[tq, g, r, :, tl:tl + n], t_[:, 0:n], reads=[t_], writes=[rsin])
        return f
    return mk


def emit_A(P, layer_idx, hT, g_mix, w_a, lamv_d, subln_d, cos_d, sin_d, rm_d, out_d, out_mk=None):
    out_fn = out_mk(P, lambda i: P.sb([128, 512], F32, f"mo{i}")) if out_mk else simple_out(P, out_d)
    lam_init = 0.8 - 0.6 * math.exp(-0.3 * layer_idx)
    pb = [P.ps([128, 512], F32, f"bank{i}") for i in range(8)]
    nctx = NormCtx(P, g_mix)
    wb = load_w_bf16(P, w_a, 384, "wA", stg_buf=nctx.x32[1])
    cosT = P.sb([128, S], F32, "cosT"); sinT = P.sb([128, S], F32, "sinT")
    P.dma("sp", cosT[:], cos_d[:], writes=[cosT]); P.dma("pool", sinT[:], sin_d[:], writes=[sinT])
    rm = P.sb([128, 128], F32, "rm"); P.dma("sp", rm[:], rm_d[:], writes=[rm])
    QT = P.sb([128, S], BF16, "QT"); KTt = P.sb([128, S], BF16, "KTt")
    V = P.sb([128, S // 128, 128], BF16, "Vall")
    lamv = P.sb([128, 2, 2, 64], F32, "lamv")
    P.dma("sp", lamv[:].rearrange("p a b c -> p (a b c)"), bcast_rows(lamv_d, 256), writes=[lamv])
    lprod = P.sb([128, 2, 64], F32, "lprod"); lsum = P.sb([128, 2], F32, "lsum"); lexp = P.sb([128, 2], F32, "lexp")
    nlam = P.sb([128, 1], F32, "nlam")
    P.tt("dve", lprod[:], lamv[:, :, 0, :], lamv[:, :, 1, :], ALU.mult, reads=[lamv], writes=[lprod])
    P.red(lsum[:], lprod[:], ALU.add, reads=[lprod], writes=[lsum])
    P.act(lexp[:], lsum[:], AF.Exp, reads=[lsum], writes=[lexp])
    P.tt("dve", nlam[:], lexp[:, 1:2], lexp[:, 0:1], ALU.subtract, reads=[lexp], writes=[nlam])
    P.ts("dve", nlam[:], nlam[:], -lam_init, None, ALU.add, reads=[nlam], writes=[nlam])
    gsub = P.sb([128, 1], F32, "gsub")
    P.dma("sp", gsub[:], subln_d[:].rearrange("(p o) -> p o", o=1), writes=[gsub])
    P.ts("dve", gsub[:], gsub[:], 1.0 - lam_init, None, ALU.mult, reads=[gsub], writes=[gsub])

    proj_qkv(P, nctx, hT, wb, pb, cosT, sinT, rm, QT, KTt, V)

    NKT = S // 128
    par = nctx.x32[0]
    def cv32(k):
        return P.carve(par, par.t[:, k, :], f"cv{k}")
    def cv16(k, h):
        return P.carve(par, par.t[:, k, h * 256:(h + 1) * 256].bitcast(BF16), f"cvb{k}_{h}")
    E = [[cv16(c * 2 + i // 2, i % 2) if i < 2 else cv16(4, c) for i in range(3)] for c in range(2)]
    R = [cv32(5 + c) for c in range(2)]
    tt0 = cv32(7); tt1 = cv32(8)
    o32 = cv32(9); osq = cv16(10, 0)
    sd2 = cv32(11); rs2 = cv32(12)
    oout = [cv32(13 + i) for i in range(2)]

    def s_mm(qc, kt):
        for c in range(2):
            Sb = pb[4 + 2 * c + (kt % 2)]
            P.mm(Sb[:], KTt[64 * c:64 * c + 64, kt * 128:(kt + 1) * 128], QT[64 * c:64 * c + 64, qc * CH:(qc + 1) * CH],
                 reads=[KTt, QT], writes=[Sb])

    for qc in range(NCH):
        s_mm(qc, 0)
        for kt in range(NKT):
            if kt + 1 < NKT:
                s_mm(qc, kt + 1)
            for c in range(2):
                Sb = pb[4 + 2 * c + (kt % 2)]
                e = E[c][kt % 3]
                P.act(e[:], Sb[:], AF.Exp, reads=[Sb], writes=[e], scale=0.125)
                P.mm(pb[c][:], V[:, kt, :], e[:], start=(kt == 0), stop=(kt == NKT - 1), reads=[V, e], writes=[pb[c]])
                P.mm(pb[2 + c][:], nctx.ones_bf[:], e[:], start=(kt == 0), stop=(kt == NKT - 1),
                     reads=[nctx.ones_bf, e], writes=[pb[2 + c]])
        for c in range(2):
            P.recip(R[c][:], pb[2 + c][:], reads=[pb[2 + c]], writes=[R[c]])
        P.tt("dve", tt0[:], pb[0][:], R[0][:], ALU.mult, reads=[pb[0], R[0]], writes=[tt0])
        P.tt("dve", tt1[:], pb[1][:], R[1][:], ALU.mult, reads=[pb[1], R[1]], writes=[tt1])
        P.stt(o32[:], tt1[:], nlam[:, 0:1], tt0[:], ALU.mult, ALU.add, reads=[tt1, nlam, tt0], writes=[o32])
        P.act(osq[:], o32[:], AF.Square, reads=[o32], writes=[osq])
        pss = pb[4]
        P.mm(pss[:], nctx.ones_bf[:], osq[:], reads=[nctx.ones_bf, osq], writes=[pss])
        P.act(sd2[:], pss[:], AF.Sqrt, reads=[pss, nctx.eps], writes=[sd2], scale=1.0 / 128, bias=nctx.eps[:])
        P.recip(rs2[:], sd2[:], reads=[sd2], writes=[rs2])
        oo = oout[qc % 2]
        P.stt(oo[:], o32[:], gsub[:, 0:1], rs2[:], ALU.mult, ALU.mult, reads=[o32, gsub, rs2], writes=[oo])
        out_fn(oo, oo[:], qc * CH, CH)


def rope_tables_np(seq, dim, theta=10000.0):
    inv = (1.0 / (np.float32(theta) ** (np.arange(0, dim, 2, dtype=np.float32) / np.float32(dim)))).astype(np.float32)
    ang = np.arange(seq, dtype=np.float32)[:, None] * inv[None, :]
    return np.cos(ang).astype(np.float32), np.sin(ang).astype(np.float32)


def consts_A():
    cos, sin = rope_tables_np(S, 64)
    cosT = np.tile(cos.T, (4, 1)).astype(np.float32)
    sinT = np.tile(sin.T, (4, 1)).astype(np.float32)
    rm = np.zeros((128, 128), np.float32)
    for blk in range(2):
        for dp in range(64):
            if dp < 32:
                rm[blk * 64 + dp + 32, blk * 64 + dp] = -1.0
            else:
                rm[blk * 64 + dp - 32, blk * 64 + dp] = 1.0
    return cosT, sinT, rm


def emit_C(P, hT, g_mix, w_c, cos_d, sin_d, rm_d, mask_d, out_d, out_mk=None):
    pb = [P.ps([128, 512], F32, f"bank{i}") for i in range(8)]
    nctx = NormCtx(P, g_mix)
    wb = load_w_bf16(P, w_c, 384, "wC", stg_buf=nctx.x32[1])
    mk = P.sb([128, 20, 512], BF16, "mk")
    for h in range(2):
        stg = nctx.x32[0]
        P.dma("sp", stg[:, 0:10, :], mask_d[:, h * 10:(h + 1) * 10, :], writes=[stg])
        P.cp("pool", mk[:, h * 10:(h + 1) * 10, :], stg[:, 0:10, :], reads=[stg], writes=[mk])
    cosT = P.sb([128, S], F32, "cosT"); sinT = P.sb([128, S], F32, "sinT")
    P.dma("sp", cosT[:], cos_d[:], writes=[cosT]); P.dma("pool", sinT[:], sin_d[:], writes=[sinT])
    rm = P.sb([128, 128], F32, "rm"); P.dma("sp", rm[:], rm_d[:], writes=[rm])
    QT = P.sb([128, S], BF16, "QT"); KTt = P.sb([128, S], BF16, "KTt")
    V = P.sb([128, S // 128, 128], BF16, "Vall")
    proj_qkv(P, nctx, hT, wb, pb, cosT, sinT, rm, QT, KTt, V)

    par = nctx.x32[1]
    def cv32(k):
        return P.carve(par, par.t[:, k, :], f"cv{k}")
    def cv16(k, h):
        return P.carve(par, par.t[:, k, h * 256:(h + 1) * 256].bitcast(BF16), f"cvb{k}_{h}")
    E = [cv16(0, 0), cv16(0, 1), cv16(1, 0)]
    EM = [cv16(2, 0), cv16(2, 1), cv16(3, 0)]
    Rr = cv32(4)
    oout = [cv32(5), cv32(6)]
    out_fn = out_mk(P, lambda i: cv32(8 + i)) if out_mk else simple_out(P, out_d)
    scale = 128 ** -0.5
    for qc in range(NCH):
        kts = [kt for kt in range(4 * qc - 8, 4 * qc + 12) if 0 <= kt < S // 128]
        Sb = [pb[4], pb[5], pb[6]]
        O = pb[qc % 2]; Dn = pb[2 + qc % 2]

        def s_mm(i):
            kt = kts[i]
            P.mm(Sb[i % 3][:], KTt[:, kt * 128:(kt + 1) * 128], QT[:, qc * CH:(qc + 1) * CH],
                 reads=[KTt, QT], writes=[Sb[i % 3]])
        s_mm(0)
        for i, kt in enumerate(kts):
            if i + 1 < len(kts):
                s_mm(i + 1)
            kk = kt - 4 * qc + 8
            e = E[i % 3]; em = EM[i % 3]
            P.act(e[:], Sb[i % 3][:], AF.Exp, reads=[Sb[i % 3]], writes=[e], scale=scale)
            P.tt("dve", em[:], e[:], mk[:, kk, :], ALU.mult, reads=[e, mk], writes=[em])
            P.mm(O[:], V[:, kt, :], em[:], start=(i == 0), stop=(i == len(kts) - 1), reads=[V, em], writes=[O])
            P.mm(Dn[:], nctx.ones_bf[:], em[:], start=(i == 0), stop=(i == len(kts) - 1),
                 reads=[nctx.ones_bf, em], writes=[Dn])
        P.recip(Rr[:], Dn[:], reads=[Dn], writes=[Rr])
        oo = oout[qc % 2]
        P.tt("dve", oo[:], O[:], Rr[:], ALU.mult, reads=[O, Rr], writes=[oo])
        out_fn(oo, oo[:], qc * CH, CH)


def consts_C():
    cos, sin = rope_tables_np(S, 128)
    cosT = np.tile(cos.T, (2, 1)).astype(np.float32)
    sinT = np.tile(sin.T, (2, 1)).astype(np.float32)
    rm = np.zeros((128, 128), np.float32)
    for dp in range(128):
        if dp < 64:
            rm[dp + 64, dp] = -1.0
        else:
            rm[dp - 64, dp] = 1.0
    jl = np.arange(128)[:, None]; il = np.arange(512)[None, :]
    mask = np.zeros((128, 20, 512), np.float32)
    for kk in range(20):
        dlt = (kk - 8) * 128 + jl - il
        a = np.abs(dlt)
        mask[:, kk, :] = (a <= 64).astype(np.float32) + ((a <= 256) & (dlt % 4 == 0)) + ((a <= 1024) & (dlt % 16 == 0))
    return cosT, sinT, rm, mask


CHK = 512


def emit_D(P, hT, g_mix, w_d, dpar_d, dskip_d, bblk_d, cblk_d, out_d, out_mk=None):
    out_fn = out_mk(P, lambda i: P.sb([128, 512], F32, f"mo{i}")) if out_mk else simple_out(P, out_d)
    pb = [P.ps([128, 512], F32, f"bank{i}") for i in range(8)]
    nctx = NormCtx(P, g_mix)
    wb = load_w_bf16(P, w_d, 128, "wD", stg_buf=nctx.x32[1])
    U = P.sb([128, S], F32, "U"); Y = P.sb([128, S], F32, "Y")
    bblk = P.sb([128, 2, 4, 128], F32, "bblk"); cblk = P.sb([128, 2, 4, 128], F32, "cblk")
    P.dma("sp", bblk[:], bblk_d[:], writes=[bblk]); P.dma("pool", cblk[:], cblk_d[:], writes=[cblk])
    dpar = P.sb([128, 3, 8], F32, "dpar"); P.dma("sp", dpar[:], dpar_d[:], writes=[dpar])
    dskip = P.sb([128, 1], F32, "dskip"); P.dma("sp", dskip[:], dskip_d[:].rearrange("(p o) -> p o", o=1), writes=[dskip])
    nctx.load(hT, 0)
    for c in range(NCH):
        if c + 1 < NCH:
            nctx.load(hT, c + 1)
        hn = nctx.norm(c, pb[0])
        pu = pb[1 + c % 2]
        for k in range(KT):
            P.mm(pu[:], wb[:, k, :], hn[:, k, :], start=(k == 0), stop=(k == KT - 1), reads=[wb, hn], writes=[pu])
        P.act(U[:, c * CH:(c + 1) * CH], pu[:], AF.Copy, reads=[pu], writes=[U])

    def pt(name):
        return P.sb([128, 8], F32, name)
    lr = pt("lr"); dt = pt("dt"); mag = pt("mag"); th = pt("th"); cs = pt("cs"); sn = pt("sn")
    ta = pt("ta"); tb = pt("tb"); tc = pt("tc"); fr = pt("fr"); fi = pt("fi"); abr = pt("abr"); abi = pt("abi")
    hpi = P.sb([128, 1], F32, "hpi"); P.memset("dve", hpi[:], math.pi / 2, writes=[hpi])
    li = dpar[:, 1, :]
    P.ts("dve", lr[:], dpar[:, 0, :], -1e-4, None, ALU.min, reads=[dpar], writes=[lr])
    P.act(dt[:], dpar[:, 2, :], AF.Exp, reads=[dpar], writes=[dt])
    P.tt("dve", ta[:], dt[:], lr[:], ALU.mult, reads=[dt, lr], writes=[ta])
    P.act(mag[:], ta[:], AF.Exp, reads=[ta], writes=[mag])
    P.tt("dve", th[:], dt[:], li, ALU.mult, reads=[dt, dpar], writes=[th])
    P.act(sn[:], th[:], AF.Sin, reads=[th], writes=[sn], scale=1.0 / 32)
    P.act(cs[:], th[:], AF.Sin, reads=[th, hpi], writes=[cs], scale=1.0 / 32, bias=hpi[:])
    for _ in range(5):
        P.tt("dve", ta[:], cs[:], cs[:], ALU.mult, reads=[cs], writes=[ta])
        P.tt("dve", tb[:], sn[:], sn[:], ALU.mult, reads=[sn], writes=[tb])
        P.tt("dve", tc[:], cs[:], sn[:], ALU.mult, reads=[cs, sn], writes=[tc])
        P.tt("dve", cs[:], ta[:], tb[:], ALU.subtract, reads=[ta, tb], writes=[cs])
        P.ts("dve", sn[:], tc[:], 2.0, None, ALU.mult, reads=[tc], writes=[sn])
    P.tt("dve", abr[:], mag[:], cs[:], ALU.mult, reads=[mag, cs], writes=[abr])
    P.tt("dve", abi[:], mag[:], sn[:], ALU.mult, reads=[mag, sn], writes=[abi])
    P.tt("dve", ta[:], lr[:], lr[:], ALU.mult, reads=[lr], writes=[ta])
    P.tt("dve", tb[:], li, li, ALU.mult, reads=[dpar], writes=[tb])
    P.tt("dve", ta[:], ta[:], tb[:], ALU.add, reads=[ta, tb], writes=[ta])
    P.recip(tc[:], ta[:], reads=[ta], writes=[tc])
    P.ts("dve", abr[:], abr[:], -1.0, None, ALU.add, reads=[abr], writes=[abr])
    P.tt("dve", ta[:], abr[:], lr[:], ALU.mult, reads=[abr, lr], writes=[ta])
    P.tt("dve", tb[:], abi[:], li, ALU.mult, reads=[abi, dpar], writes=[tb])
    P.tt("dve", ta[:], ta[:], tb[:], ALU.add, reads=[ta, tb], writes=[ta])
    P.tt("dve", fr[:], ta[:], tc[:], ALU.mult, reads=[ta, tc], writes=[fr])
    P.tt("dve", ta[:], abi[:], lr[:], ALU.mult, reads=[abi, lr], writes=[ta])
    P.tt("dve", tb[:], abr[:], li, ALU.mult, reads=[abr, dpar], writes=[tb])
    P.tt("dve", ta[:], ta[:], tb[:], ALU.subtract, reads=[ta, tb], writes=[ta])
    P.tt("dve", fi[:], ta[:], tc[:], ALU.mult, reads=[ta, tc], writes=[fi])

    x0 = nctx.x32[0]; x1 = nctx.x32[1]
    Tc = P.carve(x0, x0.t[:, 0:8, :], "Tc"); Ts = P.carve(x0, x0.t[:, 8:16, :], "Ts")
    Tr = P.carve(x1, x1.t[:, 0:8, :], "Tinr"); Ti = P.carve(x1, x1.t[:, 8:16, :], "Tini")
    h0 = nctx.hn[0]; h1 = nctx.hn[1]
    G0 = P.carve(h0, h0.t[:].rearrange("p a b -> p (a b)").bitcast(F32), "G0")
    G1 = P.carve(h1, h1.t[:].rearrange("p a b -> p (a b)").bitcast(F32), "G1")
    g0v = G0.t.rearrange("p (a b) -> p a b", a=8); g1v = G1.t.rearrange("p (a b) -> p a b", a=8)

    def bc(buf, m):
        return bass.AP(buf.t, 0, [[8, 128], [1, 8], [0, m]])

    def bcT(T, idx, m):
        return bass.AP(T.t.tensor, T.t.offset + idx, [[T.t.ap[0][0], 128], [T.t.ap[1][0], 8], [0, m]])

    P.cp("dve", Tc[:, :, 0:1], cs[:].rearrange("p (a o) -> p a o", o=1), reads=[cs], writes=[Tc])
    P.cp("dve", Ts[:, :, 0:1], sn[:].rearrange("p (a o) -> p a o", o=1), reads=[sn], writes=[Ts])
    m = 1
    while m < CHK:
        ec = bcT(Tc, m - 1, m); es = bcT(Ts, m - 1, m)
        a = g0v[:, :, 0:m]; b = g1v[:, :, 0:m]
        P.tt("dve", a, Tc[:, :, 0:m], ec, ALU.mult, reads=[Tc], writes=[G0])
        P.tt("dve", b, Ts[:, :, 0:m], es, ALU.mult, reads=[Ts], writes=[G1])
        P.tt("dve", Tc[:, :, m:2 * m], a, b, ALU.subtract, reads=[G0, G1], writes=[Tc])
        P.tt("dve", a, Tc[:, :, 0:m], es, ALU.mult, reads=[Tc, Ts], writes=[G0])
        P.tt("dve", b, Ts[:, :, 0:m], ec, ALU.mult, reads=[Ts, Tc], writes=[G1])
        P.tt("dve", Ts[:, :, m:2 * m], a, b, ALU.add, reads=[G0, G1], writes=[Ts])
        m *= 2
    P.tt("dve", g0v, Tc[:], bc(fr, CHK), ALU.mult, reads=[Tc, fr], writes=[G0])
    P.tt("dve", g1v, Ts[:], bc(fi, CHK), ALU.mult, reads=[Ts, fi], writes=[G1])
    P.tt("dve", Tr[:], g0v, g1v, ALU.add, reads=[G0, G1], writes=[Tr])
    P.tt("dve", g0v, Tc[:], bc(fi, CHK), ALU.mult, reads=[Tc, fi], writes=[G0])
    P.tt("dve", g1v, Ts[:], bc(fr, CHK), ALU.mult, reads=[Ts, fr], writes=[G1])
    P.tt("dve", Ti[:], g0v, g1v, ALU.subtract, reads=[G0, G1], writes=[Ti])

    def wb_(name, n=2):
        return [P.sb([128, CHK], F32, f"{name}{i}") for i in range(n)]
    m1 = wb_("m1"); m2 = wb_("m2"); wr = wb_("wr"); wi = wb_("wi"); xh_r = wb_("xhr"); xh_i = wb_("xhi")
    xr = wb_("xr"); nxi = wb_("nxi")
    carry = [[P.sb([128, 1], F32, f"car{d}_{s}_{ri}") for ri in range(2)] for d in range(2) for s in range(4)]
    for cc in carry:
        for t_ in cc:
            P.memset("pool", t_[:], 0.0, writes=[t_])

    def rv(ap_full, base_buf_is_psum=False):
        return ap_full

    it = 0
    for d in range(2):
        order = range(NCH) if d == 0 else range(NCH - 1, -1, -1)
        for c in order:
            t0 = c * CHK
            py = pb[6 + (it % 2)]
            for s in range(4):
                col = d * 4 + s
                pr_ = pb[(it * 4 + s) % 3 * 2]; pi_ = pb[(it * 4 + s) % 3 * 2 + 1]
                P.mm(pr_[:], bblk[:, 0, s, :], U[:, t0:t0 + CHK], reads=[bblk, U], writes=[pr_])
                P.mm(pi_[:], bblk[:, 1, s, :], U[:, t0:t0 + CHK], reads=[bblk, U], writes=[pi_])
                i2 = (it * 4 + s) % 2
                if d == 0:
                    rvs = lambda ap: ap
                else:
                    rvs = lambda ap: ap[:, ::-1]
                bur = pr_[:]; bui = pi_[:]
                P.tt("dve", rvs(m1[i2][:]), bur, rvs(Tr[:, col, :]), ALU.mult, reads=[pr_, Tr], writes=[m1[i2]])
                P.tt("dve", rvs(m2[i2][:]), bui, rvs(Ti[:, col, :]), ALU.mult, reads=[pi_, Ti], writes=[m2[i2]])
                P.tt("dve", wr[i2][:], m1[i2][:], m2[i2][:], ALU.subtract, reads=[m1[i2], m2[i2]], writes=[wr[i2]])
                P.tt("dve", rvs(m1[i2][:]), bur, rvs(Ti[:, col, :]), ALU.mult, reads=[pr_, Ti], writes=[m1[i2]])
                P.tt("dve", rvs(m2[i2][:]), bui, rvs(Tr[:, col, :]), ALU.mult, reads=[pi_, Tr], writes=[m2[i2]])
                P.tt("dve", wi[i2][:], m1[i2][:], m2[i2][:], ALU.add, reads=[m1[i2], m2[i2]], writes=[wi[i2]])
                magb = bass.AP(mag.t, col, [[8, 128], [0, CHK]])
                cr, ci = carry[d * 4 + s]
                P.scan(xh_r[i2][:], magb, wr[i2][:], cr[:, 0:1], ALU.mult, ALU.add, reads=[mag, wr[i2], cr], writes=[xh_r[i2]])
                P.scan(xh_i[i2][:], magb, wi[i2][:], ci[:, 0:1], ALU.mult, ALU.add, reads=[mag, wi[i2], ci], writes=[xh_i[i2]])
                P.tt("dve", m1[i2][:], xh_r[i2][:], Tc[:, col, :], ALU.mult, reads=[xh_r[i2], Tc], writes=[m1[i2]])
                P.tt("dve", m2[i2][:], xh_i[i2][:], Ts[:, col, :], ALU.mult, reads=[xh_i[i2], Ts], writes=[m2[i2]])
                P.tt("dve", xr[i2][:], m1[i2][:], m2[i2][:], ALU.subtract, reads=[m1[i2], m2[i2]], writes=[xr[i2]])
                P.tt("dve", m1[i2][:], xh_r[i2][:], Ts[:, col, :], ALU.mult, reads=[xh_r[i2], Ts], writes=[m1[i2]])
                P.tt("dve", m2[i2][:], xh_i[i2][:], Tc[:, col, :], ALU.mult, reads=[xh_i[i2], Tc], writes=[m2[i2]])
                P.stt(nxi[i2][:], m1[i2][:], -1.0, m2[i2][:], ALU.mult, ALU.subtract, reads=[m1[i2], m2[i2]], writes=[nxi[i2]])
                P.cp("dve", cr[:], xr[i2][:, CHK - 1:CHK], reads=[xr[i2]], writes=[cr])
                P.ts("dve", ci[:], nxi[i2][:, CHK - 1:CHK], -1.0, None, ALU.mult, reads=[nxi[i2]], writes=[ci])
                P.mm(py[:], cblk[:, 0, s, :], xr[i2][:], start=(s == 0), stop=False, reads=[cblk, xr[i2]], writes=[py])
                P.mm(py[:], cblk[:, 1, s, :], nxi[i2][:], start=False, stop=(s == 3), reads=[cblk, nxi[i2]], writes=[py])
            if d == 0:
                P.stt(Y[:, t0:t0 + CHK], U[:, t0:t0 + CHK], dskip[:, 0:1], py[:], ALU.mult, ALU.add,
                      reads=[U, dskip, py], writes=[Y])
            else:
                P.tt("dve", Y[:, t0:t0 + CHK][:, ::-1], Y[:, t0:t0 + CHK][:, ::-1], py[:], ALU.add, reads=[Y, py], writes=[Y])
            it += 1
    P.act(G0[:], Y[:], AF.Square, reads=[Y], writes=[G0])
    P.ts("dve", G0[:], G0[:], 0.044715, 1.0, ALU.mult, ALU.add, reads=[G0], writes=[G0])
    P.tt("dve", G0[:], G0[:], Y[:], ALU.mult, reads=[G0, Y], writes=[G0])
    P.act(G1[:], G0[:], AF.Sigmoid, reads=[G0], writes=[G1], scale=2.0 * math.sqrt(2.0 / math.pi))
    P.tt("dve", G0[:], G1[:], Y[:], ALU.mult, reads=[G1, Y], writes=[G0])
    for c in range(NCH):
        out_fn(G0, G0[:, c * CH:(c + 1) * CH], c * CH, CH)


def host_D(d, L, j):
    G0 = 8 * j
    a_re = d["s5_a_re"][L]; a_im = d["s5_a_im"][L]; ldt = d["s5_log_dt"][L]
    dpar = np.zeros((128, 3, 8), np.float32)
    for dr in range(2):
        for s in range(4):
            for gh in range(2):
                g = G0 + 2 * s + gh
                dpar[gh * 64:(gh + 1) * 64, 0, dr * 4 + s] = a_re[dr, g]
                dpar[gh * 64:(gh + 1) * 64, 1, dr * 4 + s] = a_im[dr, g]
                dpar[gh * 64:(gh + 1) * 64, 2, dr * 4 + s] = ldt[dr, g]
    bblk = np.zeros((128, 2, 4, 128), np.float32); cblk = np.zeros((128, 2, 4, 128), np.float32)
    for ri, (bn, cn) in enumerate((("s5_b_re", "s5_c_re"), ("s5_b_im", "s5_c_im"))):
        B = d[bn][L]; C = d[cn][L]
        for s in range(4):
            for gh in range(2):
                gl = 2 * s + gh
                bblk[gl * 16:(gl + 1) * 16, ri, s, gh * 64:(gh + 1) * 64] = B[G0 + gl].T
                cblk[gh * 64:(gh + 1) * 64, ri, s, gl * 16:(gl + 1) * 16] = C[G0 + gl].T
    return dpar, bblk, cblk, np.ascontiguousarray(d["s5_d"][L][128 * j:128 * (j + 1)])


LC = 64
SCH = 8
LN_X_EPS = 64e-5
NV = 21


def carve_multi(P, parents, ap, name=""):
    b = Buf(ap, name)
    b.last_w = None
    rd = []
    for p in parents:
        if p.last_w is not None:
            rd.append(p.last_w)
        rd.extend(p.readers)
    b.readers = rd
    return b


def emit_B(P, hT, g_mix, w_b, bvec_d, w2_d, a2_d, g2_d, ident_d, bones_d, out_d, tag="", out_mk=None, scr=None):
    CH1 = 256
    pb = [P.ps([128, 512], F32, f"bank{i}") for i in range(8)]
    nctx = NormCtx(P, g_mix, ch=CH1)
    zscr = P.dram("zscr" + tag, [6, 128, S + 2], F32)
    ROWS = P.dram("rows" + tag, [2, 2, S, 4, 64], F32)
    PEND = P.dram("pend" + tag, [2, 64, 2, 64], F32)
    bvec = P.sb([128, NV], F32, "bvec"); P.dma("sp", bvec[:], bvec_d[:], writes=[bvec])
    w2 = P.sb([128, 128], F32, "w2"); P.dma("sp", w2[:], w2_d[:], writes=[w2])
    a2 = P.sb([128, 128], F32, "a2"); P.dma("sp", a2[:], a2_d[:], writes=[a2])
    g2 = P.sb([128, 128], F32, "g2"); P.dma("sp", g2[:], g2_d[:], writes=[g2])
    ident = P.sb([128, 128], F32, "ident"); P.dma("pool", ident[:], ident_d[:], writes=[ident])
    bones = P.sb([128, 128], F32, "bones"); P.dma("pool", bones[:], bones_d[:], writes=[bones])
    dv = P.sb([128, 12], F32, "dv")
    P.tt("dve", dv[:, 0:6], bvec[:, 0:6], bvec[:, 6:12], ALU.add, reads=[bvec], writes=[dv])
    P.ts("dve", dv[:, 0:6], dv[:, 0:6], -1.0, 1.0, ALU.mult, ALU.add, reads=[dv], writes=[dv])
    P.ts("dve", dv[:, 6:7], bvec[:, 17:18], -1.0, 1.0, ALU.mult, ALU.add, reads=[bvec], writes=[dv])
    P.ts("dve", dv[:, 7:8], bvec[:, 18:19], 0.5, None, ALU.mult, reads=[bvec], writes=[dv])
    P.memset("dve", dv[:, 8:9], 1e-12, writes=[dv])
    P.memset("dve", dv[:, 9:10], LN_X_EPS, writes=[dv])
    mask = P.sb([128, 512], F32, "cmask")
    P.memset("dve", mask[:], 1.0, writes=[mask])
    P.memset("dve", mask[:].rearrange("p (a b) -> p a b", b=LC)[:, :, 0:1], 0.0, writes=[mask])
    zero = P.sb([128, 6, 1], F32, "zero6"); P.memset("dve", zero[:], 0.0, writes=[zero])
    P.dma("sp", zscr[:, :, 0:1].rearrange("k p o -> p k o"), zero[:], reads=[zero], writes=[zscr], allow_slow_non_contiguous=True)
    P.dma("sp", zscr[:, :, S + 1:S + 2].rearrange("k p o -> p k o"), zero[:], reads=[zero], writes=[zscr], allow_slow_non_contiguous=True)

    wB = P.sb([128, KT, 768], BF16, "wB")
    srcw = w_b[:].rearrange("(kt p) n -> p kt n", p=128)
    for hh_ in range(4):
        stg = nctx.x32[hh_ % 2]
        P.dma("sp" if hh_ % 2 == 0 else "pool", stg[:, :, 0:192], srcw[:, :, hh_ * 192:(hh_ + 1) * 192], writes=[stg])
        P.cp("pool", wB[:, :, hh_ * 192:(hh_ + 1) * 192], stg[:, :, 0:192], reads=[stg], writes=[wB])
    zst = [P.sb([128, 3, CH1], F32, f"zst{i}") for i in range(2)]
    n1 = S // CH1
    nctx.load(hT, 0)
    for c in range(n1):
        if c + 1 < n1:
            nctx.load(hT, c + 1)
        hn = nctx.norm(c, pb[0])
        for half in range(2):
            zs_ = zst[half]
            for kk_ in range(3):
                kind = half * 3 + kk_
                pz = pb[1 + kind % 3]
                for k in range(KT):
                    P.mm(pz[:, 0:CH1], wB[:, k, kind * 128:(kind + 1) * 128], hn[:, k, :], start=(k == 0), stop=(k == KT - 1),
                         reads=[wB, hn], writes=[pz])
                P.act(zs_[:, kk_, :], pz[:, 0:CH1], AF.Copy, reads=[pz], writes=[zs_])
            P.dma("sp", zscr[half * 3:(half + 1) * 3, :, 1 + c * CH1:1 + (c + 1) * CH1].rearrange("k p t -> p k t"),
                  zs_[:], reads=[zs_], writes=[zscr])

    V = P.sb([128, S], F32, "Vres")
    BG = P.dram("bgscr" + tag, [128, 2, S], F32)
    bgs = [P.sb([128, 2, 512], F32, f"bgs{i}") for i in range(2)]
    Yout = P.sb([128, 2, S], F32, "Yout")

    big = P.sb([128, 23, 512], F32, "bigtmp")
    slot_i = [0]
    slots = []

    def T(name, shape=(128, 512)):
        i = slot_i[0]; slot_i[0] += 1
        b = Buf(big.t[:, i, :], name)
        slots.append(b)
        return b
    zc = P.sb([128, 6, 514], F32, "zc")
    zr = T("zr"); zk = T("zk"); zwd = T("zwd"); zad = T("zad"); zgd = T("zgd")
    lw = [T("lw0"), T("lw1")]; aa = [T("aa0"), T("aa1")]; km = [T("km0"), T("km1")]
    kkt = T("kkt"); tA = T("tA"); tB = T("tB"); cum = T("cum"); cumx = T("cumx")
    Ex = T("Ex"); Ei = T("Ei"); En = T("En")
    kinds4 = [T("k4_0"), T("k4_1"), T("k4_2"), T("k4_3")]
    rows_sb = [P.sb([128, 2, 4, 64], F32, f"rows_sb{i}") for i in range(2)]
    pe8 = P.sb([128, 8], F32, "pe8"); pe8T = P.sb([8, 128], F32, "pe8T")
    NE05 = -math.exp(-0.5)
    n2 = S // 512
    rot = [0]

    def pbank():
        rot[0] += 1
        return pb[rot[0] % 6]

    for c in range(n2):
        t0 = c * 512
        P.dma("sp", zc[:], zscr[:, :, t0:t0 + 514].rearrange("k p t -> p k t"), reads=[zscr], writes=[zc])
        dsts = [zr, zk, None, zwd, zad, zgd]
        for kind in range(6):
            dst = dsts[kind][:] if dsts[kind] is not None else V[:, t0:t0 + 512]
            dbuf = dsts[kind] if dsts[kind] is not None else V
            P.ts("dve", dst, zc[:, kind, 1:513], dv[:, kind:kind + 1], None, ALU.mult, reads=[zc, dv], writes=[dbuf])
            P.stt(dst, zc[:, kind, 0:512], bvec[:, kind:kind + 1], dst, ALU.mult, ALU.add, reads=[zc, bvec, dbuf], writes=[dbuf])
            P.stt(dst, zc[:, kind, 2:514], bvec[:, 6 + kind:7 + kind], dst, ALU.mult, ALU.add, reads=[zc, bvec, dbuf], writes=[dbuf])
        vch = V[:, t0:t0 + 512]
        P.act(zwd[:], zwd[:], AF.Tanh, reads=[zwd], writes=[zwd])
        for d in range(2):
            pw = pbank()
            P.mm(pw[:], w2[64 * d:64 * d + 64, :], zwd[64 * d:64 * d + 64, :], reads=[w2, zwd], writes=[pw])
            P.act(lw[d][:], pw[:], AF.Sigmoid, reads=[pw, bvec], writes=[lw[d]], bias=bvec[:, 12 + d:13 + d])
            P.ts("dve", lw[d][:], lw[d][:], NE05, None, ALU.mult, reads=[lw[d]], writes=[lw[d]])
            pa = pbank()
            P.mm(pa[:], a2[64 * d:64 * d + 64, :], zad[64 * d:64 * d + 64, :], reads=[a2, zad], writes=[pa])
            P.act(aa[d][:], pa[:], AF.Sigmoid, reads=[pa, bvec], writes=[aa[d]], bias=bvec[:, 14 + d:15 + d])
        P.act(zgd[:], zgd[:], AF.Sigmoid, reads=[zgd], writes=[zgd])
        pg = pbank()
        P.mm(pg[:], g2[:], zgd[:], reads=[g2, zgd], writes=[pg])
        bg_ = bgs[c % 2]
        P.act(bg_[:, 1, :], pg[:], AF.Copy, reads=[pg], writes=[bg_])
        P.ts("dve", kkt[:], zk[:], bvec[:, 16:17], None, ALU.mult, reads=[zk, bvec], writes=[kkt])
        P.tt("dve", tA[:], kkt[:], kkt[:], ALU.mult, reads=[kkt], writes=[tA])
        pk = pbank()
        P.mm(pk[:], bones[:], tA[:], reads=[bones, tA], writes=[pk])
        P.act(tB[:], pk[:], AF.Sqrt, reads=[pk, dv], writes=[tB], bias=dv[:, 8:9])
        P.recip(tB[:], tB[:], reads=[tB], writes=[tB])
        P.tt("dve", kkt[:], kkt[:], tB[:], ALU.mult, reads=[kkt, tB], writes=[kkt])
        for d in range(2):
            P.ts("dve", km[d][:], aa[d][:], bvec[:, 17:18], dv[:, 6:7], ALU.mult, ALU.add, reads=[aa[d], bvec, dv], writes=[km[d]])
            P.tt("dve", km[d][:], km[d][:], zk[:], ALU.mult, reads=[km[d], zk], writes=[km[d]])
        P.tt("dve", tA[:], km[0][:], km[1][:], ALU.add, reads=[km[0], km[1]], writes=[tA])
        P.tt("dve", tA[:], tA[:], zr[:], ALU.mult, reads=[tA, zr], writes=[tA])
        P.ts("dve", tA[:], tA[:], dv[:, 7:8], None, ALU.mult, reads=[tA, dv], writes=[tA])
        pbn = pbank()
        P.mm(pbn[:], bones[:], tA[:], reads=[bones, tA], writes=[pbn])
        P.tt("dve", bg_[:, 0, :], pbn[:], vch, ALU.mult, reads=[pbn, V], writes=[bg_])
        P.dma("pool", BG[:, :, t0:t0 + 512], bg_[:], reads=[bg_], writes=[BG])
        for d in range(2):
            rv = (lambda ap: ap) if d == 0 else (lambda ap: ap[:, ::-1])
            P.scan(rv(cum[:]), mask[:], rv(lw[d][:]), 0.0, ALU.mult, ALU.add, reads=[mask, lw[d]], writes=[cum])
            P.tt("dve", cumx[:], cum[:], lw[d][:], ALU.subtract, reads=[cum, lw[d]], writes=[cumx])
            P.act(Ex[:], cumx[:], AF.Exp, reads=[cumx], writes=[Ex])
            P.act(Ei[:], cum[:], AF.Exp, reads=[cum], writes=[Ei])
            P.act(En[:], cum[:], AF.Exp, reads=[cum], writes=[En], scale=-1.0)
            at, rt, bt, kt_ = kinds4
            P.stt(at[:], kkt[:], -1.0, Ex[:], ALU.mult, ALU.mult, reads=[kkt, Ex], writes=[at])
            P.tt("dve", rt[:], zr[:], Ei[:], ALU.mult, reads=[zr, Ei], writes=[rt])
            P.tt("dve", tA[:], kkt[:], aa[d][:], ALU.mult, reads=[kkt, aa[d]], writes=[tA])
            P.tt("dve", bt[:], tA[:], En[:], ALU.mult, reads=[tA, En], writes=[bt])
            P.tt("dve", kt_[:], km[d][:], En[:], ALU.mult, reads=[km[d], En], writes=[kt_])
            for blk in range(4):
                ptr = pb[6 + blk % 2]
                for ki in range(4):
                    P.tr(ptr[:, ki * 128:(ki + 1) * 128], kinds4[ki][:, blk * 128:(blk + 1) * 128], ident[:],
                         reads=[kinds4[ki], ident], writes=[ptr])
                rs_ = rows_sb[blk % 2]
                P.act(rs_[:].rearrange("p h k j -> p k h j"), ptr[:].rearrange("p (k h j) -> p k h j", k=4, h=2),
                      AF.Copy, reads=[ptr], writes=[rs_])
                for hh in range(2):
                    P.dma("sp" if hh == 0 else "pool", ROWS[hh, d, t0 + blk * 128:t0 + (blk + 1) * 128, :, :], rs_[:, hh, :, :],
                          reads=[rs_], writes=[ROWS])
            col = LC - 1 if d == 0 else 0
            P.act(pe8[:], cum[:].rearrange("p (a b) -> p a b", b=LC)[:, :, col], AF.Exp, reads=[cum], writes=[pe8])
            ptp = pb[6]
            P.tr(ptp[0:8, 0:128], pe8[:], ident[:], reads=[pe8, ident], writes=[ptp])
            P.act(pe8T[:], ptp[0:8, 0:128], AF.Copy, reads=[ptp], writes=[pe8T])
            P.dma("sp", PEND[d, c * 8:(c + 1) * 8, :, :].rearrange("m h j -> m (h j)"), pe8T[:], reads=[pe8T], writes=[PEND])

    x0 = nctx.x32[0]; x1 = nctx.x32[1]
    BC = [[None, None], [None, None]]
    for i, par in enumerate((x0, x1)):
        flat = par.t[:].rearrange("p a b -> p (a b)")
        for d in range(2):
            v_ = flat[:, d * 2048:(d + 1) * 2048].rearrange("p (s k j) -> p s k j", s=SCH, k=4)
            BC[d][i] = carve_multi(P, [par], v_, f"BC{d}_{i}")
    PE_sb = [carve_multi(P, [zc], zc.t[:].rearrange("p a b -> p (a b)")[:, 0:2048].rearrange("p (d m j) -> p d m j", d=2, m=16), "pend_sb0"),
             carve_multi(P, slots[0:4], big.t[:, 0:4, :].rearrange("p a b -> p (a b)").rearrange("p (d m j) -> p d m j", d=2, m=16), "pend_sb1")]
    St = P.sb([128, 2, 64], F32, "state"); P.memset("dve", St[:], 0.0, writes=[St])
    tmp = P.sb([128, 2, 64], F32, "sc_tmp"); sa = P.sb([128, 2], F32, "sc_sa")
    NST = S // SCH

    def load_bc(q):
        for d in range(2):
            buf = BC[d][q % 2]
            tstart = q * SCH if d == 0 else S - (q + 1) * SCH
            for hh in range(2):
                src = bass.AP(ROWS.t, ((hh * 2 + d) * S + tstart) * 256, [[0, 64], [1, SCH * 256]])
                P.dma("sp" if d == 0 else "act", buf.t[64 * hh:64 * hh + 64].rearrange("p s k j -> p (s k j)"), src,
                      reads=[ROWS], writes=[buf])

    def load_pend(gq):
        buf = PE_sb[gq % 2]
        for d in range(2):
            m0 = 16 * gq if d == 0 else 48 - 16 * gq
            for hh in range(2):
                src = bass.AP(PEND.t, (d * 64 + m0) * 128 + hh * 64, [[0, 64], [128, 16], [1, 64]])
                P.dma("pool", buf[64 * hh:64 * hh + 64, d, :, :], src, reads=[PEND], writes=[buf])

    load_bc(0)
    load_pend(0)
    load_pend(1)
    pstride = BC[0][0].t.ap[0][0]
    for q in range(NST):
        if q + 1 < NST:
            load_bc(q + 1)
        b0 = BC[0][q % 2]; b1 = BC[1][q % 2]
        for s_ in range(SCH):
            tau = q * SCH + s_
            l0 = s_; l1 = SCH - 1 - s_
            if tau % LC == 0 and tau > 0:
                qc_ = tau // LC - 1
                pbuf = PE_sb[(qc_ // 16) % 2]
                m0_ = qc_ % 16
                m1_ = 15 - (qc_ % 16)
                base = pbuf.t[:, 0, m0_, :]
                off1 = pbuf.t[:, 1, m1_, :]
                pap = bass.AP(base.tensor, base.offset, [[base.ap[0][0], 128], [off1.offset - base.offset, 2], [1, 64]])
                P.tt("dve", St[:], St[:], pap, ALU.mult, reads=[St, pbuf], writes=[St])
                if tau % 1024 == 0 and tau + 1024 < S:
                    load_pend(tau // 1024 + 1)

            def rowap(kind):
                a0_ = b0.t[:, l0, kind, :]
                a1_ = b1.t[:, l1, kind, :]
                return bass.AP(a0_.tensor, a0_.offset, [[a0_.ap[0][0], 128], [a1_.offset - a0_.offset, 2], [1, 64]])
            P.tt("dve", tmp[:], St[:], rowap(0), ALU.mult, reads=[St, b0, b1], writes=[tmp])
            P.red(sa[:], tmp[:], ALU.add, reads=[tmp], writes=[sa])
            for d in range(2):
                bb = b0 if d == 0 else b1
                ld = l0 if d == 0 else l1
                P.stt(St[:, d, :], bb.t[:, ld, 2, :], sa[:, d:d + 1], St[:, d, :], ALU.mult, ALU.add,
                      reads=[bb, sa, St], writes=[St])
            for d in range(2):
                bb = b0 if d == 0 else b1
                ld = l0 if d == 0 else l1
                tcol = tau if d == 0 else S - 1 - tau
                P.stt(St[:, d, :], bb.t[:, ld, 3, :], V[:, tcol:tcol + 1], St[:, d, :], ALU.mult, ALU.add,
                      reads=[bb, V, St], writes=[St])
            P.tt("dve", tmp[:], St[:], rowap(1), ALU.mult, reads=[St, b0, b1], writes=[tmp])
            P.red(Yout[:, :, tau], tmp[:], ALU.add, reads=[tmp], writes=[Yout])

    out_fn = out_mk(P, lambda i: carve_multi(P, [zst[i]], zst[i].t[:].rearrange("p a b -> p (a b)")[:, 0:512], f"mo{i}")) if out_mk else simple_out(P, out_d)
    h0 = nctx.hn[0]; h1 = nctx.hn[1]
    def hslot(par, i):
        return carve_multi(P, [par], par.t[:].rearrange("p a b -> p (a b)").bitcast(F32)[:, i * 512:(i + 1) * 512], f"hs{i}")
    ysum = hslot(h0, 0); yc = hslot(h0, 1); ysq = hslot(h0, 2); rs4 = hslot(h0, 3)
    oo4 = [hslot(h1, 0), hslot(h1, 1)]
    for c in range(n2):
        t0 = c * 512
        bg_ = bgs[c % 2]
        P.dma("pool", bg_[:], BG[:, :, t0:t0 + 512], reads=[BG], writes=[bg_])
        yb_rev = Yout[:, 1, S - t0 - 512:S - t0][:, ::-1]
        P.tt("dve", ysum[:], Yout[:, 0, t0:t0 + 512], yb_rev, ALU.add, reads=[Yout], writes=[ysum])
        pm = pb[c % 2]
        P.mm(pm[:], bones[:], ysum[:], reads=[bones, ysum], writes=[pm])
        P.stt(yc[:], pm[:], -1.0 / 64, ysum[:], ALU.mult, ALU.add, reads=[pm, ysum], writes=[yc])
        P.tt("dve", ysq[:], yc[:], yc[:], ALU.mult, reads=[yc], writes=[ysq])
        pv_ = pb[2 + c % 2]
        P.mm(pv_[:], bones[:], ysq[:], reads=[bones, ysq], writes=[pv_])
        P.act(rs4[:], pv_[:], AF.Sqrt, reads=[pv_, dv], writes=[rs4], scale=1.0 / 64, bias=dv[:, 9:10])
        P.recip(rs4[:], rs4[:], reads=[rs4], writes=[rs4])
        P.tt("dve", yc[:], yc[:], rs4[:], ALU.mult, reads=[yc, rs4], writes=[yc])
        P.ts("dve", yc[:], yc[:], bvec[:, 19:20], bvec[:, 20:21], ALU.mult, ALU.add, reads=[yc, bvec], writes=[yc])
        P.tt("dve", yc[:], yc[:], bg_[:, 0, :], ALU.add, reads=[yc, bg_], writes=[yc])
        o_ = oo4[c % 2]
        P.tt("dve", o_[:], yc[:], bg_[:, 1, :], ALU.mult, reads=[yc, bg_], writes=[o_])
        out_fn(o_, o_[:], t0, 512)


def host_B(d, L, j):
    cs = slice(128 * j, 128 * (j + 1))
    mu = d["rwkv_mu"][L]
    colsets = [np.arange(128 * j, 128 * j + 128), 512 + np.arange(128 * j, 128 * j + 128),
               1024 + np.arange(128 * j, 128 * j + 128), 1536 + np.arange(128), 1664 + np.arange(128), 1792 + np.arange(128)]
    bvec = np.zeros((128, NV), np.float32)
    for k, cols in enumerate(colsets):
        bvec[:, k] = mu[0, cols]; bvec[:, 6 + k] = mu[1, cols]
    bvec[:, 12] = d["rwkv_w0"][L][0, cs]; bvec[:, 13] = d["rwkv_w0"][L][1, cs]
    bvec[:, 14] = d["rwkv_a0"][L][0, cs]; bvec[:, 15] = d["rwkv_a0"][L][1, cs]
    bvec[:, 16] = d["rwkv_kk"][L][cs]; bvec[:, 17] = d["rwkv_ka"][L][cs]
    bvec[:, 18] = d["rwkv_rk"][L].reshape(-1)[cs]
    bvec[:, 19] = d["rwkv_lnx_w"][L][cs]; bvec[:, 20] = d["rwkv_lnx_b"][L][cs]
    w2 = np.ascontiguousarray(d["rwkv_w2"][L][:, :, cs].reshape(128, 128))
    a2 = np.ascontiguousarray(d["rwkv_a2"][L][:, :, cs].reshape(128, 128))
    g2 = np.ascontiguousarray(d["rwkv_g2"][L][:, cs])
    return colsets, bvec, w2, a2, g2


def consts_B():
    ident = np.eye(128, dtype=np.float32)
    bones = np.zeros((128, 128), np.float32)
    bones[:64, :64] = 1.0; bones[64:, 64:] = 1.0
    return ident, bones


TT = 1024
NE = 32


class WStream:
    def __init__(self, P, nstg=4, nbf=3):
        self.P = P
        self.stg = [P.sb([128, 2048], F32, f"ws_stg{i}") for i in range(nstg)]
        self.bf = [P.sb([128, 4096], BF16, f"ws_bf{i}") for i in range(nbf)]
        self.i = 0
        self.j = 0

    def fetch(self, src, shape, src_buf, q=None):
        P = self.P
        a, b = shape
        n = a * b
        bf = self.bf[self.i % len(self.bf)]
        self.i += 1
        bv = bf[:, 0:n].rearrange("p (a b) -> p a b", a=a)
        h = a // 2 if a >= 2 else a
        for (lo, hi) in ((0, h), (h, a)):
            if lo >= hi:
                continue
            st = self.stg[self.j % len(self.stg)]
            qq = q or ("sp", "act", "pool")[self.j % 3]
            self.j += 1
            sv = st[:, 0:(hi - lo) * b].rearrange("p (a b) -> p a b", a=hi - lo)
            P.dma(qq, sv, src[:, lo:hi, :], reads=[src_buf], writes=[st])
            P.cp("dve", bv[:, lo:hi, :], sv, reads=[st], writes=[bf])
        return bf, bv


class Scratch:
    def __init__(self, P, nfloats, name):
        self.P = P
        self.buf = P.sb([128, nfloats], F32, name)
        self.kids = []

    def take(self, off, n, dt=F32, name=""):
        v = self.buf.t[:, off:off + n]
        if dt == BF16:
            v = v.bitcast(BF16)
        b = Buf(v, name)
        rd = []
        for (o2, n2, k) in self.kids:
            if o2 < off + n and off < o2 + n2:
                if k.last_w is not None:
                    rd.append(k.last_w)
                rd.extend(k.readers)
        b.readers = rd
        self.kids.append((off, n, b))
        return b


def stream(ws, pieces):
    nxt = ws.fetch(*pieces[0])
    for i in range(len(pieces)):
        cur = nxt
        if i + 1 < len(pieces):
            nxt = ws.fetch(*pieces[i + 1])
        yield i, cur[0], cur[1]


def emit_P2(P, hT_d, mixT_d, memT_d, w_out_d, glu_w_d, pvec_d, nrm_d, wq_d, wkv_d, wo_d, router_d,
            w1_d, w3_d, w2_d, ident_d, out_d, final_norm=False, xwaits=(), pre=None):
    if pre is not None:
        pre(P)
    pb = [P.ps([128, 512], F32, f"bank{i}") for i in range(8)]
    ws = WStream(P)
    HT = P.sb([128, KT, TT], F32, "HT")
    XN = P.sb([128, KT, TT], BF16, "XN")
    ones_bf = P.sb([128, 128], BF16, "ones_bf"); P.memset("pool", ones_bf[:], 1.0, writes=[ones_bf])
    ones32 = P.sb([128, 128], F32, "ones32"); P.memset("pool", ones32[:], 1.0, writes=[ones32])
    ident = P.sb([128, 128], F32, "ident"); P.dma("sp", ident[:], ident_d[:], writes=[ident])
    eps = P.sb([128, 1], F32, "eps"); P.memset("pool", eps[:], RMS_EPS, writes=[eps])
    pvec = P.sb([128, 4, 3], F32, "pvec"); P.dma("sp", pvec[:], pvec_d[:], writes=[pvec])
    nrm = P.sb([128, KT, 4], F32, "nrm"); P.dma("sp", nrm[:], nrm_d[:], writes=[nrm])
    sq = [P.sb([128, 512], BF16, f"sq{i}") for i in range(2)]
    sd = P.sb([128, 512], F32, "sd"); rstd = P.sb([128, 512], F32, "rstd")
    hsrc = hT_d[:].rearrange("(kt p) t -> p kt t", p=128)
    for k4 in range(4):
        P.dma("sp" if k4 % 2 == 0 else "act", HT[:, k4 * 4:(k4 + 1) * 4, :], hsrc[:, k4 * 4:(k4 + 1) * 4, :], writes=[HT])

    def rms_rstd(tiles, nfeat, ps):
        for i, (b_, ap_) in enumerate(tiles):
            s_ = sq[i % 2]
            P.act(s_[:], ap_, AF.Square, reads=[b_], writes=[s_])
            P.mm(ps[:], ones_bf[:], s_[:], start=(i == 0), stop=(i == len(tiles) - 1), reads=[ones_bf, s_], writes=[ps])
        P.act(sd[:], ps[:], AF.Sqrt, reads=[ps, eps], writes=[sd], scale=1.0 / nfeat, bias=eps[:])
        P.recip(rstd[:], sd[:], reads=[sd], writes=[rstd])

    S1 = Scratch(P, 11520, "SC"); S2 = S1
    gluw_b = S1.take(0, 1024, BF16, "gluw"); gluw = Buf(gluw_b.t.rearrange("p (a b) -> p a b", a=4), "gluw"); gluw.readers = gluw_b.readers
    S1.kids[-1] = (0, 1024, gluw)
    gsrc = glu_w_d[:].rearrange("(kt p) n -> p kt n", p=128)
    _, gv = ws.fetch(gsrc, (4, 512), glu_w_d, q="act")
    P.cp("pool", gluw[:], gv, reads=[ws.bf[(ws.i - 1) % 3]], writes=[gluw])
    msrc = mixT_d[:].rearrange("(kt p) t -> p kt t", p=128)
    glb_b = S1.take(1024, 1024, BF16, "glb"); glb = Buf(glb_b.t.rearrange("p (a b) -> p a b", a=4), "glb"); S1.kids[-1] = (1024, 1024, glb)
    od0 = [S1.take(2048 + 512 * i, 512, F32, f"od0_{i}") for i in range(4)]
    t32 = [S1.take(4096 + 512 * i, 512, F32, f"t32_{i}") for i in range(2)]
    for ch in range(2):
        cs = slice(ch * 512, (ch + 1) * 512)
        sQ = [ws.stg[0], ws.stg[1], ws.stg[2]]
        vQ = [q_[:].rearrange("p (a b) -> p a b", a=4) for q_ in sQ]
        for g4 in range(2):
            P.dma("sp", vQ[g4], msrc[:, g4 * 4:(g4 + 1) * 4, cs], reads=[mixT_d], writes=[sQ[g4]])
            P.cp("pool", XN[:, g4 * 4:(g4 + 1) * 4, cs], vQ[g4], reads=[sQ[g4]], writes=[XN])
        sB = sQ[2]; vB = vQ[2]
        P.dma("act", vB, msrc[:, 8:12, cs], reads=[mixT_d], writes=[sB])
        rms_rstd([(sB, vB[:, i, :]) for i in range(4)], 512, pb[0])
        for i in range(4):
            P.stt(XN[:, 8 + i, cs], vB[:, i, :], pvec[:, i, 1:2], rstd[:], ALU.mult, ALU.mult,
                  reads=[sB, pvec, rstd], writes=[XN])
        sG = sQ[0]; vG = vQ[0]
        P.dma("sp", vG, msrc[:, 12:16, cs], reads=[mixT_d], writes=[sG])
        P.cp("pool", glb[:], vG, reads=[sG], writes=[glb])
        for n in range(4):
            pg = pb[1 + n % 2]
            for kk in range(4):
                P.mm(pg[:], gluw[:, kk, n * 128:(n + 1) * 128], glb[:, kk, :], start=(kk == 0), stop=(kk == 3),
                     reads=[gluw, glb], writes=[pg])
            tg = t32[n % 2]
            P.act(tg[:], pg[:], AF.Sigmoid, reads=[pg, pvec], writes=[tg], bias=pvec[:, n, 0:1])
            P.tt("dve", od0[n][:], vG[:, n, :], tg[:], ALU.mult, reads=[sG, tg], writes=[od0[n]])
        rms_rstd([(od0[n], od0[n][:]) for n in range(4)], 512, pb[3])
        for n in range(4):
            P.stt(XN[:, 12 + n, cs], od0[n][:], pvec[:, n, 2:3], rstd[:], ALU.mult, ALU.mult,
                  reads=[od0[n], pvec, rstd], writes=[XN])

    def linear_acc(w_d, X, nkt, row0=0):
        wsrc = w_d[row0:row0 + nkt * 128, :].rearrange("(kt p) n -> p kt n", p=128)
        cw = 4096 // nkt
        pieces = [(wsrc[:, :, i * cw:(i + 1) * cw], (nkt, cw), w_d) for i in range(2048 // cw)]
        it = 0
        for pi, wb_, wv in stream(ws, pieces):
            for dl in range(cw // 128):
                dt = pi * (cw // 128) + dl
                for ch in range(2):
                    ps = pb[it % 4]; it += 1
                    for k in range(nkt):
                        P.mm(ps[:], wv[:, k, dl * 128:(dl + 1) * 128], X[:, k, ch * 512:(ch + 1) * 512],
                             start=(k == 0), stop=(k == nkt - 1), reads=[wb_, X], writes=[ps])
                    P.tt("dve", HT[:, dt, ch * 512:(ch + 1) * 512], HT[:, dt, ch * 512:(ch + 1) * 512], ps[:], ALU.add,
                         reads=[HT, ps], writes=[HT])

    def norm_to_XN(gcol):
        for ch in range(2):
            cs = slice(ch * 512, (ch + 1) * 512)
            rms_rstd([(HT, HT[:, k, cs]) for k in range(KT)], D, pb[4 + ch])
            for k in range(KT):
                P.stt(XN[:, k, cs], HT[:, k, cs], nrm[:, k, gcol:gcol + 1], rstd[:], ALU.mult, ALU.mult,
                      reads=[HT, nrm, rstd], writes=[XN])

    linear_acc(w_out_d, XN, KT)

    norm_to_XN(0)
    def shaped(b, fmt, **kw):
        nb = Buf(b.t.rearrange(fmt, **kw), b.name); nb.readers = b.readers; nb.last_w = b.last_w
        return nb
    sc1 = S1; sc2 = S2
    mnb = sc1.take(0, 2048, BF16, "MN"); MN = shaped(mnb, "p (a b) -> p a b", a=KT); sc1.kids[-1] = (0, 2048, MN)
    kxb = sc1.take(2048, 2048, BF16, "KX"); KX = shaped(kxb, "p (a b) -> p a b", a=KT); sc1.kids[-1] = (2048, 2048, KX)
    vxb = sc2.take(4096, 2048, BF16, "VX"); VX = shaped(vxb, "p (a b) -> p a b", a=2); sc2.kids[-1] = (4096, 2048, VX)
    qhb = sc1.take(6144, 2048, BF16, "QH"); QH = shaped(qhb, "p (a b) -> p a b", a=4); sc1.kids[-1] = (6144, 2048, QH)
    ohb = sc1.take(8192, 2048, BF16, "OH"); OH = shaped(ohb, "p (a b) -> p a b", a=4); sc1.kids[-1] = (8192, 2048, OH)
    sMs = [ws.stg[0], ws.stg[1]]
    vMs = [q_[:].rearrange("p (a b) -> p a b", a=8) for q_ in sMs]
    msrc2 = memT_d[:].rearrange("(kt p) m -> p kt m", p=128)
    for g8 in range(2):
        P.dma("sp", vMs[g8], msrc2[:, g8 * 8:(g8 + 1) * 8, :], writes=[sMs[g8]])
    for i in range(KT):
        s_ = sq[i % 2]
        P.act(s_[:, 0:256], vMs[i // 8][:, i % 8, :], AF.Square, reads=[sMs[i // 8]], writes=[s_])
        P.mm(pb[0][:, 0:256], ones_bf[:], s_[:, 0:256], start=(i == 0), stop=(i == KT - 1), reads=[ones_bf, s_], writes=[pb[0]])
    P.act(sd[:, 0:256], pb[0][:, 0:256], AF.Sqrt, reads=[pb[0], eps], writes=[sd], scale=1.0 / D, bias=eps[:])
    P.recip(rstd[:, 0:256], sd[:, 0:256], reads=[sd], writes=[rstd])
    for k in range(KT):
        P.stt(MN[:, k, :], vMs[k // 8][:, k % 8, :], nrm[:, k, 1:2], rstd[:, 0:256], ALU.mult, ALU.mult,
              reads=[sMs[k // 8], nrm, rstd], writes=[MN])
    ksrc = wkv_d[:].rearrange("(kt p) n -> p kt n", p=128)
    pieces = [(ksrc[:, :, i * 256:(i + 1) * 256], (KT, 256), wkv_d) for i in range(16)]
    for pi, wb_, wv in stream(ws, pieces):
        if pi < 8:
            for dl in range(2):
                ps = pb[(pi * 2 + dl) % 4]
                for k in range(KT):
                    P.mm(ps[:, 0:256], wv[:, k, dl * 128:(dl + 1) * 128], MN[:, k, :], start=(k == 0), stop=(k == KT - 1),
                         reads=[wb_, MN], writes=[ps])
                P.act(KX[:, pi * 2 + dl, :], ps[:, 0:256], AF.Copy, reads=[ps], writes=[KX])
        else:
            for mt in range(2):
                ps = pb[(pi * 2 + mt) % 4]
                for k in range(KT):
                    P.mm(ps[:, 0:256], MN[:, k, mt * 128:(mt + 1) * 128], wv[:, k, :], start=(k == 0), stop=(k == KT - 1),
                         reads=[wb_, MN], writes=[ps])
                P.act(VX[:, mt, (pi - 8) * 256:(pi - 7) * 256], ps[:, 0:256], AF.Copy, reads=[ps], writes=[VX])
    E = [sc2.take(10240 + 256 * i, 256, BF16, f"E{i}") for i in range(3)]
    rden = sc2.take(11008, 512, F32, "rden")
    qsrc = wq_d[:].rearrange("(kt p) n -> p kt n", p=128)
    xscale = 512 ** -0.5
    for h in range(4):
        pieces = [(qsrc[:, :, h * 512 + i * 256:h * 512 + (i + 1) * 256], (KT, 256), wq_d) for i in range(2)]
        for pi, wb_, wv in stream(ws, pieces):
            for dl in range(2):
                for ch in range(2):
                    ps = pb[(dl * 2 + ch) % 4]
                    for k in range(KT):
                        P.mm(ps[:], wv[:, k, dl * 128:(dl + 1) * 128], XN[:, k, ch * 512:(ch + 1) * 512],
                             start=(k == 0), stop=(k == KT - 1), reads=[wb_, XN], writes=[ps])
                    P.act(QH[:, pi * 2 + dl, ch * 512:(ch + 1) * 512], ps[:], AF.Copy, reads=[ps], writes=[QH])
        for ch in range(2):
            cs = slice(ch * 512, (ch + 1) * 512)
            es = []
            for mt in range(2):
                ps = pb[mt]
                for dt in range(4):
                    P.mm(ps[:], KX[:, 4 * h + dt, mt * 128:(mt + 1) * 128], QH[:, dt, cs], start=(dt == 0), stop=(dt == 3),
                         reads=[KX, QH], writes=[ps])
                e = E[(ch * 2 + mt) % 3]
                P.act(e[:], ps[:], AF.Exp, reads=[ps], writes=[e], scale=xscale)
                es.append(e)
            pdn = pb[2]
            for mt in range(2):
                P.mm(pdn[:], ones_bf[:], es[mt][:], start=(mt == 0), stop=(mt == 1), reads=[ones_bf, es[mt]], writes=[pdn])
            P.recip(rden[:], pdn[:], reads=[pdn], writes=[rden])
            for dt in range(4):
                po = pb[4 + dt % 4]
                for mt in range(2):
                    P.mm(po[:], VX[:, mt, h * 512 + dt * 128:h * 512 + (dt + 1) * 128], es[mt][:], start=(mt == 0), stop=(mt == 1),
                         reads=[VX, es[mt]], writes=[po])
                P.tt("dve", OH[:, dt, cs], po[:], rden[:], ALU.mult, reads=[po, rden], writes=[OH])
        linear_acc(wo_d, OH, 4, row0=h * 512)

    norm_to_XN(2)
    wrb = sc1.take(0, KT * 36, F32, "wr"); wr = shaped(wrb, "p (a b) -> p a b", a=KT); sc1.kids[-1] = (0, KT * 36, wr)
    P.dma("sp", wr[:], router_d[:].rearrange("(kt p) n -> p kt n", p=128), writes=[wr])
    for k in range(KT):
        P.ts("dve", wr[:, k, :], wr[:, k, :], nrm[:, k, 2:3], None, ALU.mult, reads=[wr, nrm], writes=[wr])
    ones_col = P.sb([128, 1], BF16, "ones_col"); P.memset("pool", ones_col[:], 1.0, writes=[ones_col])
    Gt = P.sb([128, 8, 32], F32, "Gt")
    lg = P.sb([128, 36], F32, "lg"); g8 = P.sb([128, 8], F32, "g8"); m8 = P.sb([128, 8], F32, "m8")
    sm = P.sb([128, 8], F32, "sm"); lem = P.sb([128, 4, 8], F32, "lem"); sel = P.sb([128, 32], F32, "sel")
    ex = P.sb([128, 32], F32, "ex")
    for tt_ in range(8):
        tsl = slice(tt_ * 128, (tt_ + 1) * 128)
        pss = pb[tt_ % 2]; pl = pb[2 + tt_ % 2]
        for k in range(KT):
            s_ = sq[k % 2]
            P.act(s_[:, 0:128], HT[:, k, tsl], AF.Square, reads=[HT], writes=[s_])
            P.mm(pss[:, 0:1], s_[:, 0:128], ones_col[:], start=(k == 0), stop=(k == KT - 1), reads=[s_, ones_col], writes=[pss])
        for k in range(KT):
            P.mm(pl[:, 0:36], HT[:, k, tsl], wr[:, k, :], start=(k == 0), stop=(k == KT - 1), reads=[HT, wr], writes=[pl])
        P.act(sm[:, 0:1], pss[:, 0:1], AF.Sqrt, reads=[pss, eps], writes=[sm], scale=1.0 / D, bias=eps[:])
        P.recip(sm[:, 0:1], sm[:, 0:1], reads=[sm], writes=[sm])
        P.ts("dve", lg[:], pl[:, 0:36], sm[:, 0:1], None, ALU.mult, reads=[pl, sm], writes=[lg])
        P.red(sm[:, 1:2], lg[:, 0:4], ALU.max, reads=[lg], writes=[sm])
        P.ts("dve", g8[:, 0:4], lg[:, 0:4], sm[:, 1:2], None, ALU.subtract, reads=[lg, sm], writes=[g8])
        P.act(g8[:, 4:8], g8[:, 0:4], AF.Exp, reads=[g8], writes=[g8])
        P.red(sm[:, 2:3], g8[:, 4:8], ALU.add, reads=[g8], writes=[sm])
        P.ts("dve", g8[:, 0:4], lg[:, 0:4], sm[:, 1:2], None, ALU.is_ge, reads=[lg, sm], writes=[g8])
        P.ts("dve", g8[:, 0:4], g8[:, 0:4], 1e30, -1e30, ALU.mult, ALU.add, reads=[g8], writes=[g8])
        pen = bass.AP(g8.t, 0, [[8, 128], [1, 4], [0, 8]])
        P.tt("dve", lem[:], lg[:, 4:36].rearrange("p (g e) -> p g e", g=4), pen, ALU.add, reads=[lg, g8], writes=[lem])
        lem2 = lem[:].rearrange("p g e -> p (g e)")
        P.op("dve", (lambda o_, i_: (lambda e: e.max(o_, i_)))(m8[:], lem2), reads=[lem], writes=[m8])
        P.ts("dve", sel[:], lem2, m8[:, 1:2], None, ALU.is_ge, reads=[lem, m8], writes=[sel])
        P.ts("dve", sm[:, 3:4], m8[:, 0:1], -1.0, None, ALU.mult, reads=[m8], writes=[sm])
        P.act(ex[:], lem2, AF.Exp, reads=[lem, sm], writes=[ex], bias=sm[:, 3:4])
        P.act(sm[:, 4:5], m8[:, 1:2], AF.Exp, reads=[m8, sm], writes=[sm], bias=sm[:, 3:4])
        P.ts("dve", sm[:, 4:5], sm[:, 4:5], 1.0, None, ALU.add, reads=[sm], writes=[sm])
        P.tt("dve", sm[:, 4:5], sm[:, 4:5], sm[:, 2:3], ALU.mult, reads=[sm], writes=[sm])
        P.recip(sm[:, 5:6], sm[:, 4:5], reads=[sm], writes=[sm])
        P.stt(Gt[:, tt_, :], ex[:], sm[:, 5:6], sel[:], ALU.mult, ALU.mult, reads=[ex, sm, sel], writes=[Gt])

    H1 = QH
    dg = [sc1.take(640 + 128 * i, 128, F32, f"dg{i}") for i in range(2)]
    gbc = [sc2.take(1024 + 512 * i, 512, F32, f"gbc{i}") for i in range(2)]
    sl = [sc2.take(2048 + 512 * i, 512, F32, f"sl{i}") for i in range(2)]
    for xs_ in xwaits:
        P.ext_wait("sp", xs_, 1)
        P.ext_wait("act", xs_, 1)
    for e_ in range(NE):
        w1s = w1_d[e_].rearrange("(kt p) n -> p kt n", p=128)
        w3s = w3_d[e_].rearrange("(kt p) n -> p kt n", p=128)
        w2s = w2_d[e_].rearrange("(ft p) n -> p ft n", p=128)
        pieces = [(w1s[:, :, 0:256], (KT, 256), w1_d), (w3s[:, :, 0:256], (KT, 256), w3_d),
                  (w1s[:, :, 256:512], (KT, 256), w1_d), (w3s[:, :, 256:512], (KT, 256), w3_d),
                  (w2s[:, :, 0:1024], (4, 1024), w2_d), (w2s[:, :, 1024:2048], (4, 1024), w2_d)]
        for ch in range(2):
            pgt = pb[6 + ch]
            for tq in range(4):
                tt_ = ch * 4 + tq
                d_ = dg[tq % 2]
                P.ts("dve", d_[:], ident[:], Gt[:, tt_, e_:e_ + 1], None, ALU.mult, reads=[ident, Gt], writes=[d_])
                P.mm(pgt[:, tq * 128:(tq + 1) * 128], ones32[:], d_[:], reads=[ones32, d_], writes=[pgt])
            P.act(gbc[ch][:], pgt[:], AF.Copy, reads=[pgt], writes=[gbc[ch]])
        hold = {}
        it = 0
        for pi, wb_, wv in stream(ws, pieces):
            if pi in (0, 2):
                hold["w1"] = (wb_, wv)
                continue
            if pi in (1, 3):
                half = pi // 2
                w1b, w1v = hold["w1"]
                for fl in range(2):
                    ft = half * 2 + fl
                    for ch in range(2):
                        cs = slice(ch * 512, (ch + 1) * 512)
                        p1 = pb[(it % 2) * 2]; p3 = pb[(it % 2) * 2 + 1]; it += 1
                        for k in range(KT):
                            P.mm(p1[:], w1v[:, k, fl * 128:(fl + 1) * 128], XN[:, k, cs], start=(k == 0), stop=(k == KT - 1),
                                 reads=[w1b, XN], writes=[p1])
                        for k in range(KT):
                            P.mm(p3[:], wv[:, k, fl * 128:(fl + 1) * 128], XN[:, k, cs], start=(k == 0), stop=(k == KT - 1),
                                 reads=[wb_, XN], writes=[p3])
                        s_ = sl[it % 2]
                        P.act(s_[:], p1[:], AF.Silu, reads=[p1], writes=[s_])
                        P.tt("dve", s_[:], s_[:], p3[:], ALU.mult, reads=[s_, p3], writes=[s_])
                        P.tt("dve", H1[:, ft, cs], s_[:], gbc[ch][:], ALU.mult, reads=[s_, gbc[ch]], writes=[H1])
                continue
            half = pi - 4
            for dl in range(8):
                dt = half * 8 + dl
                for ch in range(2):
                    cs = slice(ch * 512, (ch + 1) * 512)
                    ps = pb[4 + it % 2]; it += 1
                    for ft in range(4):
                        P.mm(ps[:], wv[:, ft, dl * 128:(dl + 1) * 128], H1[:, ft, cs], start=(ft == 0), stop=(ft == 3),
                             reads=[wb_, H1], writes=[ps])
                    P.tt("dve", HT[:, dt, cs], HT[:, dt, cs], ps[:], ALU.add, reads=[HT, ps], writes=[HT])

    osrc = out_d[:].rearrange("(kt p) t -> p kt t", p=128)
    if final_norm:
        for ch in range(2):
            cs = slice(ch * 512, (ch + 1) * 512)
            rms_rstd([(HT, HT[:, k, cs]) for k in range(KT)], D, pb[ch])
            for k in range(KT):
                P.stt(HT[:, k, cs], HT[:, k, cs], nrm[:, k, 3:4], rstd[:], ALU.mult, ALU.mult, reads=[HT, nrm, rstd], writes=[HT])
    for k4 in range(4):
        P.dma("sp" if k4 % 2 == 0 else "act", osrc[:, k4 * 4:(k4 + 1) * 4, :], HT[:, k4 * 4:(k4 + 1) * 4, :], reads=[HT], writes=[out_d])


def host_P2(d, L):
    pvec = np.zeros((128, 4, 3), np.float32)
    pvec[:, :, 0] = d["s5_glu_b"][L].reshape(4, 128).T
    pvec[:, :, 1] = d["mix_out_norm"][L][0].reshape(4, 128).T
    pvec[:, :, 2] = d["mix_out_norm"][L][1].reshape(4, 128).T
    nrm = np.zeros((128, 16, 4), np.float32)
    for i, v in enumerate((d["norm_cross"][L], d["norm_mem"][L], d["norm_moe"][L], d["norm_final"])):
        nrm[:, :, i] = v.reshape(16, 128).T
    router = np.ascontiguousarray(np.concatenate([d["router_group"][L], d["router_expert"][L]], axis=1))
    return pvec, nrm, router


import time as _time
from concourse.bass_utils import run_bass_kernel_spmd

O_B = 1536; O_C = 1536 + 1920; O_D = 1536 + 1920 + 1536
_PROGS = {}


def _din(P, name, shape):
    return P.dram(name, shape, F32, kind="ExternalInput")


def build_P1(layer_idx):
    nc = bass.Bass("TRN2", target_bir_lowering=False)
    P = _mk(Prog(nc))
    hT = _din(P, "hT", [D, S]); g_mix = _din(P, "g_mix", [D])
    w_a = _din(P, "w_a", [D, 384]); lamv = _din(P, "lamv_d", [4, 64]); subln = _din(P, "subln_d", [128])
    cosA = _din(P, "cosA", [128, S]); sinA = _din(P, "sinA", [128, S]); rmA = _din(P, "rmA", [128, 128])
    oaT = P.dram("oaT", [128, S], F32, kind="ExternalOutput")
    emit_A(P, layer_idx, hT, g_mix, w_a, lamv, subln, cosA, sinA, rmA, oaT)
    P.wait_all_dma("sp"); P.emit()
    P = _mk(Prog(nc))
    hT = Buf(hT.t, "hT"); g_mix = Buf(g_mix.t, "g_mix")
    w_c = _din(P, "w_c", [D, 384]); cosC = _din(P, "cosC", [128, S]); sinC = _din(P, "sinC", [128, S])
    rmC = _din(P, "rmC", [128, 128]); maskC = _din(P, "maskC", [128, 20, 512])
    ocT = P.dram("ocT", [128, S], F32, kind="ExternalOutput")
    emit_C(P, hT, g_mix, w_c, cosC, sinC, rmC, maskC, ocT)
    P.wait_all_dma("sp"); P.emit()
    P = _mk(Prog(nc))
    hT = Buf(hT.t, "hT"); g_mix = Buf(g_mix.t, "g_mix")
    w_d = _din(P, "w_d", [D, 128]); dpar_d = _din(P, "dpar_d", [128, 3, 8]); dskip_d = _din(P, "dskip_d", [128])
    bblk_d = _din(P, "bblk_d", [128, 2, 4, 128]); cblk_d = _din(P, "cblk_d", [128, 2, 4, 128])
    glT = P.dram("glT", [128, S], F32, kind="ExternalOutput")
    emit_D(P, hT, g_mix, w_d, dpar_d, dskip_d, bblk_d, cblk_d, glT)
    P.wait_all_dma("sp"); P.emit()
    P = _mk(Prog(nc))
    hT = Buf(hT.t, "hT"); g_mix = Buf(g_mix.t, "g_mix")
    w_b = _din(P, "w_b", [D, 768]); bvec_d = _din(P, "bvec_d", [128, NV])
    w2_d = _din(P, "w2_d", [128, 128]); a2_d = _din(P, "a2_d", [128, 128]); g2_d = _din(P, "g2_d", [128, 128])
    ident_d = _din(P, "ident_d", [128, 128]); bones_d = _din(P, "bones_d", [128, 128])
    obT = P.dram("obT", [128, S], F32, kind="ExternalOutput")
    emit_B(P, hT, g_mix, w_b, bvec_d, w2_d, a2_d, g2_d, ident_d, bones_d, obT)
    P.wait_all_dma("sp"); P.emit()
    return nc


def build_P2(final_norm):
    nc = bass.Bass("TRN2", target_bir_lowering=False)
    P = _mk(Prog(nc))
    hT_d = _din(P, "hT", [D, TT]); mixT_d = _din(P, "mixT", [D, TT]); memT_d = _din(P, "memT", [D, 256])
    w_out_d = _din(P, "w_out", [D, D]); glu_w_d = _din(P, "glu_w", [512, 512]); pvec_d = _din(P, "pvec", [128, 4, 3])
    nrm_d = _din(P, "nrm", [128, 16, 4]); wq_d = _din(P, "wq", [D, D]); wkv_d = _din(P, "wkv", [D, 2 * D]); wo_d = _din(P, "wo", [D, D])
    router_d = _din(P, "router", [D, 36]); w1_d = _din(P, "w1", [NE, D, 512]); w3_d = _din(P, "w3", [NE, D, 512])
    w2_d = _din(P, "w2", [NE, 512, D]); ident_d = _din(P, "ident_d", [128, 128])
    out_d = P.dram("outT", [D, TT], F32, kind="ExternalOutput")
    emit_P2(P, hT_d, mixT_d, memT_d, w_out_d, glu_w_d, pvec_d, nrm_d, wq_d, wkv_d, wo_d, router_d, w1_d, w3_d, w2_d, ident_d,
            out_d, final_norm=final_norm)
    P.wait_all_dma("sp"); P.emit()
    return nc


def kernel(**inputs):
    d = {k: np.asarray(v) for k, v in inputs.items()}
    x = d["x"]
    cosA, sinA, rmA = consts_A()
    cosC, sinC, rmC, maskC = consts_C()
    ident, bones = consts_B()
    hT_b = [np.ascontiguousarray(x[b].T) for b in range(2)]
    out = None
    for L in range(2):
        w_in = d["w_in"][L]
        nc1 = build_P1(L)
        ins = []
        for c in range(8):
            b, j = divmod(c, 4)
            sl = lambda o: w_in[:, o + j * 128:o + (j + 1) * 128]
            colsets, bvec, w2, a2, g2 = host_B(d, L, j)
            dpar, bblk, cblk, dsk = host_D(d, L, j)
            ins.append({
                "hT": hT_b[b], "g_mix": d["norm_mix"][L],
                "w_a": np.ascontiguousarray(np.concatenate([sl(0), sl(512), sl(1024)], 1)),
                "lamv_d": d["diff_lambda"][L], "subln_d": d["diff_subln"][L], "cosA": cosA, "sinA": sinA, "rmA": rmA,
                "w_c": np.ascontiguousarray(np.concatenate([sl(O_C), sl(O_C + 512), sl(O_C + 1024)], 1)),
                "cosC": cosC, "sinC": sinC, "rmC": rmC, "maskC": maskC,
                "w_d": np.ascontiguousarray(sl(O_D)), "dpar_d": dpar, "dskip_d": dsk, "bblk_d": bblk, "cblk_d": cblk,
                "w_b": np.ascontiguousarray(w_in[:, O_B + np.concatenate(colsets)]), "bvec_d": bvec, "w2_d": w2, "a2_d": a2, "g2_d": g2,
                "ident_d": ident, "bones_d": bones,
            })
        res = run_bass_kernel_spmd(nc1, ins, core_ids=list(range(8)))
        mixT = [np.zeros((D, S), np.float32) for _ in range(2)]
        for c in range(8):
            b, j = divmod(c, 4)
            r = res.results[c]
            for gi, nm in enumerate(("oaT", "obT", "ocT", "glT")):
                mixT[b][gi * 512 + j * 128:gi * 512 + (j + 1) * 128, :] = r[nm]
        del res
        nc2 = build_P2(final_norm=(L == 1))
        pvec, nrm, router = host_P2(d, L)
        shared = {"w_out": d["w_out"][L], "glu_w": d["s5_glu_w"][L], "pvec": pvec, "nrm": nrm, "wq": d["xa_wq"][L],
                  "wkv": d["xa_wkv"][L], "wo": d["xa_wo"][L], "router": router, "w1": d["moe_w1"][L], "w3": d["moe_w3"][L],
                  "w2": d["moe_w2"][L], "ident_d": ident}
        ins = []
        for c in range(8):
            b, tq = divmod(c, 4)
            ts_ = slice(tq * TT, (tq + 1) * TT)
            m = dict(shared)
            m.update({"hT": np.ascontiguousarray(hT_b[b][:, ts_]), "mixT": np.ascontiguousarray(mixT[b][:, ts_]),
                      "memT": np.ascontiguousarray(d["mem"][b].T)})
            ins.append(m)
        res = run_bass_kernel_spmd(nc2, ins, core_ids=list(range(8)))
        new_hT = [np.zeros((D, S), np.float32) for _ in range(2)]
        for c in range(8):
            b, tq = divmod(c, 4)
            new_hT[b][:, tq * TT:(tq + 1) * TT] = res.results[c]["outT"]
        del res
        hT_b = new_hT
    out = np.stack([hT_b[b].T for b in range(2)], axis=0).astype(np.float32)
    return np.ascontiguousarray(out)
```

```python
import math


import contextlib
import numpy as np
import concourse.bass as bass
import concourse.mybir as mybir

F32 = mybir.dt.float32
BF16 = mybir.dt.bfloat16
I32 = mybir.dt.int32
U32 = mybir.dt.uint32
AF = mybir.ActivationFunctionType
ALU = mybir.AluOpType
AX = mybir.AxisListType

ENGS = ("pe", "act", "dve", "pool", "sp")


class Buf:
    __slots__ = ("t", "last_w", "readers", "name")

    def __init__(self, t, name=""):
        self.t = t
        self.last_w = None
        self.readers = []
        self.name = name

    def __getitem__(self, k):
        return self.t[k]


class Op:
    __slots__ = ("eng", "fn", "deps", "marked", "tick", "is_dma", "dsem", "dval", "dwaits", "inc", "xw", "xs")

    def __init__(self, eng, fn, is_dma=False):
        self.eng = eng
        self.fn = fn
        self.deps = []
        self.dwaits = {}
        self.marked = False
        self.tick = 0
        self.is_dma = is_dma
        self.dsem = None
        self.dval = 0
        self.inc = 16
        self.xw = None
        self.xs = None


_GUID = [0]


class Prog:
    def __init__(self, nc, n_dma_sems=40):
        _GUID[0] += 1
        self.pid = _GUID[0]
        self.nc = nc
        self.st = contextlib.ExitStack()
        self.ops = {e: [] for e in ENGS}
        self.n_dma_sems = n_dma_sems
        self.dma_tot = [0] * n_dma_sems
        self.dma_rr = 0
        self.buf_sem = {}
        self.uid = 0
        self.coll = []

    def sb(self, shape, dt=F32, name=None):
        self.uid += 1
        t = self.st.enter_context(self.nc.sbuf_tensor(f"S{self.pid}_{self.uid}_" + (name or "sb"), list(shape), dt))
        return Buf(t, name or f"sb{self.uid}")

    def ps(self, shape, dt=F32, name=None):
        self.uid += 1
        t = self.st.enter_context(self.nc.psum_tensor(f"P{self.pid}_{self.uid}_" + (name or "ps"), list(shape), dt))
        return Buf(t, name or f"ps{self.uid}")

    def dram(self, name, shape, dt=F32, kind="Internal"):
        t = self.nc.dram_tensor(name, list(shape), dt, kind=kind)
        return Buf(t, name)

    def carve(self, parent, ap, name=""):
        b = Buf(ap, name)
        b.last_w = parent.last_w
        b.readers = list(parent.readers)
        return b

    def view(self, name=""):
        return Buf(None, name)

    def _dep_on(self, op, ev):
        if ev is None:
            return
        if ev.is_dma:
            s = ev.dsem
            op.dwaits[s] = max(op.dwaits.get(s, 0), self.dma_tot[s])
        else:
            if ev.eng == "pe" and op.eng == "pe":
                return
            op.deps.append(ev)
            ev.marked = True

    def op(self, eng, fn, reads=(), writes=()):
        o = Op(eng, fn)
        self._track(o, reads, writes)
        self.ops[eng].append(o)
        return o

    def _track(self, o, reads, writes):
        for b in reads:
            self._dep_on(o, b.last_w)
        for b in writes:
            self._dep_on(o, b.last_w)
            for r in b.readers:
                if r is not o and not (r.eng == o.eng and not r.is_dma):
                    self._dep_on(o, r)
        for b in writes:
            b.last_w = o
            b.readers = []
        for b in reads:
            if b.last_w is o:
                continue
            if not o.is_dma:
                b.readers = [r for r in b.readers if r.is_dma or r.eng != o.eng]
            b.readers.append(o)

    def dma(self, eng, out_ap, in_ap, reads=(), writes=(), sem_key=None, **kw):
        o = Op(eng, lambda e: e.dma_start(out=out_ap, in_=in_ap, **kw), is_dma=True)
        key = sem_key if sem_key is not None else (id(writes[0]) if writes else id(reads[0]))
        if key not in self.buf_sem:
            self.buf_sem[key] = self.dma_rr % self.n_dma_sems
            self.dma_rr += 1
        s = self.buf_sem[key]
        self._track(o, reads, writes)
        self.dma_tot[s] += 16
        o.dsem = s
        o.dval = self.dma_tot[s]
        self.ops[eng].append(o)
        return o

    def ext_wait(self, eng, sem, val):
        o = Op(eng, None)
        o.xw = (sem, val)
        self.ops[eng].append(o)
        return o

    def collective(self, kind, in_ap, out_ap, groups, reads=(), writes=(), op=None, ext_sem=None):
        alu = op if op is not None else mybir.AluOpType.bypass
        o = Op("pool", lambda e: e.collective_compute(kind, alu, replica_groups=groups, ins=[in_ap], outs=[out_ap]), is_dma=True)
        if ext_sem is not None:
            for b in reads:
                self._dep_on(o, b.last_w)
            o.xs = ext_sem
            o.dsem = None
            self.ops["pool"].append(o)
            return o
        s = self.n_dma_sems + len(self.coll)
        self.coll.append(o)
        self.dma_tot.append(0)
        self._track(o, reads, writes)
        self.dma_tot[s] += 1
        o.dsem = s
        o.dval = 1
        o.inc = 1
        self.ops["pool"].append(o)
        return o

    def wait_all_dma(self, eng="sp"):
        tot = list(self.dma_tot)
        o = Op(eng, None)
        o.dwaits = {s: v for s, v in enumerate(tot) if v > 0}
        self.ops[eng].append(o)

    def emit(self):
        nc = self.nc
        st = self.st
        esem = {e: nc.alloc_semaphore(name=f"es{self.pid}_{e}") for e in ENGS}
        dsem = [nc.alloc_semaphore(name=f"ds{self.pid}_{i}") for i in range(self.n_dma_sems + len(self.coll))]
        for e in ENGS:
            c = 0
            for o in self.ops[e]:
                if o.marked and not o.is_dma:
                    c += 1
                    o.tick = c
        ops = self.ops

        def replay(ename, h):
            known = {}
            for o in ops[ename]:
                for p in o.deps:
                    k = ("e", p.eng)
                    if known.get(k, 0) < p.tick:
                        h.wait_ge(esem[p.eng], p.tick)
                        known[k] = p.tick
                for s, v in o.dwaits.items():
                    k = ("d", s)
                    if known.get(k, 0) < v:
                        h.wait_ge(dsem[s], v)
                        known[k] = v
                if o.xw is not None:
                    h.wait_ge(o.xw[0], o.xw[1])
                if o.fn is None:
                    continue
                ins = o.fn(h)
                if o.xs is not None:
                    ins.then_inc(o.xs, 1)
                elif o.is_dma:
                    ins.then_inc(dsem[o.dsem], getattr(o, "inc", 16))
                elif o.marked:
                    ins.then_inc(esem[ename], 1)

        with nc.Block() as block:
            @block.tensor
            def _(h):
                replay("pe", h)

            @block.scalar
            def _(h):
                replay("act", h)

            @block.vector
            def _(h):
                replay("dve", h)

            @block.gpsimd
            def _(h):
                replay("pool", h)

            @block.sync
            def _(h):
                replay("sp", h)
        st.close()
        nc.all_engine_barrier()
        nc.clear_and_free_semaphores(list(esem.values()) + dsem)
        nc.all_engine_barrier()


def _mk(P):
    def mm(out, lhsT, rhs, start=True, stop=True, reads=(), writes=()):
        return P.op("pe", lambda e: e.matmul(out, lhsT, rhs, start=start, stop=stop), reads, writes)

    def tr(out, in_, ident, reads=(), writes=()):
        return P.op("pe", lambda e: e.transpose(out, in_, ident), reads, writes)

    def act(out, in_, func, reads=(), writes=(), **kw):
        return P.op("act", lambda e: e.activation(out, in_, func, **kw), reads, writes)

    def tt(eng, out, in0, in1, op, reads=(), writes=()):
        return P.op(eng, lambda e: e.tensor_tensor(out, in0, in1, op), reads, writes)

    def ts(eng, out, in0, s1, s2, op0, op1=None, reads=(), writes=(), **kw):
        if op1 is None:
            return P.op(eng, lambda e: e.tensor_scalar(out, in0, s1, None, op0, **kw), reads, writes)
        return P.op(eng, lambda e: e.tensor_scalar(out, in0, s1, s2, op0, op1, **kw), reads, writes)

    def stt(out, in0, scalar, in1, op0, op1, reads=(), writes=(), **kw):
        return P.op("dve", lambda e: e.scalar_tensor_tensor(out, in0, scalar, in1, op0, op1, **kw), reads, writes)

    def cp(eng, out, in_, reads=(), writes=()):
        if eng == "act":
            return P.op(eng, lambda e: e.copy(out, in_), reads, writes)
        return P.op(eng, lambda e: e.tensor_copy(out, in_), reads, writes)

    def red(out, in_, op, axis=AX.X, reads=(), writes=()):
        return P.op("dve", lambda e: e.tensor_reduce(out, in_, axis, op), reads, writes)

    def memset(eng, ap, val, writes=()):
        return P.op(eng, lambda e: e.memset(ap, val), (), writes)

    def scan(out, d0, d1, init, op0, op1, reads=(), writes=()):
        return P.op("dve", lambda e: e.tensor_tensor_scan(out, d0, d1, init, op0, op1), reads, writes)

    def recip(out, in_, reads=(), writes=()):
        return P.op("dve", lambda e: e.reciprocal(out, in_), reads, writes)

    P.mm, P.tr, P.act, P.tt, P.ts, P.stt, P.cp, P.red, P.memset, P.scan, P.recip = (
        mm, tr, act, tt, ts, stt, cp, red, memset, scan, recip)
    return P


def new_prog(n_dma_sems=40):
    nc = bass.Bass("TRN2", target_bir_lowering=False)
    return _mk(Prog(nc, n_dma_sems))


import math
import numpy as np

S = 4096; D = 2048; KT = 16; CH = 512; NCH = S // CH
RMS_EPS = 1e-6


def bcast_rows(d, nelem, parts=128, offset=0):
    return bass.AP(d.t, offset, [[0, parts], [1, nelem]])


def load_w_bf16(P, w_dram, ncols, name, q0="sp", q1="pool", cast_eng="pool", stg_buf=None):
    stg = stg_buf if stg_buf is not None else P.sb([128, KT, ncols], F32, name + "_stg")
    wb = P.sb([128, KT, ncols], BF16, name)
    src = w_dram[:].rearrange("(kt p) n -> p kt n", p=128)
    h = KT // 2
    P.dma(q0, stg[:, 0:h, 0:ncols], src[:, 0:h, :], writes=[stg])
    P.dma(q1, stg[:, h:, 0:ncols], src[:, h:, :], writes=[stg])
    P.cp(cast_eng, wb[:], stg[:, :, 0:ncols], reads=[stg], writes=[wb])
    return wb


class NormCtx:
    def __init__(self, P, g_dram, ch=CH):
        self.P = P
        self.ch = ch
        CH = ch
        self.ones_bf = P.sb([128, 128], BF16, "ones_bf")
        P.memset("pool", self.ones_bf[:], 1.0, writes=[self.ones_bf])
        self.g = P.sb([128, KT], F32, "g_norm")
        P.dma("sp", self.g[:], g_dram[:].rearrange("(kt p) -> p kt", p=128), writes=[self.g],
              allow_slow_non_contiguous=True)
        self.eps = P.sb([128, 1], F32, "eps_t")
        P.memset("pool", self.eps[:], RMS_EPS, writes=[self.eps])
        self.x32 = [P.sb([128, KT, CH], F32, f"x32_{i}") for i in range(2)]
        self.hn = [P.sb([128, KT, CH], BF16, f"hn_{i}") for i in range(2)]
        self.sq = [P.sb([128, CH], BF16, f"sq_{i}") for i in range(3)]
        self.sd = P.sb([128, CH], F32, "sd")
        self.rstd = P.sb([128, CH], F32, "rstd")

    def load(self, hT_dram, c, col0=None):
        P = self.P
        CH = self.ch
        x32 = self.x32[c % 2]
        c0 = c * CH if col0 is None else col0
        if callable(hT_dram):
            src, sbuf_ = hT_dram(c0, CH)
        else:
            src, sbuf_ = hT_dram[:, c0:c0 + CH].rearrange("(kt p) t -> p kt t", p=128), hT_dram
        h = KT // 2
        P.dma("sp", x32[:, 0:h, :], src[:, 0:h, :], reads=[sbuf_], writes=[x32])
        P.dma("pool", x32[:, h:, :], src[:, h:, :], reads=[sbuf_], writes=[x32])

    def norm(self, c, ps_ss, dmodel=D):
        P = self.P
        CH = self.ch
        x32 = self.x32[c % 2]; hn = self.hn[c % 2]
        for k in range(KT):
            sq = self.sq[k % 3]
            P.act(sq[:], x32[:, k, :], AF.Square, reads=[x32], writes=[sq])
            P.mm(ps_ss[:, 0:CH], self.ones_bf[:], sq[:], start=(k == 0), stop=(k == KT - 1),
                 reads=[self.ones_bf, sq], writes=[ps_ss])
        P.act(self.sd[:], ps_ss[:, 0:CH], AF.Sqrt, reads=[ps_ss, self.eps], writes=[self.sd],
              scale=1.0 / dmodel, bias=self.eps[:])
        P.recip(self.rstd[:], self.sd[:], reads=[self.sd], writes=[self.rstd])
        for k in range(KT):
            P.stt(hn[:, k, :], x32[:, k, :], self.g[:, k:k + 1], self.rstd[:], ALU.mult, ALU.mult,
                  reads=[x32, self.g, self.rstd], writes=[hn])
        return hn


def proj_qkv(P, nctx, hT, wb, pb, cosT, sinT, rm, QT, KTt, V):
    q32 = [P.sb([128, CH], F32, f"q32_{i}") for i in range(2)]
    t1 = [P.sb([128, CH], F32, f"t1_{i}") for i in range(2)]
    t2 = [P.sb([128, CH], F32, f"t2_{i}") for i in range(2)]
    nctx.load(hT, 0)
    for c in range(NCH):
        if c + 1 < NCH:
            nctx.load(hT, c + 1)
        hn = nctx.norm(c, pb[0])
        cs = slice(c * CH, (c + 1) * CH)
        for qi, (dst, col0) in enumerate(((QT, 0), (KTt, 128))):
            pq = pb[1 + 2 * qi]; pr = pb[2 + 2 * qi]
            for k in range(KT):
                P.mm(pq[:], wb[:, k, col0:col0 + 128], hn[:, k, :], start=(k == 0), stop=(k == KT - 1),
                     reads=[wb, hn], writes=[pq])
            P.act(q32[qi][:], pq[:], AF.Copy, reads=[pq], writes=[q32[qi]])
            P.mm(pr[:], rm[:], q32[qi][:], reads=[rm, q32[qi]], writes=[pr])
            P.tt("dve", t1[qi][:], q32[qi][:], cosT[:, cs], ALU.mult, reads=[q32[qi], cosT], writes=[t1[qi]])
            P.tt("dve", t2[qi][:], pr[:], sinT[:, cs], ALU.mult, reads=[pr, sinT], writes=[t2[qi]])
            P.tt("dve", dst[:, cs], t1[qi][:], t2[qi][:], ALU.add, reads=[t1[qi], t2[qi]], writes=[dst])
        pv = pb[5]
        for ts_ in range(4):
            for k in range(KT):
                P.mm(pv[:, ts_ * 128:(ts_ + 1) * 128], hn[:, k, ts_ * 128:(ts_ + 1) * 128], wb[:, k, 256:384],
                     start=(k == 0), stop=(k == KT - 1), reads=[hn, wb], writes=[pv])
        P.act(V[:, c * 4:(c + 1) * 4, :], pv[:].rearrange("p (a b) -> p a b", a=4), AF.Copy, reads=[pv], writes=[V])


def simple_out(P, out_d):
    def f(buf, ap, c0, n):
        P.dma("sp", out_d[:, c0:c0 + n], ap, reads=[buf], writes=[out_d])
    return f


def masked_out(rsin_t, g, rmask_d):
    def mk(P, alloc):
        tmp = [alloc(0), alloc(1)]
        cnt = [0]
        rsin = Buf(rsin_t, "rsin")
        rmask = P.sb([128, 4], F32, "rmask")
        P.dma("sp", rmask[:], rmask_d[:], writes=[rmask])

        def f(buf, ap, c0, n):
            tq, tl = divmod(c0, 1024)
            for r in range(4):
                t_ = tmp[cnt[0] % 2]; cnt[0] += 1
                P.ts("pool", t_[:, 0:n], ap, rmask[:, r:r + 1], None, ALU.mult, reads=[buf, rmask], writes=[t_])
                P.dma("sp", rsin[tq, g, r, :, tl:tl + n], t_[:, 0:n], reads=[t_], writes=[rsin])
        return f
    return mk


def emit_A(P, layer_idx, hT, g_mix, w_a, lamv_d, subln_d, cos_d, sin_d, rm_d, out_d, out_mk=None):
    out_fn = out_mk(P, lambda i: P.sb([128, 512], F32, f"mo{i}")) if out_mk else simple_out(P, out_d)
    lam_init = 0.8 - 0.6 * math.exp(-0.3 * layer_idx)
    pb = [P.ps([128, 512], F32, f"bank{i}") for i in range(8)]
    nctx = NormCtx(P, g_mix)
    wb = load_w_bf16(P, w_a, 384, "wA", stg_buf=nctx.x32[1])
    cosT = P.sb([128, S], F32, "cosT"); sinT = P.sb([128, S], F32, "sinT")
    P.dma("sp", cosT[:], cos_d[:], writes=[cosT]); P.dma("pool", sinT[:], sin_d[:], writes=[sinT])
    rm = P.sb([128, 128], F32, "rm"); P.dma("sp", rm[:], rm_d[:], writes=[rm])
    QT = P.sb([128, S], BF16, "QT"); KTt = P.sb([128, S], BF16, "KTt")
    V = P.sb([128, S // 128, 128], BF16, "Vall")
    lamv = P.sb([128, 2, 2, 64], F32, "lamv")
    P.dma("sp", lamv[:].rearrange("p a b c -> p (a b c)"), bcast_rows(lamv_d, 256), writes=[lamv])
    lprod = P.sb([128, 2, 64], F32, "lprod"); lsum = P.sb([128, 2], F32, "lsum"); lexp = P.sb([128, 2], F32, "lexp")
    nlam = P.sb([128, 1], F32, "nlam")
    P.tt("dve", lprod[:], lamv[:, :, 0, :], lamv[:, :, 1, :], ALU.mult, reads=[lamv], writes=[lprod])
    P.red(lsum[:], lprod[:], ALU.add, reads=[lprod], writes=[lsum])
    P.act(lexp[:], lsum[:], AF.Exp, reads=[lsum], writes=[lexp])
    P.tt("dve", nlam[:], lexp[:, 1:2], lexp[:, 0:1], ALU.subtract, reads=[lexp], writes=[nlam])
    P.ts("dve", nlam[:], nlam[:], -lam_init, None, ALU.add, reads=[nlam], writes=[nlam])
    gsub = P.sb([128, 1], F32, "gsub")
    P.dma("sp", gsub[:], subln_d[:].rearrange("(p o) -> p o", o=1), writes=[gsub])
    P.ts("dve", gsub[:], gsub[:], 1.0 - lam_init, None, ALU.mult, reads=[gsub], writes=[gsub])

    proj_qkv(P, nctx, hT, wb, pb, cosT, sinT, rm, QT, KTt, V)

    NKT = S // 128
    par = nctx.x32[0]
    def cv32(k):
        return P.carve(par, par.t[:, k, :], f"cv{k}")
    def cv16(k, h):
        return P.carve(par, par.t[:, k, h * 256:(h + 1) * 256].bitcast(BF16), f"cvb{k}_{h}")
    E = [[cv16(c * 2 + i // 2, i % 2) if i < 2 else cv16(4, c) for i in range(3)] for c in range(2)]
    R = [cv32(5 + c) for c in range(2)]
    tt0 = cv32(7); tt1 = cv32(8)
    o32 = cv32(9); osq = cv16(10, 0)
    sd2 = cv32(11); rs2 = cv32(12)
    oout = [cv32(13 + i) for i in range(2)]

    def s_mm(qc, kt):
        for c in range(2):
            Sb = pb[4 + 2 * c + (kt % 2)]
            P.mm(Sb[:], KTt[64 * c:64 * c + 64, kt * 128:(kt + 1) * 128], QT[64 * c:64 * c + 64, qc * CH:(qc + 1) * CH],
                 reads=[KTt, QT], writes=[Sb])

    for qc in range(NCH):
        s_mm(qc, 0)
        for kt in range(NKT):
            if kt + 1 < NKT:
                s_mm(qc, kt + 1)
            for c in range(2):
                Sb = pb[4 + 2 * c + (kt % 2)]
                e = E[c][kt % 3]
                P.act(e[:], Sb[:], AF.Exp, reads=[Sb], writes=[e], scale=0.125)
                P.mm(pb[c][:], V[:, kt, :], e[:], start=(kt == 0), stop=(kt == NKT - 1), reads=[V, e], writes=[pb[c]])
                P.mm(pb[2 + c][:], nctx.ones_bf[:], e[:], start=(kt == 0), stop=(kt == NKT - 1),
                     reads=[nctx.ones_bf, e], writes=[pb[2 + c]])
        for c in range(2):
            P.recip(R[c][:], pb[2 + c][:], reads=[pb[2 + c]], writes=[R[c]])
        P.tt("dve", tt0[:], pb[0][:], R[0][:], ALU.mult, reads=[pb[0], R[0]], writes=[tt0])
        P.tt("dve", tt1[:], pb[1][:], R[1][:], ALU.mult, reads=[pb[1], R[1]], writes=[tt1])
        P.stt(o32[:], tt1[:], nlam[:, 0:1], tt0[:], ALU.mult, ALU.add, reads=[tt1, nlam, tt0], writes=[o32])
        P.act(osq[:], o32[:], AF.Square, reads=[o32], writes=[osq])
        pss = pb[4]
        P.mm(pss[:], nctx.ones_bf[:], osq[:], reads=[nctx.ones_bf, osq], writes=[pss])
        P.act(sd2[:], pss[:], AF.Sqrt, reads=[pss, nctx.eps], writes=[sd2], scale=1.0 / 128, bias=nctx.eps[:])
        P.recip(rs2[:], sd2[:], reads=[sd2], writes=[rs2])
        oo = oout[qc % 2]
        P.stt(oo[:], o32[:], gsub[:, 0:1], rs2[:], ALU.mult, ALU.mult, reads=[o32, gsub, rs2], writes=[oo])
        out_fn(oo, oo[:], qc * CH, CH)


def rope_tables_np(seq, dim, theta=10000.0):
    inv = (1.0 / (np.float32(theta) ** (np.arange(0, dim, 2, dtype=np.float32) / np.float32(dim)))).astype(np.float32)
    ang = np.arange(seq, dtype=np.float32)[:, None] * inv[None, :]
    return np.cos(ang).astype(np.float32), np.sin(ang).astype(np.float32)


def consts_A():
    cos, sin = rope_tables_np(S, 64)
    cosT = np.tile(cos.T, (4, 1)).astype(np.float32)
    sinT = np.tile(sin.T, (4, 1)).astype(np.float32)
    rm = np.zeros((128, 128), np.float32)
    for blk in range(2):
        for dp in range(64):
            if dp < 32:
                rm[blk * 64 + dp + 32, blk * 64 + dp] = -1.0
            else:
                rm[blk * 64 + dp - 32, blk * 64 + dp] = 1.0
    return cosT, sinT, rm


def emit_C(P, hT, g_mix, w_c, cos_d, sin_d, rm_d, mask_d, out_d, out_mk=None):
    pb = [P.ps([128, 512], F32, f"bank{i}") for i in range(8)]
    nctx = NormCtx(P, g_mix)
    wb = load_w_bf16(P, w_c, 384, "wC", stg_buf=nctx.x32[1])
    mk = P.sb([128, 20, 512], BF16, "mk")
    for h in range(2):
        stg = nctx.x32[0]
        P.dma("sp", stg[:, 0:10, :], mask_d[:, h * 10:(h + 1) * 10, :], writes=[stg])
        P.cp("pool", mk[:, h * 10:(h + 1) * 10, :], stg[:, 0:10, :], reads=[stg], writes=[mk])
    cosT = P.sb([128, S], F32, "cosT"); sinT = P.sb([128, S], F32, "sinT")
    P.dma("sp", cosT[:], cos_d[:], writes=[cosT]); P.dma("pool", sinT[:], sin_d[:], writes=[sinT])
    rm = P.sb([128, 128], F32, "rm"); P.dma("sp", rm[:], rm_d[:], writes=[rm])
    QT = P.sb([128, S], BF16, "QT"); KTt = P.sb([128, S], BF16, "KTt")
    V = P.sb([128, S // 128, 128], BF16, "Vall")
    proj_qkv(P, nctx, hT, wb, pb, cosT, sinT, rm, QT, KTt, V)

    par = nctx.x32[1]
    def cv32(k):
        return P.carve(par, par.t[:, k, :], f"cv{k}")
    def cv16(k, h):
        return P.carve(par, par.t[:, k, h * 256:(h + 1) * 256].bitcast(BF16), f"cvb{k}_{h}")
    E = [cv16(0, 0), cv16(0, 1), cv16(1, 0)]
    EM = [cv16(2, 0), cv16(2, 1), cv16(3, 0)]
    Rr = cv32(4)
    oout = [cv32(5), cv32(6)]
    out_fn = out_mk(P, lambda i: cv32(8 + i)) if out_mk else simple_out(P, out_d)
    scale = 128 ** -0.5
    for qc in range(NCH):
        kts = [kt for kt in range(4 * qc - 8, 4 * qc + 12) if 0 <= kt < S // 128]
        Sb = [pb[4], pb[5], pb[6]]
        O = pb[qc % 2]; Dn = pb[2 + qc % 2]

        def s_mm(i):
            kt = kts[i]
            P.mm(Sb[i % 3][:], KTt[:, kt * 128:(kt + 1) * 128], QT[:, qc * CH:(qc + 1) * CH],
                 reads=[KTt, QT], writes=[Sb[i % 3]])
        s_mm(0)
        for i, kt in enumerate(kts):
            if i + 1 < len(kts):
                s_mm(i + 1)
            kk = kt - 4 * qc + 8
            e = E[i % 3]; em = EM[i % 3]
            P.act(e[:], Sb[i % 3][:], AF.Exp, reads=[Sb[i % 3]], writes=[e], scale=scale)
            P.tt("dve", em[:], e[:], mk[:, kk, :], ALU.mult, reads=[e, mk], writes=[em])
            P.mm(O[:], V[:, kt, :], em[:], start=(i == 0), stop=(i == len(kts) - 1), reads=[V, em], writes=[O])
            P.mm(Dn[:], nctx.ones_bf[:], em[:], start=(i == 0), stop=(i == len(kts) - 1),
                 reads=[nctx.ones_bf, em], writes=[Dn])
        P.recip(Rr[:], Dn[:], reads=[Dn], writes=[Rr])
        oo = oout[qc % 2]
        P.tt("dve", oo[:], O[:], Rr[:], ALU.mult, reads=[O, Rr], writes=[oo])
        out_fn(oo, oo[:], qc * CH, CH)


def consts_C():
    cos, sin = rope_tables_np(S, 128)
    cosT = np.tile(cos.T, (2, 1)).astype(np.float32)
    sinT = np.tile(sin.T, (2, 1)).astype(np.float32)
    rm = np.zeros((128, 128), np.float32)
    for dp in range(128):
        if dp < 64:
            rm[dp + 64, dp] = -1.0
        else:
            rm[dp - 64, dp] = 1.0
    jl = np.arange(128)[:, None]; il = np.arange(512)[None, :]
    mask = np.zeros((128, 20, 512), np.float32)
    for kk in range(20):
        dlt = (kk - 8) * 128 + jl - il
        a = np.abs(dlt)
        mask[:, kk, :] = (a <= 64).astype(np.float32) + ((a <= 256) & (dlt % 4 == 0)) + ((a <= 1024) & (dlt % 16 == 0))
    return cosT, sinT, rm, mask


CHK = 512


def emit_D(P, hT, g_mix, w_d, dpar_d, dskip_d, bblk_d, cblk_d, out_d, out_mk=None):
    out_fn = out_mk(P, lambda i: P.sb([128, 512], F32, f"mo{i}")) if out_mk else simple_out(P, out_d)
    pb = [P.ps([128, 512], F32, f"bank{i}") for i in range(8)]
    nctx = NormCtx(P, g_mix)
    wb = load_w_bf16(P, w_d, 128, "wD", stg_buf=nctx.x32[1])
    U = P.sb([128, S], F32, "U"); Y = P.sb([128, S], F32, "Y")
    bblk = P.sb([128, 2, 4, 128], F32, "bblk"); cblk = P.sb([128, 2, 4, 128], F32, "cblk")
    P.dma("sp", bblk[:], bblk_d[:], writes=[bblk]); P.dma("pool", cblk[:], cblk_d[:], writes=[cblk])
    dpar = P.sb([128, 3, 8], F32, "dpar"); P.dma("sp", dpar[:], dpar_d[:], writes=[dpar])
    dskip = P.sb([128, 1], F32, "dskip"); P.dma("sp", dskip[:], dskip_d[:].rearrange("(p o) -> p o", o=1), writes=[dskip])
    nctx.load(hT, 0)
    for c in range(NCH):
        if c + 1 < NCH:
            nctx.load(hT, c + 1)
        hn = nctx.norm(c, pb[0])
        pu = pb[1 + c % 2]
        for k in range(KT):
            P.mm(pu[:], wb[:, k, :], hn[:, k, :], start=(k == 0), stop=(k == KT - 1), reads=[wb, hn], writes=[pu])
        P.act(U[:, c * CH:(c + 1) * CH], pu[:], AF.Copy, reads=[pu], writes=[U])

    def pt(name):
        return P.sb([128, 8], F32, name)
    lr = pt("lr"); dt = pt("dt"); mag = pt("mag"); th = pt("th"); cs = pt("cs"); sn = pt("sn")
    ta = pt("ta"); tb = pt("tb"); tc = pt("tc"); fr = pt("fr"); fi = pt("fi"); abr = pt("abr"); abi = pt("abi")
    hpi = P.sb([128, 1], F32, "hpi"); P.memset("dve", hpi[:], math.pi / 2, writes=[hpi])
    li = dpar[:, 1, :]
    P.ts("dve", lr[:], dpar[:, 0, :], -1e-4, None, ALU.min, reads=[dpar], writes=[lr])
    P.act(dt[:], dpar[:, 2, :], AF.Exp, reads=[dpar], writes=[dt])
    P.tt("dve", ta[:], dt[:], lr[:], ALU.mult, reads=[dt, lr], writes=[ta])
    P.act(mag[:], ta[:], AF.Exp, reads=[ta], writes=[mag])
    P.tt("dve", th[:], dt[:], li, ALU.mult, reads=[dt, dpar], writes=[th])
    P.act(sn[:], th[:], AF.Sin, reads=[th], writes=[sn], scale=1.0 / 32)
    P.act(cs[:], th[:], AF.Sin, reads=[th, hpi], writes=[cs], scale=1.0 / 32, bias=hpi[:])
    for _ in range(5):
        P.tt("dve", ta[:], cs[:], cs[:], ALU.mult, reads=[cs], writes=[ta])
        P.tt("dve", tb[:], sn[:], sn[:], ALU.mult, reads=[sn], writes=[tb])
        P.tt("dve", tc[:], cs[:], sn[:], ALU.mult, reads=[cs, sn], writes=[tc])
        P.tt("dve", cs[:], ta[:], tb[:], ALU.subtract, reads=[ta, tb], writes=[cs])
        P.ts("dve", sn[:], tc[:], 2.0, None, ALU.mult, reads=[tc], writes=[sn])
    P.tt("dve", abr[:], mag[:], cs[:], ALU.mult, reads=[mag, cs], writes=[abr])
    P.tt("dve", abi[:], mag[:], sn[:], ALU.mult, reads=[mag, sn], writes=[abi])
    P.tt("dve", ta[:], lr[:], lr[:], ALU.mult, reads=[lr], writes=[ta])
    P.tt("dve", tb[:], li, li, ALU.mult, reads=[dpar], writes=[tb])
    P.tt("dve", ta[:], ta[:], tb[:], ALU.add, reads=[ta, tb], writes=[ta])
    P.recip(tc[:], ta[:], reads=[ta], writes=[tc])
    P.ts("dve", abr[:], abr[:], -1.0, None, ALU.add, reads=[abr], writes=[abr])
    P.tt("dve", ta[:], abr[:], lr[:], ALU.mult, reads=[abr, lr], writes=[ta])
    P.tt("dve", tb[:], abi[:], li, ALU.mult, reads=[abi, dpar], writes=[tb])
    P.tt("dve", ta[:], ta[:], tb[:], ALU.add, reads=[ta, tb], writes=[ta])
    P.tt("dve", fr[:], ta[:], tc[:], ALU.mult, reads=[ta, tc], writes=[fr])
    P.tt("dve", ta[:], abi[:], lr[:], ALU.mult, reads=[abi, lr], writes=[ta])
    P.tt("dve", tb[:], abr[:], li, ALU.mult, reads=[abr, dpar], writes=[tb])
    P.tt("dve", ta[:], ta[:], tb[:], ALU.subtract, reads=[ta, tb], writes=[ta])
    P.tt("dve", fi[:], ta[:], tc[:], ALU.mult, reads=[ta, tc], writes=[fi])

    x0 = nctx.x32[0]; x1 = nctx.x32[1]
    Tc = P.carve(x0, x0.t[:, 0:8, :], "Tc"); Ts = P.carve(x0, x0.t[:, 8:16, :], "Ts")
    Tr = P.carve(x1, x1.t[:, 0:8, :], "Tinr"); Ti = P.carve(x1, x1.t[:, 8:16, :], "Tini")
    h0 = nctx.hn[0]; h1 = nctx.hn[1]
    G0 = P.carve(h0, h0.t[:].rearrange("p a b -> p (a b)").bitcast(F32), "G0")
    G1 = P.carve(h1, h1.t[:].rearrange("p a b -> p (a b)").bitcast(F32), "G1")
    g0v = G0.t.rearrange("p (a b) -> p a b", a=8); g1v = G1.t.rearrange("p (a b) -> p a b", a=8)

    def bc(buf, m):
        return bass.AP(buf.t, 0, [[8, 128], [1, 8], [0, m]])

    def bcT(T, idx, m):
        return bass.AP(T.t.tensor, T.t.offset + idx, [[T.t.ap[0][0], 128], [T.t.ap[1][0], 8], [0, m]])

    P.cp("dve", Tc[:, :, 0:1], cs[:].rearrange("p (a o) -> p a o", o=1), reads=[cs], writes=[Tc])
    P.cp("dve", Ts[:, :, 0:1], sn[:].rearrange("p (a o) -> p a o", o=1), reads=[sn], writes=[Ts])
    m = 1
    while m < CHK:
        ec = bcT(Tc, m - 1, m); es = bcT(Ts, m - 1, m)
        a = g0v[:, :, 0:m]; b = g1v[:, :, 0:m]
        P.tt("dve", a, Tc[:, :, 0:m], ec, ALU.mult, reads=[Tc], writes=[G0])
        P.tt("dve", b, Ts[:, :, 0:m], es, ALU.mult, reads=[Ts], writes=[G1])
        P.tt("dve", Tc[:, :, m:2 * m], a, b, ALU.subtract, reads=[G0, G1], writes=[Tc])
        P.tt("dve", a, Tc[:, :, 0:m], es, ALU.mult, reads=[Tc, Ts], writes=[G0])
        P.tt("dve", b, Ts[:, :, 0:m], ec, ALU.mult, reads=[Ts, Tc], writes=[G1])
        P.tt("dve", Ts[:, :, m:2 * m], a, b, ALU.add, reads=[G0, G1], writes=[Ts])
        m *= 2
    P.tt("dve", g0v, Tc[:], bc(fr, CHK), ALU.mult, reads=[Tc, fr], writes=[G0])
    P.tt("dve", g1v, Ts[:], bc(fi, CHK), ALU.mult, reads=[Ts, fi], writes=[G1])
    P.tt("dve", Tr[:], g0v, g1v, ALU.add, reads=[G0, G1], writes=[Tr])
    P.tt("dve", g0v, Tc[:], bc(fi, CHK), ALU.mult, reads=[Tc, fi], writes=[G0])
    P.tt("dve", g1v, Ts[:], bc(fr, CHK), ALU.mult, reads=[Ts, fr], writes=[G1])
    P.tt("dve", Ti[:], g0v, g1v, ALU.subtract, reads=[G0, G1], writes=[Ti])

    def wb_(name, n=2):
        return [P.sb([128, CHK], F32, f"{name}{i}") for i in range(n)]
    m1 = wb_("m1"); m2 = wb_("m2"); wr = wb_("wr"); wi = wb_("wi"); xh_r = wb_("xhr"); xh_i = wb_("xhi")
    xr = wb_("xr"); nxi = wb_("nxi")
    carry = [[P.sb([128, 1], F32, f"car{d}_{s}_{ri}") for ri in range(2)] for d in range(2) for s in range(4)]
    for cc in carry:
        for t_ in cc:
            P.memset("pool", t_[:], 0.0, writes=[t_])

    def rv(ap_full, base_buf_is_psum=False):
        return ap_full

    it = 0
    for d in range(2):
        order = range(NCH) if d == 0 else range(NCH - 1, -1, -1)
        for c in order:
            t0 = c * CHK
            py = pb[6 + (it % 2)]
            for s in range(4):
                col = d * 4 + s
                pr_ = pb[(it * 4 + s) % 3 * 2]; pi_ = pb[(it * 4 + s) % 3 * 2 + 1]
                P.mm(pr_[:], bblk[:, 0, s, :], U[:, t0:t0 + CHK], reads=[bblk, U], writes=[pr_])
                P.mm(pi_[:], bblk[:, 1, s, :], U[:, t0:t0 + CHK], reads=[bblk, U], writes=[pi_])
                i2 = (it * 4 + s) % 2
                if d == 0:
                    rvs = lambda ap: ap
                else:
                    rvs = lambda ap: ap[:, ::-1]
                bur = pr_[:]; bui = pi_[:]
                P.tt("dve", rvs(m1[i2][:]), bur, rvs(Tr[:, col, :]), ALU.mult, reads=[pr_, Tr], writes=[m1[i2]])
                P.tt("dve", rvs(m2[i2][:]), bui, rvs(Ti[:, col, :]), ALU.mult, reads=[pi_, Ti], writes=[m2[i2]])
                P.tt("dve", wr[i2][:], m1[i2][:], m2[i2][:], ALU.subtract, reads=[m1[i2], m2[i2]], writes=[wr[i2]])
                P.tt("dve", rvs(m1[i2][:]), bur, rvs(Ti[:, col, :]), ALU.mult, reads=[pr_, Ti], writes=[m1[i2]])
                P.tt("dve", rvs(m2[i2][:]), bui, rvs(Tr[:, col, :]), ALU.mult, reads=[pi_, Tr], writes=[m2[i2]])
                P.tt("dve", wi[i2][:], m1[i2][:], m2[i2][:], ALU.add, reads=[m1[i2], m2[i2]], writes=[wi[i2]])
                magb = bass.AP(mag.t, col, [[8, 128], [0, CHK]])
                cr, ci = carry[d * 4 + s]
                P.scan(xh_r[i2][:], magb, wr[i2][:], cr[:, 0:1], ALU.mult, ALU.add, reads=[mag, wr[i2], cr], writes=[xh_r[i2]])
                P.scan(xh_i[i2][:], magb, wi[i2][:], ci[:, 0:1], ALU.mult, ALU.add, reads=[mag, wi[i2], ci], writes=[xh_i[i2]])
                P.tt("dve", m1[i2][:], xh_r[i2][:], Tc[:, col, :], ALU.mult, reads=[xh_r[i2], Tc], writes=[m1[i2]])
                P.tt("dve", m2[i2][:], xh_i[i2][:], Ts[:, col, :], ALU.mult, reads=[xh_i[i2], Ts], writes=[m2[i2]])
                P.tt("dve", xr[i2][:], m1[i2][:], m2[i2][:], ALU.subtract, reads=[m1[i2], m2[i2]], writes=[xr[i2]])
                P.tt("dve", m1[i2][:], xh_r[i2][:], Ts[:, col, :], ALU.mult, reads=[xh_r[i2], Ts], writes=[m1[i2]])
                P.tt("dve", m2[i2][:], xh_i[i2][:], Tc[:, col, :], ALU.mult, reads=[xh_i[i2], Tc], writes=[m2[i2]])
                P.stt(nxi[i2][:], m1[i2][:], -1.0, m2[i2][:], ALU.mult, ALU.subtract, reads=[m1[i2], m2[i2]], writes=[nxi[i2]])
                P.cp("dve", cr[:], xr[i2][:, CHK - 1:CHK], reads=[xr[i2]], writes=[cr])
                P.ts("dve", ci[:], nxi[i2][:, CHK - 1:CHK], -1.0, None, ALU.mult, reads=[nxi[i2]], writes=[ci])
                P.mm(py[:], cblk[:, 0, s, :], xr[i2][:], start=(s == 0), stop=False, reads=[cblk, xr[i2]], writes=[py])
                P.mm(py[:], cblk[:, 1, s, :], nxi[i2][:], start=False, stop=(s == 3), reads=[cblk, nxi[i2]], writes=[py])
            if d == 0:
                P.stt(Y[:, t0:t0 + CHK], U[:, t0:t0 + CHK], dskip[:, 0:1], py[:], ALU.mult, ALU.add,
                      reads=[U, dskip, py], writes=[Y])
            else:
                P.tt("dve", Y[:, t0:t0 + CHK][:, ::-1], Y[:, t0:t0 + CHK][:, ::-1], py[:], ALU.add, reads=[Y, py], writes=[Y])
            it += 1
    P.act(G0[:], Y[:], AF.Square, reads=[Y], writes=[G0])
    P.ts("dve", G0[:], G0[:], 0.044715, 1.0, ALU.mult, ALU.add, reads=[G0], writes=[G0])
    P.tt("dve", G0[:], G0[:], Y[:], ALU.mult, reads=[G0, Y], writes=[G0])
    P.act(G1[:], G0[:], AF.Sigmoid, reads=[G0], writes=[G1], scale=2.0 * math.sqrt(2.0 / math.pi))
    P.tt("dve", G0[:], G1[:], Y[:], ALU.mult, reads=[G1, Y], writes=[G0])
    for c in range(NCH):
        out_fn(G0, G0[:, c * CH:(c + 1) * CH], c * CH, CH)


def host_D(d, L, j):
    G0 = 8 * j
    a_re = d["s5_a_re"][L]; a_im = d["s5_a_im"][L]; ldt = d["s5_log_dt"][L]
    dpar = np.zeros((128, 3, 8), np.float32)
    for dr in range(2):
        for s in range(4):
            for gh in range(2):
                g = G0 + 2 * s + gh
                dpar[gh * 64:(gh + 1) * 64, 0, dr * 4 + s] = a_re[dr, g]
                dpar[gh * 64:(gh + 1) * 64, 1, dr * 4 + s] = a_im[dr, g]
                dpar[gh * 64:(gh + 1) * 64, 2, dr * 4 + s] = ldt[dr, g]
    bblk = np.zeros((128, 2, 4, 128), np.float32); cblk = np.zeros((128, 2, 4, 128), np.float32)
    for ri, (bn, cn) in enumerate((("s5_b_re", "s5_c_re"), ("s5_b_im", "s5_c_im"))):
        B = d[bn][L]; C = d[cn][L]
        for s in range(4):
            for gh in range(2):
                gl = 2 * s + gh
                bblk[gl * 16:(gl + 1) * 16, ri, s, gh * 64:(gh + 1) * 64] = B[G0 + gl].T
                cblk[gh * 64:(gh + 1) * 64, ri, s, gl * 16:(gl + 1) * 16] = C[G0 + gl].T
    return dpar, bblk, cblk, np.ascontiguousarray(d["s5_d"][L][128 * j:128 * (j + 1)])


LC = 64
SCH = 8
LN_X_EPS = 64e-5
NV = 21


def carve_multi(P, parents, ap, name=""):
    b = Buf(ap, name)
    b.last_w = None
    rd = []
    for p in parents:
        if p.last_w is not None:
            rd.append(p.last_w)
        rd.extend(p.readers)
    b.readers = rd
    return b


def emit_B(P, hT, g_mix, w_b, bvec_d, w2_d, a2_d, g2_d, ident_d, bones_d, out_d, tag="", out_mk=None, scr=None):
    CH1 = 256
    pb = [P.ps([128, 512], F32, f"bank{i}") for i in range(8)]
    nctx = NormCtx(P, g_mix, ch=CH1)
    zscr = P.dram("zscr" + tag, [6, 128, S + 2], F32)
    ROWS = P.dram("rows" + tag, [2, 2, S, 4, 64], F32)
    PEND = P.dram("pend" + tag, [2, 64, 2, 64], F32)
    bvec = P.sb([128, NV], F32, "bvec"); P.dma("sp", bvec[:], bvec_d[:], writes=[bvec])
    w2 = P.sb([128, 128], F32, "w2"); P.dma("sp", w2[:], w2_d[:], writes=[w2])
    a2 = P.sb([128, 128], F32, "a2"); P.dma("sp", a2[:], a2_d[:], writes=[a2])
    g2 = P.sb([128, 128], F32, "g2"); P.dma("sp", g2[:], g2_d[:], writes=[g2])
    ident = P.sb([128, 128], F32, "ident"); P.dma("pool", ident[:], ident_d[:], writes=[ident])
    bones = P.sb([128, 128], F32, "bones"); P.dma("pool", bones[:], bones_d[:], writes=[bones])
    dv = P.sb([128, 12], F32, "dv")
    P.tt("dve", dv[:, 0:6], bvec[:, 0:6], bvec[:, 6:12], ALU.add, reads=[bvec], writes=[dv])
    P.ts("dve", dv[:, 0:6], dv[:, 0:6], -1.0, 1.0, ALU.mult, ALU.add, reads=[dv], writes=[dv])
    P.ts("dve", dv[:, 6:7], bvec[:, 17:18], -1.0, 1.0, ALU.mult, ALU.add, reads=[bvec], writes=[dv])
    P.ts("dve", dv[:, 7:8], bvec[:, 18:19], 0.5, None, ALU.mult, reads=[bvec], writes=[dv])
    P.memset("dve", dv[:, 8:9], 1e-12, writes=[dv])
    P.memset("dve", dv[:, 9:10], LN_X_EPS, writes=[dv])
    mask = P.sb([128, 512], F32, "cmask")
    P.memset("dve", mask[:], 1.0, writes=[mask])
    P.memset("dve", mask[:].rearrange("p (a b) -> p a b", b=LC)[:, :, 0:1], 0.0, writes=[mask])
    zero = P.sb([128, 6, 1], F32, "zero6"); P.memset("dve", zero[:], 0.0, writes=[zero])
    P.dma("sp", zscr[:, :, 0:1].rearrange("k p o -> p k o"), zero[:], reads=[zero], writes=[zscr], allow_slow_non_contiguous=True)
    P.dma("sp", zscr[:, :, S + 1:S + 2].rearrange("k p o -> p k o"), zero[:], reads=[zero], writes=[zscr], allow_slow_non_contiguous=True)

    wB = P.sb([128, KT, 768], BF16, "wB")
    srcw = w_b[:].rearrange("(kt p) n -> p kt n", p=128)
    for hh_ in range(4):
        stg = nctx.x32[hh_ % 2]
        P.dma("sp" if hh_ % 2 == 0 else "pool", stg[:, :, 0:192], srcw[:, :, hh_ * 192:(hh_ + 1) * 192], writes=[stg])
        P.cp("pool", wB[:, :, hh_ * 192:(hh_ + 1) * 192], stg[:, :, 0:192], reads=[stg], writes=[wB])
    zst = [P.sb([128, 3, CH1], F32, f"zst{i}") for i in range(2)]
    n1 = S // CH1
    nctx.load(hT, 0)
    for c in range(n1):
        if c + 1 < n1:
            nctx.load(hT, c + 1)
        hn = nctx.norm(c, pb[0])
        for half in range(2):
            zs_ = zst[half]
            for kk_ in range(3):
                kind = half * 3 + kk_
                pz = pb[1 + kind % 3]
                for k in range(KT):
                    P.mm(pz[:, 0:CH1], wB[:, k, kind * 128:(kind + 1) * 128], hn[:, k, :], start=(k == 0), stop=(k == KT - 1),
                         reads=[wB, hn], writes=[pz])
                P.act(zs_[:, kk_, :], pz[:, 0:CH1], AF.Copy, reads=[pz], writes=[zs_])
            P.dma("sp", zscr[half * 3:(half + 1) * 3, :, 1 + c * CH1:1 + (c + 1) * CH1].rearrange("k p t -> p k t"),
                  zs_[:], reads=[zs_], writes=[zscr])

    V = P.sb([128, S], F32, "Vres")
    BG = P.dram("bgscr" + tag, [128, 2, S], F32)
    bgs = [P.sb([128, 2, 512], F32, f"bgs{i}") for i in range(2)]
    Yout = P.sb([128, 2, S], F32, "Yout")

    big = P.sb([128, 23, 512], F32, "bigtmp")
    slot_i = [0]
    slots = []

    def T(name, shape=(128, 512)):
        i = slot_i[0]; slot_i[0] += 1
        b = Buf(big.t[:, i, :], name)
        slots.append(b)
        return b
    zc = P.sb([128, 6, 514], F32, "zc")
    zr = T("zr"); zk = T("zk"); zwd = T("zwd"); zad = T("zad"); zgd = T("zgd")
    lw = [T("lw0"), T("lw1")]; aa = [T("aa0"), T("aa1")]; km = [T("km0"), T("km1")]
    kkt = T("kkt"); tA = T("tA"); tB = T("tB"); cum = T("cum"); cumx = T("cumx")
    Ex = T("Ex"); Ei = T("Ei"); En = T("En")
    kinds4 = [T("k4_0"), T("k4_1"), T("k4_2"), T("k4_3")]
    rows_sb = [P.sb([128, 2, 4, 64], F32, f"rows_sb{i}") for i in range(2)]
    pe8 = P.sb([128, 8], F32, "pe8"); pe8T = P.sb([8, 128], F32, "pe8T")
    NE05 = -math.exp(-0.5)
    n2 = S // 512
    rot = [0]

    def pbank():
        rot[0] += 1
        return pb[rot[0] % 6]

    for c in range(n2):
        t0 = c * 512
        P.dma("sp", zc[:], zscr[:, :, t0:t0 + 514].rearrange("k p t -> p k t"), reads=[zscr], writes=[zc])
        dsts = [zr, zk, None, zwd, zad, zgd]
        for kind in range(6):
            dst = dsts[kind][:] if dsts[kind] is not None else V[:, t0:t0 + 512]
            dbuf = dsts[kind] if dsts[kind] is not None else V
            P.ts("dve", dst, zc[:, kind, 1:513], dv[:, kind:kind + 1], None, ALU.mult, reads=[zc, dv], writes=[dbuf])
            P.stt(dst, zc[:, kind, 0:512], bvec[:, kind:kind + 1], dst, ALU.mult, ALU.add, reads=[zc, bvec, dbuf], writes=[dbuf])
            P.stt(dst, zc[:, kind, 2:514], bvec[:, 6 + kind:7 + kind], dst, ALU.mult, ALU.add, reads=[zc, bvec, dbuf], writes=[dbuf])
        vch = V[:, t0:t0 + 512]
        P.act(zwd[:], zwd[:], AF.Tanh, reads=[zwd], writes=[zwd])
        for d in range(2):
            pw = pbank()
            P.mm(pw[:], w2[64 * d:64 * d + 64, :], zwd[64 * d:64 * d + 64, :], reads=[w2, zwd], writes=[pw])
            P.act(lw[d][:], pw[:], AF.Sigmoid, reads=[pw, bvec], writes=[lw[d]], bias=bvec[:, 12 + d:13 + d])
            P.ts("dve", lw[d][:], lw[d][:], NE05, None, ALU.mult, reads=[lw[d]], writes=[lw[d]])
            pa = pbank()
            P.mm(pa[:], a2[64 * d:64 * d + 64, :], zad[64 * d:64 * d + 64, :], reads=[a2, zad], writes=[pa])
            P.act(aa[d][:], pa[:], AF.Sigmoid, reads=[pa, bvec], writes=[aa[d]], bias=bvec[:, 14 + d:15 + d])
        P.act(zgd[:], zgd[:], AF.Sigmoid, reads=[zgd], writes=[zgd])
        pg = pbank()
        P.mm(pg[:], g2[:], zgd[:], reads=[g2, zgd], writes=[pg])
        bg_ = bgs[c % 2]
        P.act(bg_[:, 1, :], pg[:], AF.Copy, reads=[pg], writes=[bg_])
        P.ts("dve", kkt[:], zk[:], bvec[:, 16:17], None, ALU.mult, reads=[zk, bvec], writes=[kkt])
        P.tt("dve", tA[:], kkt[:], kkt[:], ALU.mult, reads=[kkt], writes=[tA])
        pk = pbank()
        P.mm(pk[:], bones[:], tA[:], reads=[bones, tA], writes=[pk])
        P.act(tB[:], pk[:], AF.Sqrt, reads=[pk, dv], writes=[tB], bias=dv[:, 8:9])
        P.recip(tB[:], tB[:], reads=[tB], writes=[tB])
        P.tt("dve", kkt[:], kkt[:], tB[:], ALU.mult, reads=[kkt, tB], writes=[kkt])
        for d in range(2):
            P.ts("dve", km[d][:], aa[d][:], bvec[:, 17:18], dv[:, 6:7], ALU.mult, ALU.add, reads=[aa[d], bvec, dv], writes=[km[d]])
            P.tt("dve", km[d][:], km[d][:], zk[:], ALU.mult, reads=[km[d], zk], writes=[km[d]])
        P.tt("dve", tA[:], km[0][:], km[1][:], ALU.add, reads=[km[0], km[1]], writes=[tA])
        P.tt("dve", tA[:], tA[:], zr[:], ALU.mult, reads=[tA, zr], writes=[tA])
        P.ts("dve", tA[:], tA[:], dv[:, 7:8], None, ALU.mult, reads=[tA, dv], writes=[tA])
        pbn = pbank()
        P.mm(pbn[:], bones[:], tA[:], reads=[bones, tA], writes=[pbn])
        P.tt("dve", bg_[:, 0, :], pbn[:], vch, ALU.mult, reads=[pbn, V], writes=[bg_])
        P.dma("pool", BG[:, :, t0:t0 + 512], bg_[:], reads=[bg_], writes=[BG])
        for d in range(2):
            rv = (lambda ap: ap) if d == 0 else (lambda ap: ap[:, ::-1])
            P.scan(rv(cum[:]), mask[:], rv(lw[d][:]), 0.0, ALU.mult, ALU.add, reads=[mask, lw[d]], writes=[cum])
            P.tt("dve", cumx[:], cum[:], lw[d][:], ALU.subtract, reads=[cum, lw[d]], writes=[cumx])
            P.act(Ex[:], cumx[:], AF.Exp, reads=[cumx], writes=[Ex])
            P.act(Ei[:], cum[:], AF.Exp, reads=[cum], writes=[Ei])
            P.act(En[:], cum[:], AF.Exp, reads=[cum], writes=[En], scale=-1.0)
            at, rt, bt, kt_ = kinds4
            P.stt(at[:], kkt[:], -1.0, Ex[:], ALU.mult, ALU.mult, reads=[kkt, Ex], writes=[at])
            P.tt("dve", rt[:], zr[:], Ei[:], ALU.mult, reads=[zr, Ei], writes=[rt])
            P.tt("dve", tA[:], kkt[:], aa[d][:], ALU.mult, reads=[kkt, aa[d]], writes=[tA])
            P.tt("dve", bt[:], tA[:], En[:], ALU.mult, reads=[tA, En], writes=[bt])
            P.tt("dve", kt_[:], km[d][:], En[:], ALU.mult, reads=[km[d], En], writes=[kt_])
            for blk in range(4):
                ptr = pb[6 + blk % 2]
                for ki in range(4):
                    P.tr(ptr[:, ki * 128:(ki + 1) * 128], kinds4[ki][:, blk * 128:(blk + 1) * 128], ident[:],
                         reads=[kinds4[ki], ident], writes=[ptr])
                rs_ = rows_sb[blk % 2]
                P.act(rs_[:].rearrange("p h k j -> p k h j"), ptr[:].rearrange("p (k h j) -> p k h j", k=4, h=2),
                      AF.Copy, reads=[ptr], writes=[rs_])
                for hh in range(2):
                    P.dma("sp" if hh == 0 else "pool", ROWS[hh, d, t0 + blk * 128:t0 + (blk + 1) * 128, :, :], rs_[:, hh, :, :],
                          reads=[rs_], writes=[ROWS])
            col = LC - 1 if d == 0 else 0
            P.act(pe8[:], cum[:].rearrange("p (a b) -> p a b", b=LC)[:, :, col], AF.Exp, reads=[cum], writes=[pe8])
            ptp = pb[6]
            P.tr(ptp[0:8, 0:128], pe8[:], ident[:], reads=[pe8, ident], writes=[ptp])
            P.act(pe8T[:], ptp[0:8, 0:128], AF.Copy, reads=[ptp], writes=[pe8T])
            P.dma("sp", PEND[d, c * 8:(c + 1) * 8, :, :].rearrange("m h j -> m (h j)"), pe8T[:], reads=[pe8T], writes=[PEND])

    x0 = nctx.x32[0]; x1 = nctx.x32[1]
    BC = [[None, None], [None, None]]
    for i, par in enumerate((x0, x1)):
        flat = par.t[:].rearrange("p a b -> p (a b)")
        for d in range(2):
            v_ = flat[:, d * 2048:(d + 1) * 2048].rearrange("p (s k j) -> p s k j", s=SCH, k=4)
            BC[d][i] = carve_multi(P, [par], v_, f"BC{d}_{i}")
    PE_sb = [carve_multi(P, [zc], zc.t[:].rearrange("p a b -> p (a b)")[:, 0:2048].rearrange("p (d m j) -> p d m j", d=2, m=16), "pend_sb0"),
             carve_multi(P, slots[0:4], big.t[:, 0:4, :].rearrange("p a b -> p (a b)").rearrange("p (d m j) -> p d m j", d=2, m=16), "pend_sb1")]
    St = P.sb([128, 2, 64], F32, "state"); P.memset("dve", St[:], 0.0, writes=[St])
    tmp = P.sb([128, 2, 64], F32, "sc_tmp"); sa = P.sb([128, 2], F32, "sc_sa")
    NST = S // SCH

    def load_bc(q):
        for d in range(2):
            buf = BC[d][q % 2]
            tstart = q * SCH if d == 0 else S - (q + 1) * SCH
            for hh in range(2):
                src = bass.AP(ROWS.t, ((hh * 2 + d) * S + tstart) * 256, [[0, 64], [1, SCH * 256]])
                P.dma("sp" if d == 0 else "act", buf.t[64 * hh:64 * hh + 64].rearrange("p s k j -> p (s k j)"), src,
                      reads=[ROWS], writes=[buf])

    def load_pend(gq):
        buf = PE_sb[gq % 2]
        for d in range(2):
            m0 = 16 * gq if d == 0 else 48 - 16 * gq
            for hh in range(2):
                src = bass.AP(PEND.t, (d * 64 + m0) * 128 + hh * 64, [[0, 64], [128, 16], [1, 64]])
                P.dma("pool", buf[64 * hh:64 * hh + 64, d, :, :], src, reads=[PEND], writes=[buf])

    load_bc(0)
    load_pend(0)
    load_pend(1)
    tmpE = P.sb([128, 2, 64], F32, "sc_tmpE")

    def bufs(q):
        return BC[0][q % 2], BC[1][q % 2]

    def rowap(q, s_, kind):
        b0, b1 = bufs(q)
        a0_ = b0.t[:, s_, kind, :]
        a1_ = b1.t[:, SCH - 1 - s_, kind, :]
        return bass.AP(a0_.tensor, a0_.offset, [[a0_.ap[0][0], 128], [a1_.offset - a0_.offset, 2], [1, 64]])

    def opA(tau):
        q, s_ = divmod(tau, SCH)
        b0, b1 = bufs(q)
        P.tt("dve", tmp[:], St[:], rowap(q, s_, 0), ALU.mult, reads=[St, b0, b1], writes=[tmp])

    def renorm(tau):
        qc_ = tau // LC - 1
        pbuf = PE_sb[(qc_ // 16) % 2]
        m0_ = qc_ % 16
        m1_ = 15 - (qc_ % 16)
        base = pbuf.t[:, 0, m0_, :]
        off1 = pbuf.t[:, 1, m1_, :]
        pap = bass.AP(base.tensor, base.offset, [[base.ap[0][0], 128], [off1.offset - base.offset, 2], [1, 64]])
        P.tt("dve", St[:], St[:], pap, ALU.mult, reads=[St, pbuf], writes=[St])
        if tau % 1024 == 0 and tau + 1024 < S:
            load_pend(tau // 1024 + 1)

    opA(0)
    for q in range(NST):
        if q + 1 < NST:
            load_bc(q + 1)
        b0, b1 = bufs(q)
        for s_ in range(SCH):
            tau = q * SCH + s_
            l0 = s_; l1 = SCH - 1 - s_
            P.red(sa[:], tmp[:], ALU.add, reads=[tmp], writes=[sa])
            for d in range(2):
                bb = b0 if d == 0 else b1
                ld = l0 if d == 0 else l1
                P.stt(St[:, d, :], bb.t[:, ld, 2, :], sa[:, d:d + 1], St[:, d, :], ALU.mult, ALU.add,
                      reads=[bb, sa, St], writes=[St])
            for d in range(2):
                bb = b0 if d == 0 else b1
                ld = l0 if d == 0 else l1
                tcol = tau if d == 0 else S - 1 - tau
                P.stt(St[:, d, :], bb.t[:, ld, 3, :], V[:, tcol:tcol + 1], St[:, d, :], ALU.mult, ALU.add,
                      reads=[bb, V, St], writes=[St])
            P.tt("dve", tmpE[:], St[:], rowap(q, s_, 1), ALU.mult, reads=[St, b0, b1], writes=[tmpE])
            if tau + 1 < S:
                if (tau + 1) % LC == 0:
                    renorm(tau + 1)
                opA(tau + 1)
            P.red(Yout[:, :, tau], tmpE[:], ALU.add, reads=[tmpE], writes=[Yout])

    out_fn = out_mk(P, lambda i: carve_multi(P, [zst[i]], zst[i].t[:].rearrange("p a b -> p (a b)")[:, 0:512], f"mo{i}")) if out_mk else simple_out(P, out_d)
    h0 = nctx.hn[0]; h1 = nctx.hn[1]
    def hslot(par, i):
        return carve_multi(P, [par], par.t[:].rearrange("p a b -> p (a b)").bitcast(F32)[:, i * 512:(i + 1) * 512], f"hs{i}")
    ysum = hslot(h0, 0); yc = hslot(h0, 1); ysq = hslot(h0, 2); rs4 = hslot(h0, 3)
    oo4 = [hslot(h1, 0), hslot(h1, 1)]
    for c in range(n2):
        t0 = c * 512
        bg_ = bgs[c % 2]
        P.dma("pool", bg_[:], BG[:, :, t0:t0 + 512], reads=[BG], writes=[bg_])
        yb_rev = Yout[:, 1, S - t0 - 512:S - t0][:, ::-1]
        P.tt("dve", ysum[:], Yout[:, 0, t0:t0 + 512], yb_rev, ALU.add, reads=[Yout], writes=[ysum])
        pm = pb[c % 2]
        P.mm(pm[:], bones[:], ysum[:], reads=[bones, ysum], writes=[pm])
        P.stt(yc[:], pm[:], -1.0 / 64, ysum[:], ALU.mult, ALU.add, reads=[pm, ysum], writes=[yc])
        P.tt("dve", ysq[:], yc[:], yc[:], ALU.mult, reads=[yc], writes=[ysq])
        pv_ = pb[2 + c % 2]
        P.mm(pv_[:], bones[:], ysq[:], reads=[bones, ysq], writes=[pv_])
        P.act(rs4[:], pv_[:], AF.Sqrt, reads=[pv_, dv], writes=[rs4], scale=1.0 / 64, bias=dv[:, 9:10])
        P.recip(rs4[:], rs4[:], reads=[rs4], writes=[rs4])
        P.tt("dve", yc[:], yc[:], rs4[:], ALU.mult, reads=[yc, rs4], writes=[yc])
        P.ts("dve", yc[:], yc[:], bvec[:, 19:20], bvec[:, 20:21], ALU.mult, ALU.add, reads=[yc, bvec], writes=[yc])
        P.tt("dve", yc[:], yc[:], bg_[:, 0, :], ALU.add, reads=[yc, bg_], writes=[yc])
        o_ = oo4[c % 2]
        P.tt("dve", o_[:], yc[:], bg_[:, 1, :], ALU.mult, reads=[yc, bg_], writes=[o_])
        out_fn(o_, o_[:], t0, 512)


def host_B(d, L, j):
    cs = slice(128 * j, 128 * (j + 1))
    mu = d["rwkv_mu"][L]
    colsets = [np.arange(128 * j, 128 * j + 128), 512 + np.arange(128 * j, 128 * j + 128),
               1024 + np.arange(128 * j, 128 * j + 128), 1536 + np.arange(128), 1664 + np.arange(128), 1792 + np.arange(128)]
    bvec = np.zeros((128, NV), np.float32)
    for k, cols in enumerate(colsets):
        bvec[:, k] = mu[0, cols]; bvec[:, 6 + k] = mu[1, cols]
    bvec[:, 12] = d["rwkv_w0"][L][0, cs]; bvec[:, 13] = d["rwkv_w0"][L][1, cs]
    bvec[:, 14] = d["rwkv_a0"][L][0, cs]; bvec[:, 15] = d["rwkv_a0"][L][1, cs]
    bvec[:, 16] = d["rwkv_kk"][L][cs]; bvec[:, 17] = d["rwkv_ka"][L][cs]
    bvec[:, 18] = d["rwkv_rk"][L].reshape(-1)[cs]
    bvec[:, 19] = d["rwkv_lnx_w"][L][cs]; bvec[:, 20] = d["rwkv_lnx_b"][L][cs]
    w2 = np.ascontiguousarray(d["rwkv_w2"][L][:, :, cs].reshape(128, 128))
    a2 = np.ascontiguousarray(d["rwkv_a2"][L][:, :, cs].reshape(128, 128))
    g2 = np.ascontiguousarray(d["rwkv_g2"][L][:, cs])
    return colsets, bvec, w2, a2, g2


def consts_B():
    ident = np.eye(128, dtype=np.float32)
    bones = np.zeros((128, 128), np.float32)
    bones[:64, :64] = 1.0; bones[64:, 64:] = 1.0
    return ident, bones


TT = 1024
NE = 32


class WStream:
    def __init__(self, P, nstg=4, nbf=3):
        self.P = P
        self.stg = [P.sb([128, 2048], F32, f"ws_stg{i}") for i in range(nstg)]
        self.bf = [P.sb([128, 4096], BF16, f"ws_bf{i}") for i in range(nbf)]
        self.i = 0
        self.j = 0

    def fetch(self, src, shape, src_buf, q=None):
        P = self.P
        a, b = shape
        n = a * b
        bf = self.bf[self.i % len(self.bf)]
        self.i += 1
        bv = bf[:, 0:n].rearrange("p (a b) -> p a b", a=a)
        h = a // 2 if a >= 2 else a
        for (lo, hi) in ((0, h), (h, a)):
            if lo >= hi:
                continue
            st = self.stg[self.j % len(self.stg)]
            qq = q or ("sp", "act", "pool")[self.j % 3]
            self.j += 1
            sv = st[:, 0:(hi - lo) * b].rearrange("p (a b) -> p a b", a=hi - lo)
            P.dma(qq, sv, src[:, lo:hi, :], reads=[src_buf], writes=[st])
            P.cp("dve", bv[:, lo:hi, :], sv, reads=[st], writes=[bf])
        return bf, bv


class Scratch:
    def __init__(self, P, nfloats, name):
        self.P = P
        self.buf = P.sb([128, nfloats], F32, name)
        self.kids = []

    def take(self, off, n, dt=F32, name=""):
        v = self.buf.t[:, off:off + n]
        if dt == BF16:
            v = v.bitcast(BF16)
        b = Buf(v, name)
        rd = []
        for (o2, n2, k) in self.kids:
            if o2 < off + n and off < o2 + n2:
                if k.last_w is not None:
                    rd.append(k.last_w)
                rd.extend(k.readers)
        b.readers = rd
        self.kids.append((off, n, b))
        return b


def stream(ws, pieces):
    nxt = ws.fetch(*pieces[0])
    for i in range(len(pieces)):
        cur = nxt
        if i + 1 < len(pieces):
            nxt = ws.fetch(*pieces[i + 1])
        yield i, cur[0], cur[1]


def emit_P2(P, hT_d, mixT_d, memT_d, w_out_d, glu_w_d, pvec_d, nrm_d, wq_d, wkv_d, wo_d, router_d,
            w1_d, w3_d, w2_d, ident_d, out_d, final_norm=False, xwaits=(), pre=None):
    if pre is not None:
        pre(P)
    pb = [P.ps([128, 512], F32, f"bank{i}") for i in range(8)]
    ws = WStream(P)
    HT = P.sb([128, KT, TT], F32, "HT")
    XN = P.sb([128, KT, TT], BF16, "XN")
    ones_bf = P.sb([128, 128], BF16, "ones_bf"); P.memset("pool", ones_bf[:], 1.0, writes=[ones_bf])
    ones32 = P.sb([128, 128], F32, "ones32"); P.memset("pool", ones32[:], 1.0, writes=[ones32])
    ident = P.sb([128, 128], F32, "ident"); P.dma("sp", ident[:], ident_d[:], writes=[ident])
    eps = P.sb([128, 1], F32, "eps"); P.memset("pool", eps[:], RMS_EPS, writes=[eps])
    pvec = P.sb([128, 4, 3], F32, "pvec"); P.dma("sp", pvec[:], pvec_d[:], writes=[pvec])
    nrm = P.sb([128, KT, 4], F32, "nrm"); P.dma("sp", nrm[:], nrm_d[:], writes=[nrm])
    sq = [P.sb([128, 512], BF16, f"sq{i}") for i in range(2)]
    sd = P.sb([128, 512], F32, "sd"); rstd = P.sb([128, 512], F32, "rstd")
    hsrc = hT_d[:].rearrange("(kt p) t -> p kt t", p=128)
    for k4 in range(4):
        P.dma("sp" if k4 % 2 == 0 else "act", HT[:, k4 * 4:(k4 + 1) * 4, :], hsrc[:, k4 * 4:(k4 + 1) * 4, :], writes=[HT])

    def rms_rstd(tiles, nfeat, ps):
        for i, (b_, ap_) in enumerate(tiles):
            s_ = sq[i % 2]
            P.act(s_[:], ap_, AF.Square, reads=[b_], writes=[s_])
            P.mm(ps[:], ones_bf[:], s_[:], start=(i == 0), stop=(i == len(tiles) - 1), reads=[ones_bf, s_], writes=[ps])
        P.act(sd[:], ps[:], AF.Sqrt, reads=[ps, eps], writes=[sd], scale=1.0 / nfeat, bias=eps[:])
        P.recip(rstd[:], sd[:], reads=[sd], writes=[rstd])

    S1 = Scratch(P, 11520, "SC"); S2 = S1
    gluw_b = S1.take(0, 1024, BF16, "gluw"); gluw = Buf(gluw_b.t.rearrange("p (a b) -> p a b", a=4), "gluw"); gluw.readers = gluw_b.readers
    S1.kids[-1] = (0, 1024, gluw)
    gsrc = glu_w_d[:].rearrange("(kt p) n -> p kt n", p=128)
    _, gv = ws.fetch(gsrc, (4, 512), glu_w_d, q="act")
    P.cp("pool", gluw[:], gv, reads=[ws.bf[(ws.i - 1) % 3]], writes=[gluw])
    msrc = mixT_d[:].rearrange("(kt p) t -> p kt t", p=128)
    glb_b = S1.take(1024, 1024, BF16, "glb"); glb = Buf(glb_b.t.rearrange("p (a b) -> p a b", a=4), "glb"); S1.kids[-1] = (1024, 1024, glb)
    od0 = [S1.take(2048 + 512 * i, 512, F32, f"od0_{i}") for i in range(4)]
    t32 = [S1.take(4096 + 512 * i, 512, F32, f"t32_{i}") for i in range(2)]
    for ch in range(2):
        cs = slice(ch * 512, (ch + 1) * 512)
        sQ = [ws.stg[0], ws.stg[1], ws.stg[2]]
        vQ = [q_[:].rearrange("p (a b) -> p a b", a=4) for q_ in sQ]
        for g4 in range(2):
            P.dma("sp", vQ[g4], msrc[:, g4 * 4:(g4 + 1) * 4, cs], reads=[mixT_d], writes=[sQ[g4]])
            P.cp("pool", XN[:, g4 * 4:(g4 + 1) * 4, cs], vQ[g4], reads=[sQ[g4]], writes=[XN])
        sB = sQ[2]; vB = vQ[2]
        P.dma("act", vB, msrc[:, 8:12, cs], reads=[mixT_d], writes=[sB])
        rms_rstd([(sB, vB[:, i, :]) for i in range(4)], 512, pb[0])
        for i in range(4):
            P.stt(XN[:, 8 + i, cs], vB[:, i, :], pvec[:, i, 1:2], rstd[:], ALU.mult, ALU.mult,
                  reads=[sB, pvec, rstd], writes=[XN])
        sG = sQ[0]; vG = vQ[0]
        P.dma("sp", vG, msrc[:, 12:16, cs], reads=[mixT_d], writes=[sG])
        P.cp("pool", glb[:], vG, reads=[sG], writes=[glb])
        for n in range(4):
            pg = pb[1 + n % 2]
            for kk in range(4):
                P.mm(pg[:], gluw[:, kk, n * 128:(n + 1) * 128], glb[:, kk, :], start=(kk == 0), stop=(kk == 3),
                     reads=[gluw, glb], writes=[pg])
            tg = t32[n % 2]
            P.act(tg[:], pg[:], AF.Sigmoid, reads=[pg, pvec], writes=[tg], bias=pvec[:, n, 0:1])
            P.tt("dve", od0[n][:], vG[:, n, :], tg[:], ALU.mult, reads=[sG, tg], writes=[od0[n]])
        rms_rstd([(od0[n], od0[n][:]) for n in range(4)], 512, pb[3])
        for n in range(4):
            P.stt(XN[:, 12 + n, cs], od0[n][:], pvec[:, n, 2:3], rstd[:], ALU.mult, ALU.mult,
                  reads=[od0[n], pvec, rstd], writes=[XN])

    def linear_acc(w_d, X, nkt, row0=0):
        wsrc = w_d[row0:row0 + nkt * 128, :].rearrange("(kt p) n -> p kt n", p=128)
        cw = 4096 // nkt
        pieces = [(wsrc[:, :, i * cw:(i + 1) * cw], (nkt, cw), w_d) for i in range(2048 // cw)]
        it = 0
        for pi, wb_, wv in stream(ws, pieces):
            for dl in range(cw // 128):
                dt = pi * (cw // 128) + dl
                for ch in range(2):
                    ps = pb[it % 4]; it += 1
                    for k in range(nkt):
                        P.mm(ps[:], wv[:, k, dl * 128:(dl + 1) * 128], X[:, k, ch * 512:(ch + 1) * 512],
                             start=(k == 0), stop=(k == nkt - 1), reads=[wb_, X], writes=[ps])
                    P.tt("dve", HT[:, dt, ch * 512:(ch + 1) * 512], HT[:, dt, ch * 512:(ch + 1) * 512], ps[:], ALU.add,
                         reads=[HT, ps], writes=[HT])

    def norm_to_XN(gcol):
        for ch in range(2):
            cs = slice(ch * 512, (ch + 1) * 512)
            rms_rstd([(HT, HT[:, k, cs]) for k in range(KT)], D, pb[4 + ch])
            for k in range(KT):
                P.stt(XN[:, k, cs], HT[:, k, cs], nrm[:, k, gcol:gcol + 1], rstd[:], ALU.mult, ALU.mult,
                      reads=[HT, nrm, rstd], writes=[XN])

    linear_acc(w_out_d, XN, KT)

    norm_to_XN(0)
    def shaped(b, fmt, **kw):
        nb = Buf(b.t.rearrange(fmt, **kw), b.name); nb.readers = b.readers; nb.last_w = b.last_w
        return nb
    sc1 = S1; sc2 = S2
    mnb = sc1.take(0, 2048, BF16, "MN"); MN = shaped(mnb, "p (a b) -> p a b", a=KT); sc1.kids[-1] = (0, 2048, MN)
    kxb = sc1.take(2048, 2048, BF16, "KX"); KX = shaped(kxb, "p (a b) -> p a b", a=KT); sc1.kids[-1] = (2048, 2048, KX)
    vxb = sc2.take(4096, 2048, BF16, "VX"); VX = shaped(vxb, "p (a b) -> p a b", a=2); sc2.kids[-1] = (4096, 2048, VX)
    qhb = sc1.take(6144, 2048, BF16, "QH"); QH = shaped(qhb, "p (a b) -> p a b", a=4); sc1.kids[-1] = (6144, 2048, QH)
    ohb = sc1.take(8192, 2048, BF16, "OH"); OH = shaped(ohb, "p (a b) -> p a b", a=4); sc1.kids[-1] = (8192, 2048, OH)
    sMs = [ws.stg[0], ws.stg[1]]
    vMs = [q_[:].rearrange("p (a b) -> p a b", a=8) for q_ in sMs]
    msrc2 = memT_d[:].rearrange("(kt p) m -> p kt m", p=128)
    for g8 in range(2):
        P.dma("sp", vMs[g8], msrc2[:, g8 * 8:(g8 + 1) * 8, :], writes=[sMs[g8]])
    for i in range(KT):
        s_ = sq[i % 2]
        P.act(s_[:, 0:256], vMs[i // 8][:, i % 8, :], AF.Square, reads=[sMs[i // 8]], writes=[s_])
        P.mm(pb[0][:, 0:256], ones_bf[:], s_[:, 0:256], start=(i == 0), stop=(i == KT - 1), reads=[ones_bf, s_], writes=[pb[0]])
    P.act(sd[:, 0:256], pb[0][:, 0:256], AF.Sqrt, reads=[pb[0], eps], writes=[sd], scale=1.0 / D, bias=eps[:])
    P.recip(rstd[:, 0:256], sd[:, 0:256], reads=[sd], writes=[rstd])
    for k in range(KT):
        P.stt(MN[:, k, :], vMs[k // 8][:, k % 8, :], nrm[:, k, 1:2], rstd[:, 0:256], ALU.mult, ALU.mult,
              reads=[sMs[k // 8], nrm, rstd], writes=[MN])
    ksrc = wkv_d[:].rearrange("(kt p) n -> p kt n", p=128)
    pieces = [(ksrc[:, :, i * 256:(i + 1) * 256], (KT, 256), wkv_d) for i in range(16)]
    for pi, wb_, wv in stream(ws, pieces):
        if pi < 8:
            for dl in range(2):
                ps = pb[(pi * 2 + dl) % 4]
                for k in range(KT):
                    P.mm(ps[:, 0:256], wv[:, k, dl * 128:(dl + 1) * 128], MN[:, k, :], start=(k == 0), stop=(k == KT - 1),
                         reads=[wb_, MN], writes=[ps])
                P.act(KX[:, pi * 2 + dl, :], ps[:, 0:256], AF.Copy, reads=[ps], writes=[KX])
        else:
            for mt in range(2):
                ps = pb[(pi * 2 + mt) % 4]
                for k in range(KT):
                    P.mm(ps[:, 0:256], MN[:, k, mt * 128:(mt + 1) * 128], wv[:, k, :], start=(k == 0), stop=(k == KT - 1),
                         reads=[wb_, MN], writes=[ps])
                P.act(VX[:, mt, (pi - 8) * 256:(pi - 7) * 256], ps[:, 0:256], AF.Copy, reads=[ps], writes=[VX])
    E = [sc2.take(10240 + 256 * i, 256, BF16, f"E{i}") for i in range(3)]
    rden = sc2.take(11008, 512, F32, "rden")
    qsrc = wq_d[:].rearrange("(kt p) n -> p kt n", p=128)
    xscale = 512 ** -0.5
    for h in range(4):
        pieces = [(qsrc[:, :, h * 512 + i * 256:h * 512 + (i + 1) * 256], (KT, 256), wq_d) for i in range(2)]
        for pi, wb_, wv in stream(ws, pieces):
            for dl in range(2):
                for ch in range(2):
                    ps = pb[(dl * 2 + ch) % 4]
                    for k in range(KT):
                        P.mm(ps[:], wv[:, k, dl * 128:(dl + 1) * 128], XN[:, k, ch * 512:(ch + 1) * 512],
                             start=(k == 0), stop=(k == KT - 1), reads=[wb_, XN], writes=[ps])
                    P.act(QH[:, pi * 2 + dl, ch * 512:(ch + 1) * 512], ps[:], AF.Copy, reads=[ps], writes=[QH])
        for ch in range(2):
            cs = slice(ch * 512, (ch + 1) * 512)
            es = []
            for mt in range(2):
                ps = pb[mt]
                for dt in range(4):
                    P.mm(ps[:], KX[:, 4 * h + dt, mt * 128:(mt + 1) * 128], QH[:, dt, cs], start=(dt == 0), stop=(dt == 3),
                         reads=[KX, QH], writes=[ps])
                e = E[(ch * 2 + mt) % 3]
                P.act(e[:], ps[:], AF.Exp, reads=[ps], writes=[e], scale=xscale)
                es.append(e)
            pdn = pb[2]
            for mt in range(2):
                P.mm(pdn[:], ones_bf[:], es[mt][:], start=(mt == 0), stop=(mt == 1), reads=[ones_bf, es[mt]], writes=[pdn])
            P.recip(rden[:], pdn[:], reads=[pdn], writes=[rden])
            for dt in range(4):
                po = pb[4 + dt % 4]
                for mt in range(2):
                    P.mm(po[:], VX[:, mt, h * 512 + dt * 128:h * 512 + (dt + 1) * 128], es[mt][:], start=(mt == 0), stop=(mt == 1),
                         reads=[VX, es[mt]], writes=[po])
                P.tt("dve", OH[:, dt, cs], po[:], rden[:], ALU.mult, reads=[po, rden], writes=[OH])
        linear_acc(wo_d, OH, 4, row0=h * 512)

    norm_to_XN(2)
    wrb = sc1.take(0, KT * 36, F32, "wr"); wr = shaped(wrb, "p (a b) -> p a b", a=KT); sc1.kids[-1] = (0, KT * 36, wr)
    P.dma("sp", wr[:], router_d[:].rearrange("(kt p) n -> p kt n", p=128), writes=[wr])
    for k in range(KT):
        P.ts("dve", wr[:, k, :], wr[:, k, :], nrm[:, k, 2:3], None, ALU.mult, reads=[wr, nrm], writes=[wr])
    ones_col = P.sb([128, 1], BF16, "ones_col"); P.memset("pool", ones_col[:], 1.0, writes=[ones_col])
    Gt = P.sb([128, 8, 32], F32, "Gt")
    lg = P.sb([128, 36], F32, "lg"); g8 = P.sb([128, 8], F32, "g8"); m8 = P.sb([128, 8], F32, "m8")
    sm = P.sb([128, 8], F32, "sm"); lem = P.sb([128, 4, 8], F32, "lem"); sel = P.sb([128, 32], F32, "sel")
    ex = P.sb([128, 32], F32, "ex")
    for tt_ in range(8):
        tsl = slice(tt_ * 128, (tt_ + 1) * 128)
        pss = pb[tt_ % 2]; pl = pb[2 + tt_ % 2]
        for k in range(KT):
            s_ = sq[k % 2]
            P.act(s_[:, 0:128], HT[:, k, tsl], AF.Square, reads=[HT], writes=[s_])
            P.mm(pss[:, 0:1], s_[:, 0:128], ones_col[:], start=(k == 0), stop=(k == KT - 1), reads=[s_, ones_col], writes=[pss])
        for k in range(KT):
            P.mm(pl[:, 0:36], HT[:, k, tsl], wr[:, k, :], start=(k == 0), stop=(k == KT - 1), reads=[HT, wr], writes=[pl])
        P.act(sm[:, 0:1], pss[:, 0:1], AF.Sqrt, reads=[pss, eps], writes=[sm], scale=1.0 / D, bias=eps[:])
        P.recip(sm[:, 0:1], sm[:, 0:1], reads=[sm], writes=[sm])
        P.ts("dve", lg[:], pl[:, 0:36], sm[:, 0:1], None, ALU.mult, reads=[pl, sm], writes=[lg])
        P.red(sm[:, 1:2], lg[:, 0:4], ALU.max, reads=[lg], writes=[sm])
        P.ts("dve", g8[:, 0:4], lg[:, 0:4], sm[:, 1:2], None, ALU.subtract, reads=[lg, sm], writes=[g8])
        P.act(g8[:, 4:8], g8[:, 0:4], AF.Exp, reads=[g8], writes=[g8])
        P.red(sm[:, 2:3], g8[:, 4:8], ALU.add, reads=[g8], writes=[sm])
        P.ts("dve", g8[:, 0:4], lg[:, 0:4], sm[:, 1:2], None, ALU.is_ge, reads=[lg, sm], writes=[g8])
        P.ts("dve", g8[:, 0:4], g8[:, 0:4], 1e30, -1e30, ALU.mult, ALU.add, reads=[g8], writes=[g8])
        pen = bass.AP(g8.t, 0, [[8, 128], [1, 4], [0, 8]])
        P.tt("dve", lem[:], lg[:, 4:36].rearrange("p (g e) -> p g e", g=4), pen, ALU.add, reads=[lg, g8], writes=[lem])
        lem2 = lem[:].rearrange("p g e -> p (g e)")
        P.op("dve", (lambda o_, i_: (lambda e: e.max(o_, i_)))(m8[:], lem2), reads=[lem], writes=[m8])
        P.ts("dve", sel[:], lem2, m8[:, 1:2], None, ALU.is_ge, reads=[lem, m8], writes=[sel])
        P.ts("dve", sm[:, 3:4], m8[:, 0:1], -1.0, None, ALU.mult, reads=[m8], writes=[sm])
        P.act(ex[:], lem2, AF.Exp, reads=[lem, sm], writes=[ex], bias=sm[:, 3:4])
        P.act(sm[:, 4:5], m8[:, 1:2], AF.Exp, reads=[m8, sm], writes=[sm], bias=sm[:, 3:4])
        P.ts("dve", sm[:, 4:5], sm[:, 4:5], 1.0, None, ALU.add, reads=[sm], writes=[sm])
        P.tt("dve", sm[:, 4:5], sm[:, 4:5], sm[:, 2:3], ALU.mult, reads=[sm], writes=[sm])
        P.recip(sm[:, 5:6], sm[:, 4:5], reads=[sm], writes=[sm])
        P.stt(Gt[:, tt_, :], ex[:], sm[:, 5:6], sel[:], ALU.mult, ALU.mult, reads=[ex, sm, sel], writes=[Gt])

    H1 = QH
    dg = [sc1.take(640 + 128 * i, 128, F32, f"dg{i}") for i in range(2)]
    gbc = [sc2.take(1024 + 512 * i, 512, F32, f"gbc{i}") for i in range(2)]
    sl = [sc2.take(2048 + 512 * i, 512, F32, f"sl{i}") for i in range(2)]
    for xs_ in xwaits:
        P.ext_wait("sp", xs_, 1)
        P.ext_wait("act", xs_, 1)
    for e_ in range(NE):
        w1s = w1_d[e_].rearrange("(kt p) n -> p kt n", p=128)
        w3s = w3_d[e_].rearrange("(kt p) n -> p kt n", p=128)
        w2s = w2_d[e_].rearrange("(ft p) n -> p ft n", p=128)
        pieces = [(w1s[:, :, 0:256], (KT, 256), w1_d), (w3s[:, :, 0:256], (KT, 256), w3_d),
                  (w1s[:, :, 256:512], (KT, 256), w1_d), (w3s[:, :, 256:512], (KT, 256), w3_d),
                  (w2s[:, :, 0:1024], (4, 1024), w2_d), (w2s[:, :, 1024:2048], (4, 1024), w2_d)]
        for ch in range(2):
            pgt = pb[6 + ch]
            for tq in range(4):
                tt_ = ch * 4 + tq
                d_ = dg[tq % 2]
                P.ts("dve", d_[:], ident[:], Gt[:, tt_, e_:e_ + 1], None, ALU.mult, reads=[ident, Gt], writes=[d_])
                P.mm(pgt[:, tq * 128:(tq + 1) * 128], ones32[:], d_[:], reads=[ones32, d_], writes=[pgt])
            P.act(gbc[ch][:], pgt[:], AF.Copy, reads=[pgt], writes=[gbc[ch]])
        hold = {}
        it = 0
        for pi, wb_, wv in stream(ws, pieces):
            if pi in (0, 2):
                hold["w1"] = (wb_, wv)
                continue
            if pi in (1, 3):
                half = pi // 2
                w1b, w1v = hold["w1"]
                for fl in range(2):
                    ft = half * 2 + fl
                    for ch in range(2):
                        cs = slice(ch * 512, (ch + 1) * 512)
                        p1 = pb[(it % 2) * 2]; p3 = pb[(it % 2) * 2 + 1]; it += 1
                        for k in range(KT):
                            P.mm(p1[:], w1v[:, k, fl * 128:(fl + 1) * 128], XN[:, k, cs], start=(k == 0), stop=(k == KT - 1),
                                 reads=[w1b, XN], writes=[p1])
                        for k in range(KT):
                            P.mm(p3[:], wv[:, k, fl * 128:(fl + 1) * 128], XN[:, k, cs], start=(k == 0), stop=(k == KT - 1),
                                 reads=[wb_, XN], writes=[p3])
                        s_ = sl[it % 2]
                        P.act(s_[:], p1[:], AF.Silu, reads=[p1], writes=[s_])
                        P.tt("dve", s_[:], s_[:], p3[:], ALU.mult, reads=[s_, p3], writes=[s_])
                        P.tt("dve", H1[:, ft, cs], s_[:], gbc[ch][:], ALU.mult, reads=[s_, gbc[ch]], writes=[H1])
                continue
            half = pi - 4
            for dl in range(8):
                dt = half * 8 + dl
                for ch in range(2):
                    cs = slice(ch * 512, (ch + 1) * 512)
                    ps = pb[4 + it % 2]; it += 1
                    for ft in range(4):
                        P.mm(ps[:], wv[:, ft, dl * 128:(dl + 1) * 128], H1[:, ft, cs], start=(ft == 0), stop=(ft == 3),
                             reads=[wb_, H1], writes=[ps])
                    P.tt("dve", HT[:, dt, cs], HT[:, dt, cs], ps[:], ALU.add, reads=[HT, ps], writes=[HT])

    osrc = out_d[:].rearrange("(kt p) t -> p kt t", p=128)
    if final_norm:
        for ch in range(2):
            cs = slice(ch * 512, (ch + 1) * 512)
            rms_rstd([(HT, HT[:, k, cs]) for k in range(KT)], D, pb[ch])
            for k in range(KT):
                P.stt(HT[:, k, cs], HT[:, k, cs], nrm[:, k, 3:4], rstd[:], ALU.mult, ALU.mult, reads=[HT, nrm, rstd], writes=[HT])
    for k4 in range(4):
        P.dma("sp" if k4 % 2 == 0 else "act", osrc[:, k4 * 4:(k4 + 1) * 4, :], HT[:, k4 * 4:(k4 + 1) * 4, :], reads=[HT], writes=[out_d])


def host_P2(d, L):
    pvec = np.zeros((128, 4, 3), np.float32)
    pvec[:, :, 0] = d["s5_glu_b"][L].reshape(4, 128).T
    pvec[:, :, 1] = d["mix_out_norm"][L][0].reshape(4, 128).T
    pvec[:, :, 2] = d["mix_out_norm"][L][1].reshape(4, 128).T
    nrm = np.zeros((128, 16, 4), np.float32)
    for i, v in enumerate((d["norm_cross"][L], d["norm_mem"][L], d["norm_moe"][L], d["norm_final"])):
        nrm[:, :, i] = v.reshape(16, 128).T
    router = np.ascontiguousarray(np.concatenate([d["router_group"][L], d["router_expert"][L]], axis=1))
    return pvec, nrm, router


import time as _time
from concourse.bass_utils import run_bass_kernel_spmd

O_B = 1536; O_C = 1536 + 1920; O_D = 1536 + 1920 + 1536
_PROGS = {}


def _din(P, name, shape):
    return P.dram(name, shape, F32, kind="ExternalInput")


def build_P1(layer_idx):
    nc = bass.Bass("TRN2", target_bir_lowering=False)
    P = _mk(Prog(nc))
    hT = _din(P, "hT", [D, S]); g_mix = _din(P, "g_mix", [D])
    w_a = _din(P, "w_a", [D, 384]); lamv = _din(P, "lamv_d", [4, 64]); subln = _din(P, "subln_d", [128])
    cosA = _din(P, "cosA", [128, S]); sinA = _din(P, "sinA", [128, S]); rmA = _din(P, "rmA", [128, 128])
    oaT = P.dram("oaT", [128, S], F32, kind="ExternalOutput")
    emit_A(P, layer_idx, hT, g_mix, w_a, lamv, subln, cosA, sinA, rmA, oaT)
    P.wait_all_dma("sp"); P.emit()
    P = _mk(Prog(nc))
    hT = Buf(hT.t, "hT"); g_mix = Buf(g_mix.t, "g_mix")
    w_c = _din(P, "w_c", [D, 384]); cosC = _din(P, "cosC", [128, S]); sinC = _din(P, "sinC", [128, S])
    rmC = _din(P, "rmC", [128, 128]); maskC = _din(P, "maskC", [128, 20, 512])
    ocT = P.dram("ocT", [128, S], F32, kind="ExternalOutput")
    emit_C(P, hT, g_mix, w_c, cosC, sinC, rmC, maskC, ocT)
    P.wait_all_dma("sp"); P.emit()
    P = _mk(Prog(nc))
    hT = Buf(hT.t, "hT"); g_mix = Buf(g_mix.t, "g_mix")
    w_d = _din(P, "w_d", [D, 128]); dpar_d = _din(P, "dpar_d", [128, 3, 8]); dskip_d = _din(P, "dskip_d", [128])
    bblk_d = _din(P, "bblk_d", [128, 2, 4, 128]); cblk_d = _din(P, "cblk_d", [128, 2, 4, 128])
    glT = P.dram("glT", [128, S], F32, kind="ExternalOutput")
    emit_D(P, hT, g_mix, w_d, dpar_d, dskip_d, bblk_d, cblk_d, glT)
    P.wait_all_dma("sp"); P.emit()
    P = _mk(Prog(nc))
    hT = Buf(hT.t, "hT"); g_mix = Buf(g_mix.t, "g_mix")
    w_b = _din(P, "w_b", [D, 768]); bvec_d = _din(P, "bvec_d", [128, NV])
    w2_d = _din(P, "w2_d", [128, 128]); a2_d = _din(P, "a2_d", [128, 128]); g2_d = _din(P, "g2_d", [128, 128])
    ident_d = _din(P, "ident_d", [128, 128]); bones_d = _din(P, "bones_d", [128, 128])
    obT = P.dram("obT", [128, S], F32, kind="ExternalOutput")
    emit_B(P, hT, g_mix, w_b, bvec_d, w2_d, a2_d, g2_d, ident_d, bones_d, obT)
    P.wait_all_dma("sp"); P.emit()
    return nc


def build_P2(final_norm):
    nc = bass.Bass("TRN2", target_bir_lowering=False)
    P = _mk(Prog(nc))
    hT_d = _din(P, "hT", [D, TT]); mixT_d = _din(P, "mixT", [D, TT]); memT_d = _din(P, "memT", [D, 256])
    w_out_d = _din(P, "w_out", [D, D]); glu_w_d = _din(P, "glu_w", [512, 512]); pvec_d = _din(P, "pvec", [128, 4, 3])
    nrm_d = _din(P, "nrm", [128, 16, 4]); wq_d = _din(P, "wq", [D, D]); wkv_d = _din(P, "wkv", [D, 2 * D]); wo_d = _din(P, "wo", [D, D])
    router_d = _din(P, "router", [D, 36]); w1_d = _din(P, "w1", [NE, D, 512]); w3_d = _din(P, "w3", [NE, D, 512])
    w2_d = _din(P, "w2", [NE, 512, D]); ident_d = _din(P, "ident_d", [128, 128])
    out_d = P.dram("outT", [D, TT], F32, kind="ExternalOutput")
    emit_P2(P, hT_d, mixT_d, memT_d, w_out_d, glu_w_d, pvec_d, nrm_d, wq_d, wkv_d, wo_d, router_d, w1_d, w3_d, w2_d, ident_d,
            out_d, final_norm=final_norm)
    P.wait_all_dma("sp"); P.emit()
    return nc


def kernel(**inputs):
    d = {k: np.asarray(v) for k, v in inputs.items()}
    x = d["x"]
    cosA, sinA, rmA = consts_A()
    cosC, sinC, rmC, maskC = consts_C()
    ident, bones = consts_B()
    hT_b = [np.ascontiguousarray(x[b].T) for b in range(2)]
    out = None
    for L in range(2):
        w_in = d["w_in"][L]
        nc1 = build_P1(L)
        ins = []
        for c in range(8):
            b, j = divmod(c, 4)
            sl = lambda o: w_in[:, o + j * 128:o + (j + 1) * 128]
            colsets, bvec, w2, a2, g2 = host_B(d, L, j)
            dpar, bblk, cblk, dsk = host_D(d, L, j)
            ins.append({
                "hT": hT_b[b], "g_mix": d["norm_mix"][L],
                "w_a": np.ascontiguousarray(np.concatenate([sl(0), sl(512), sl(1024)], 1)),
                "lamv_d": d["diff_lambda"][L], "subln_d": d["diff_subln"][L], "cosA": cosA, "sinA": sinA, "rmA": rmA,
                "w_c": np.ascontiguousarray(np.concatenate([sl(O_C), sl(O_C + 512), sl(O_C + 1024)], 1)),
                "cosC": cosC, "sinC": sinC, "rmC": rmC, "maskC": maskC,
                "w_d": np.ascontiguousarray(sl(O_D)), "dpar_d": dpar, "dskip_d": dsk, "bblk_d": bblk, "cblk_d": cblk,
                "w_b": np.ascontiguousarray(w_in[:, O_B + np.concatenate(colsets)]), "bvec_d": bvec, "w2_d": w2, "a2_d": a2, "g2_d": g2,
                "ident_d": ident, "bones_d": bones,
            })
        res = run_bass_kernel_spmd(nc1, ins, core_ids=list(range(8)))
        mixT = [np.zeros((D, S), np.float32) for _ in range(2)]
        for c in range(8):
            b, j = divmod(c, 4)
            r = res.results[c]
            for gi, nm in enumerate(("oaT", "obT", "ocT", "glT")):
                mixT[b][gi * 512 + j * 128:gi * 512 + (j + 1) * 128, :] = r[nm]
        del res
        nc2 = build_P2(final_norm=(L == 1))
        pvec, nrm, router = host_P2(d, L)
        shared = {"w_out": d["w_out"][L], "glu_w": d["s5_glu_w"][L], "pvec": pvec, "nrm": nrm, "wq": d["xa_wq"][L],
                  "wkv": d["xa_wkv"][L], "wo": d["xa_wo"][L], "router": router, "w1": d["moe_w1"][L], "w3": d["moe_w3"][L],
                  "w2": d["moe_w2"][L], "ident_d": ident}
        ins = []
        for c in range(8):
            b, tq = divmod(c, 4)
            ts_ = slice(tq * TT, (tq + 1) * TT)
            m = dict(shared)
            m.update({"hT": np.ascontiguousarray(hT_b[b][:, ts_]), "mixT": np.ascontiguousarray(mixT[b][:, ts_]),
                      "memT": np.ascontiguousarray(d["mem"][b].T)})
            ins.append(m)
        res = run_bass_kernel_spmd(nc2, ins, core_ids=list(range(8)))
        new_hT = [np.zeros((D, S), np.float32) for _ in range(2)]
        for c in range(8):
            b, tq = divmod(c, 4)
            new_hT[b][:, tq * TT:(tq + 1) * TT] = res.results[c]["outT"]
        del res
        hT_b = new_hT
    out = np.stack([hT_b[b].T for b in range(2)], axis=0).astype(np.float32)
    return np.ascontiguousarray(out)
```
